# Optimizing a Trainium2 kernel written in Bass

```python
import math
import jax
import jax.numpy as jnp
from jax import lax
import numpy as np

D_MODEL = 1024
BATCH = 8
SEQ = 4096
DEPTH = 4

GRID_W = 64
CTX_LEN = 256
N_MIXERS = 4
N_MOD = 6
Q_BLOCK = 128
ROPE_THETA = 10000.0
NORM_EPS = 1e-6

A_HEADS = 8
A_DK = 128
A_DV = D_MODEL // A_HEADS
A_WIDTH = A_HEADS * A_DK
A_CHUNK = 32
B_HEADS = 16
B_Q_RANK = 384
B_KV_RANK = 256
B_NOPE = 64
B_ROPE = 32
B_V = 64
C_HEADS = 8
C_KV_HEADS = 2
C_HEAD_DIM = 128
D_HEADS = 8
D_QK_DIM = 64
D_V_DIM = 2 * D_QK_DIM
FF_DIM = 3584
N_EXPERTS = 8
TOP_K = 2

kernel_name = "hybrid_latent_diffusion_block"


def rms_norm(x, g):
    xf = x.astype(jnp.float32)
    y = xf * lax.rsqrt(jnp.mean(xf * xf, axis=-1, keepdims=True) + NORM_EPS)
    return (y * g.astype(jnp.float32)).astype(x.dtype)


def modulate(x, shift, scale):
    return x * (1 + scale) + shift


def split_last(z, sizes):
    idx, acc = [], 0
    for s in sizes[:-1]:
        acc += s
        idx.append(acc)
    return jnp.split(z, idx, axis=-1)


def _rotate(x, pos):
    n = x.shape[-1]
    inv = ROPE_THETA ** (-jnp.arange(0, n, 2, dtype=jnp.float32) / n)
    ang = pos.astype(jnp.float32)[:, None] * inv[None, :]
    cos, sin = jnp.cos(ang).astype(x.dtype), jnp.sin(ang).astype(x.dtype)
    x1, x2 = x[..., : n // 2], x[..., n // 2:]
    return jnp.concatenate([x1 * cos - x2 * sin, x2 * cos + x1 * sin], axis=-1)


def axial_rope(x, pos_row, pos_col):
    half = x.shape[-1] // 2
    return jnp.concatenate([_rotate(x[..., :half], pos_row), _rotate(x[..., half:], pos_col)], axis=-1)


def attend(q, k, v, scale):
    s = jnp.einsum("bhgqd,bhkd->bhgqk", q, k, preferred_element_type=jnp.float32) * scale
    p = jax.nn.softmax(s, axis=-1).astype(v.dtype)
    return jnp.einsum("bhgqk,bhkv->bhgqv", p, v)


def blocked_attend(q, k, v, scale):
    b, hk, g, lq, dk = q.shape
    nb = lq // Q_BLOCK
    qb = jnp.moveaxis(q.reshape(b, hk, g, nb, Q_BLOCK, dk), 3, 0)
    ob = lax.map(lambda blk: attend(blk, k, v, scale), qb)
    return jnp.moveaxis(ob, 0, 3).reshape(b, hk, g, lq, v.shape[-1])


def merge_heads(o):
    b, hk, g, l, dv = o.shape
    return o.transpose(0, 3, 1, 2, 4).reshape(b, l, hk * g * dv)


def chunked_gla(q, k, v, log_f, s0, with_output):
    bsz, h, l, _ = k.shape
    dv = v.shape[-1]
    n = l // A_CHUNK

    def chunks(t):
        return jnp.moveaxis(t.reshape(bsz, h, n, A_CHUNK, t.shape[-1]), 2, 0)

    k, v, log_f = chunks(k), chunks(v), chunks(log_f)
    cum = jnp.cumsum(log_f, axis=3)
    cum_end = cum[:, :, :, -1:, :]
    k_to_end = k * jnp.exp(cum_end - cum)
    chunk_decay = jnp.exp(cum_end[:, :, :, 0, :])
    if with_output:
        q_dec = chunks(q) * jnp.exp(cum)
        att = jnp.einsum("nbhcd,nbhsd->nbhcs", q_dec, k * jnp.exp(-cum))
        mask = jnp.tril(jnp.ones((A_CHUNK, A_CHUNK), dtype=bool))
        o_intra = jnp.einsum("nbhcs,nbhsv->nbhcv", jnp.where(mask, att, 0.0), v)
        xs = (q_dec, k_to_end, v, chunk_decay)
    else:
        xs = (k_to_end, v, chunk_decay)

    def step(state, inp):
        if with_output:
            qd, kd, vv, dec = inp
            o = jnp.einsum("bhcd,bhdv->bhcv", qd, state)
        else:
            kd, vv, dec = inp
            o = None
        state = dec[..., None] * state + jnp.einsum("bhcd,bhcv->bhdv", kd, vv)
        return state, o

    s_final, o_inter = lax.scan(step, s0, xs)
    if not with_output:
        return None, s_final
    o = jnp.moveaxis(o_intra + o_inter, 0, 2).reshape(bsz, h, l, dv)
    return o, s_final


def hgrn2_mixer(u_lat, u_ctx, w_in, lb_fwd, lb_bwd, onorm_g, w_out, ctx_out):
    def project(u):
        bsz, l, _ = u.shape
        z = (u @ w_in).astype(jnp.float32)
        q, f_fwd, f_bwd, inp, gate = split_last(
            z, (A_WIDTH, A_WIDTH, A_WIDTH, A_HEADS * A_DV, A_HEADS * A_DV))

        def heads(t):
            return t.reshape(bsz, l, A_HEADS, -1).transpose(0, 2, 1, 3)

        dirs = []
        for f_raw, lb in ((f_fwd, lb_fwd), (f_bwd, lb_bwd)):
            f = lb + (1.0 - lb) * jax.nn.sigmoid(f_raw)
            dirs.append((heads(1.0 - f), heads(jnp.log(f))))
        return heads(q) * A_DK ** -0.5, dirs, heads(inp), gate

    def flip(t):
        return jnp.flip(t, axis=2)

    q_c, (fw_c, bw_c), v_c, gate_c = project(u_ctx)
    q_l, (fw_l, bw_l), v_l, gate_l = project(u_lat)
    s0 = jnp.zeros((u_ctx.shape[0], A_HEADS, A_DK, A_DV), jnp.float32)
    o_cf, s_cf = chunked_gla(q_c, fw_c[0], v_c, fw_c[1], s0, ctx_out)
    o_cb, s_cb = chunked_gla(flip(q_c), flip(bw_c[0]), flip(v_c), flip(bw_c[1]), s0, ctx_out)
    o_lf, _ = chunked_gla(q_l, fw_l[0], v_l, fw_l[1], s_cf, True)
    o_lb, _ = chunked_gla(flip(q_l), flip(bw_l[0]), flip(v_l), flip(bw_l[1]), s_cb, True)

    def readout(o, gate, dtype):
        bsz, _, l, _ = o.shape
        o = rms_norm(o, onorm_g).transpose(0, 2, 1, 3).reshape(bsz, l, A_HEADS * A_DV)
        return (o * jax.nn.silu(gate)).astype(dtype) @ w_out

    y_lat = readout(o_lf + flip(o_lb), gate_l, u_lat.dtype)
    y_ctx = readout(o_cf + flip(o_cb), gate_c, u_ctx.dtype) if ctx_out else None
    return y_lat, y_ctx


def mla_mixer(u_lat, u_ctx, w_in, qnorm_g, kvnorm_g, w_qb, w_kvb, w_out, pos_row, pos_col, ctx_out):
    def project(u, rotary):
        bsz, l, _ = u.shape
        c_q, c_kv, k_rope = split_last(u @ w_in, (B_Q_RANK, B_KV_RANK, B_ROPE))
        q = (rms_norm(c_q, qnorm_g) @ w_qb).reshape(bsz, l, B_HEADS, B_NOPE + B_ROPE).transpose(0, 2, 1, 3)
        kv = (rms_norm(c_kv, kvnorm_g) @ w_kvb).reshape(bsz, l, B_HEADS, B_NOPE + B_V).transpose(0, 2, 1, 3)
        q_nope, q_rope = q[..., :B_NOPE], q[..., B_NOPE:]
        k_nope, v = kv[..., :B_NOPE], kv[..., B_NOPE:]
        k_rope = k_rope[:, None]
        if rotary:
            q_rope = axial_rope(q_rope, pos_row, pos_col)
            k_rope = axial_rope(k_rope, pos_row, pos_col)
        q = jnp.concatenate([q_nope, q_rope], axis=-1)[:, :, None]
        k = jnp.concatenate([k_nope, jnp.broadcast_to(k_rope, k_nope.shape[:3] + (B_ROPE,))], axis=-1)
        return q, k, v

    scale = (B_NOPE + B_ROPE) ** -0.5
    q_c, k_c, v_c = project(u_ctx, False)
    q_l, k_l, v_l = project(u_lat, True)
    o_lat = blocked_attend(q_l, jnp.concatenate([k_c, k_l], axis=2), jnp.concatenate([v_c, v_l], axis=2), scale)
    y_lat = merge_heads(o_lat) @ w_out
    y_ctx = merge_heads(attend(q_c, k_c, v_c, scale)) @ w_out if ctx_out else None
    return y_lat, y_ctx


def gqa_mixer(u_lat, u_ctx, w_in, qnorm_g, knorm_g, w_out, pos_row, pos_col, ctx_out):
    group = C_HEADS // C_KV_HEADS

    def project(u, rotary):
        bsz, l, _ = u.shape
        q, k, v = split_last(u @ w_in, (C_HEADS * C_HEAD_DIM, C_KV_HEADS * C_HEAD_DIM, C_KV_HEADS * C_HEAD_DIM))
        q = rms_norm(q.reshape(bsz, l, C_KV_HEADS, group, C_HEAD_DIM), qnorm_g).transpose(0, 2, 3, 1, 4)
        k = rms_norm(k.reshape(bsz, l, C_KV_HEADS, C_HEAD_DIM), knorm_g).transpose(0, 2, 1, 3)
        v = v.reshape(bsz, l, C_KV_HEADS, C_HEAD_DIM).transpose(0, 2, 1, 3)
        if rotary:
            q = axial_rope(q, pos_row, pos_col)
            k = axial_rope(k, pos_row, pos_col)
        return q, k, v

    scale = C_HEAD_DIM ** -0.5
    q_c, k_c, v_c = project(u_ctx, False)
    q_l, k_l, v_l = project(u_lat, True)
    o_lat = blocked_attend(q_l, jnp.concatenate([k_c, k_l], axis=2), jnp.concatenate([v_c, v_l], axis=2), scale)
    y_lat = merge_heads(o_lat) @ w_out
    y_ctx = merge_heads(attend(q_c, k_c, v_c, scale)) @ w_out if ctx_out else None
    return y_lat, y_ctx


def diff_mixer(u_lat, u_ctx, w_in, lam, onorm_g, w_out, lambda_init, pos_row, pos_col, ctx_out):
    def project(u, rotary):
        bsz, l, _ = u.shape
        q, k, v = split_last(u @ w_in, (2 * D_HEADS * D_QK_DIM, 2 * D_HEADS * D_QK_DIM, D_HEADS * D_V_DIM))
        q = q.reshape(bsz, l, D_HEADS, 2, D_QK_DIM).transpose(0, 3, 2, 1, 4)
        k = k.reshape(bsz, l, D_HEADS, 2, D_QK_DIM).transpose(0, 3, 2, 1, 4)
        v = v.reshape(bsz, l, D_HEADS, D_V_DIM).transpose(0, 2, 1, 3)
        if rotary:
            q = axial_rope(q, pos_row, pos_col)
            k = axial_rope(k, pos_row, pos_col)
        return q, k, v

    lam = lam.astype(jnp.float32)
    lam_full = jnp.exp(jnp.sum(lam[0] * lam[1])) - jnp.exp(jnp.sum(lam[2] * lam[3])) + lambda_init
    scale = D_QK_DIM ** -0.5

    def diff_attn(q, k, v, attn):
        a1 = attn(q[:, 0, :, None], k[:, 0], v, scale)
        a2 = attn(q[:, 1, :, None], k[:, 1], v, scale)
        o = a1 - lam_full.astype(a1.dtype) * a2
        return merge_heads(rms_norm(o, onorm_g) * (1.0 - lambda_init)) @ w_out

    q_c, k_c, v_c = project(u_ctx, False)
    q_l, k_l, v_l = project(u_lat, True)
    y_lat = diff_attn(q_l, jnp.concatenate([k_c, k_l], axis=3), jnp.concatenate([v_c, v_l], axis=2), blocked_attend)
    y_ctx = diff_attn(q_c, k_c, v_c, attend) if ctx_out else None
    return y_lat, y_ctx


def swiglu(h, w_gu, w_down):
    gate, up = jnp.split(h @ w_gu, 2, axis=-1)
    return (jax.nn.silu(gate) * up) @ w_down


def moe_swiglu(h, router, w_gu, w_down):
    logits = jnp.einsum("...d,de->...e", h, router, preferred_element_type=jnp.float32)
    top_v, top_i = lax.top_k(logits, TOP_K)
    wts = jax.nn.softmax(top_v, axis=-1)
    combine = jnp.einsum("...k,...ke->...e", wts, jax.nn.one_hot(top_i, N_EXPERTS, dtype=jnp.float32)).astype(h.dtype)
    out = jnp.zeros_like(h)
    for e in range(N_EXPERTS):
        out = out + combine[..., e:e + 1] * swiglu(h, w_gu[e], w_down[e])
    return out


def setup_inputs(seed: int = 0) -> dict:
    key = jax.random.key(seed)
    ks = jax.random.split(key, 30)

    def nrm(k, shape, scale):
        return jax.random.normal(k, shape, jnp.float32) * scale

    d = D_MODEL
    n_a = len(range(0, DEPTH, N_MIXERS))
    n_b = len(range(1, DEPTH, N_MIXERS))
    n_c = len(range(2, DEPTH, N_MIXERS))
    n_d = len(range(3, DEPTH, N_MIXERS))
    n_dense = len(range(0, DEPTH, 2))
    n_moe = len(range(1, DEPTH, 2))
    a_in = 3 * A_WIDTH + 2 * A_HEADS * A_DV
    b_in = B_Q_RANK + B_KV_RANK + B_ROPE
    c_in = (C_HEADS + 2 * C_KV_HEADS) * C_HEAD_DIM
    d_in = 4 * D_HEADS * D_QK_DIM + D_HEADS * D_V_DIM
    return {
        "x": nrm(ks[0], (BATCH, SEQ, d), 1.0),
        "c": nrm(ks[1], (BATCH, d), 1.0),
        "ctx": nrm(ks[2], (BATCH, CTX_LEN, d), 1.0),
        "c_ctx": nrm(ks[3], (d,), 1.0),
        "mod_w": nrm(ks[4], (DEPTH, d, N_MOD * d), 0.2 * d ** -0.5),
        "mod_b": nrm(ks[5], (DEPTH, N_MOD * d), 0.02),
        "norm_g": 1.0 + nrm(ks[6], (DEPTH, 4, d), 0.05),
        "a_w_in": nrm(ks[7], (n_a, d, a_in), d ** -0.5),
        "a_lb": nrm(ks[8], (2, DEPTH + 1, A_WIDTH), 0.1),
        "a_onorm_g": 1.0 + nrm(ks[9], (n_a, A_DV), 0.05),
        "a_w_out": nrm(ks[10], (n_a, A_HEADS * A_DV, d), (A_HEADS * A_DV) ** -0.5),
        "b_w_in": nrm(ks[11], (n_b, d, b_in), d ** -0.5),
        "b_qnorm_g": 1.0 + nrm(ks[12], (n_b, B_Q_RANK), 0.05),
        "b_kvnorm_g": 1.0 + nrm(ks[13], (n_b, B_KV_RANK), 0.05),
        "b_w_qb": nrm(ks[14], (n_b, B_Q_RANK, B_HEADS * (B_NOPE + B_ROPE)), B_Q_RANK ** -0.5),
        "b_w_kvb": nrm(ks[15], (n_b, B_KV_RANK, B_HEADS * (B_NOPE + B_V)), B_KV_RANK ** -0.5),
        "b_w_out": nrm(ks[16], (n_b, B_HEADS * B_V, d), (B_HEADS * B_V) ** -0.5),
        "c_w_in": nrm(ks[17], (n_c, d, c_in), d ** -0.5),
        "c_qnorm_g": 1.0 + nrm(ks[18], (n_c, C_HEAD_DIM), 0.05),
        "c_knorm_g": 1.0 + nrm(ks[19], (n_c, C_HEAD_DIM), 0.05),
        "c_w_out": nrm(ks[20], (n_c, C_HEADS * C_HEAD_DIM, d), (C_HEADS * C_HEAD_DIM) ** -0.5),
        "d_w_in": nrm(ks[21], (n_d, d, d_in), d ** -0.5),
        "d_lambda": nrm(ks[22], (n_d, 4, D_QK_DIM), 0.1),
        "d_onorm_g": 1.0 + nrm(ks[23], (n_d, D_V_DIM), 0.05),
        "d_w_out": nrm(ks[24], (n_d, D_HEADS * D_V_DIM, d), (D_HEADS * D_V_DIM) ** -0.5),
        "ffn_w_gu": nrm(ks[25], (n_dense, d, 2 * FF_DIM), d ** -0.5),
        "ffn_w_down": nrm(ks[26], (n_dense, FF_DIM, d), FF_DIM ** -0.5),
        "moe_router": nrm(ks[27], (n_moe, d, N_EXPERTS), d ** -0.5),
        "moe_w_gu": nrm(ks[28], (n_moe, N_EXPERTS, d, 2 * FF_DIM), d ** -0.5),
        "moe_w_down": nrm(ks[29], (n_moe, N_EXPERTS, FF_DIM, d), FF_DIM ** -0.5),
    }


def reference(x, c, ctx, c_ctx, mod_w, mod_b, norm_g,
              a_w_in, a_lb, a_onorm_g, a_w_out,
              b_w_in, b_qnorm_g, b_kvnorm_g, b_w_qb, b_w_kvb, b_w_out,
              c_w_in, c_qnorm_g, c_knorm_g, c_w_out,
              d_w_in, d_lambda, d_onorm_g, d_w_out,
              ffn_w_gu, ffn_w_down, moe_router, moe_w_gu, moe_w_down):
    seq_len = x.shape[1]
    rows = seq_len // GRID_W
    pos_row = jnp.repeat(jnp.arange(rows, dtype=jnp.int32), GRID_W)
    pos_col = jnp.arange(rows * GRID_W, dtype=jnp.int32) % GRID_W
    cond_lat = jax.nn.silu(c)
    cond_ctx = jax.nn.silu(c_ctx)
    lower_bounds = jnp.cumsum(jax.nn.softmax(a_lb.astype(jnp.float32), axis=1), axis=1)

    h_lat, h_ctx = x, ctx
    for i in range(DEPTH):
        last = i == DEPTH - 1
        kind, j = i % N_MIXERS, i // N_MIXERS
        mod_lat = jnp.split((cond_lat @ mod_w[i] + mod_b[i])[:, None, :], N_MOD, axis=-1)
        mod_ctx = jnp.split(cond_ctx @ mod_w[i] + mod_b[i], N_MOD, axis=-1)

        u_lat = modulate(rms_norm(h_lat, norm_g[i, 0]), mod_lat[0], mod_lat[1])
        u_ctx = modulate(rms_norm(h_ctx, norm_g[i, 0]), mod_ctx[0], mod_ctx[1])
        if kind == 0:
            y_lat, y_ctx = hgrn2_mixer(u_lat, u_ctx, a_w_in[j], lower_bounds[0, i], lower_bounds[1, i],
                                       a_onorm_g[j], a_w_out[j], not last)
        elif kind == 1:
            y_lat, y_ctx = mla_mixer(u_lat, u_ctx, b_w_in[j], b_qnorm_g[j], b_kvnorm_g[j], b_w_qb[j],
                                     b_w_kvb[j], b_w_out[j], pos_row, pos_col, not last)
        elif kind == 2:
            y_lat, y_ctx = gqa_mixer(u_lat, u_ctx, c_w_in[j], c_qnorm_g[j], c_knorm_g[j], c_w_out[j],
                                     pos_row, pos_col, not last)
        else:
            lambda_init = 0.8 - 0.6 * math.exp(-0.3 * i)
            y_lat, y_ctx = diff_mixer(u_lat, u_ctx, d_w_in[j], d_lambda[j], d_onorm_g[j], d_w_out[j],
                                      lambda_init, pos_row, pos_col, not last)
        h_lat = h_lat + mod_lat[2] * rms_norm(y_lat, norm_g[i, 1])

        def channel_mix(u):
            if i % 2 == 0:
                return swiglu(u, ffn_w_gu[i // 2], ffn_w_down[i // 2])
            return moe_swiglu(u, moe_router[i // 2], moe_w_gu[i // 2], moe_w_down[i // 2])

        u_lat = modulate(rms_norm(h_lat, norm_g[i, 2]), mod_lat[3], mod_lat[4])
        h_lat = h_lat + mod_lat[5] * rms_norm(channel_mix(u_lat), norm_g[i, 3])
        if not last:
            h_ctx = h_ctx + mod_ctx[2] * rms_norm(y_ctx, norm_g[i, 1])
            u_ctx = modulate(rms_norm(h_ctx, norm_g[i, 2]), mod_ctx[3], mod_ctx[4])
            h_ctx = h_ctx + mod_ctx[5] * rms_norm(channel_mix(u_ctx), norm_g[i, 3])
    return h_lat
```

```python
import math
from contextlib import ExitStack
import numpy as np
import concourse.bass as bass
import concourse.mybir as mybir
from concourse.bass_utils import run_bass_kernel_spmd

F32 = mybir.dt.float32
BF16 = mybir.dt.bfloat16
AF = mybir.ActivationFunctionType
ALU = mybir.AluOpType

D = 1024
KC = 8
CTX = 256
LAT = 4096
T = CTX + LAT
NT = T // 128
GRID_W = 64
EPS = 1e-6
FF = 3584
FC = FF // 128
NE = 8
THETA = 10000.0
HG_STOP = 3
FORCE_MOE = False
MLA_PARTS = ('scale', 'lat', 'kr', 'q', 'kn', 'v')
HG_STEPS = 1000


class Buf:
    __slots__ = ("name", "w", "r", "x")

    def __init__(self, name=""):
        self.name = name
        self.w = None
        self.r = {}
        self.x = False


class Sched:
    ENGS = ("pe", "act", "dve", "pool", "sp")

    def __init__(self, nc):
        self.nc = nc
        self.lists = {e: [] for e in self.ENGS}
        self.cnt = {e: 0 for e in self.ENGS}
        self.semh = {}
        self.dcnt = {}
        self.waited = {e: {} for e in self.ENGS}
        self.keymap = {}
        self._ctx = []
        for e in self.ENGS:
            self._sem("E_" + e)

    def _sem(self, key):
        if key not in self.semh:
            cm = self.nc.semaphore(key)
            self.semh[key] = cm.__enter__()
            self._ctx.append(cm)
        return self.semh[key]

    def _deps(self, reads, writes):
        deps = {}

        def add(k, v):
            if deps.get(k, 0) < v:
                deps[k] = v
        for t in reads:
            if t.w is not None:
                add(*t.w)
        for t in writes:
            if t.w is not None:
                add(*t.w)
            for k, v in t.r.items():
                add(k, v)
        return deps

    def _emit_waits(self, eng, deps):
        wd = self.waited[eng]
        own = "E_" + eng
        for k, v in deps.items():
            if eng == "pe" and k == own:
                continue
            if wd.get(k, 0) >= v:
                continue
            wd[k] = v
            h = self.semh[k]
            self.lists[eng].append(lambda e, h=h, v=v: e.wait_ge(h, v))

    def _mark(self, ev, reads, writes):
        k, v = ev
        for t in reads:
            if t.r.get(k, 0) < v:
                t.r[k] = v
        for t in writes:
            t.w = ev
            t.r = {}

    def op(self, eng, fn, reads=(), writes=()):
        xs = [t for t in reads if t.x]
        if xs:
            writes = list(writes) + xs
        self._emit_waits(eng, self._deps(reads, writes))
        self.cnt[eng] += 1
        h = self.semh["E_" + eng]
        self.lists[eng].append(lambda e, fn=fn, h=h: fn(e).then_inc(h, 1))
        self._mark(("E_" + eng, self.cnt[eng]), reads, writes)

    def dma(self, q, key, out, in_, reads=(), writes=()):
        if key not in self.keymap:
            idx = len(self.keymap)
            self.keymap[key] = "D_%d" % idx
        key = self.keymap[key]
        h = self._sem(key)
        deps = self._deps(reads, writes)
        prev = self.dcnt.get(key, 0)
        if prev and deps.get(key, 0) < prev:
            deps[key] = prev
        self._emit_waits(q, deps)
        self.dcnt[key] = prev + 16
        self.lists[q].append(lambda e, out=out, in_=in_, h=h: e.dma_start(out=out, in_=in_).then_inc(h, 16))
        self._mark((key, prev + 16), reads, writes)

    def barrier(self):
        cur = {("E_" + e): self.cnt[e] for e in self.ENGS if self.cnt[e]}
        cur.update({k: v for k, v in self.dcnt.items() if v})
        for e in self.ENGS:
            self._emit_waits(e, dict(cur))
        self.keymap = {}

    def emit(self):
        nc = self.nc
        L = self.lists
        with nc.Block() as block:
            @block.tensor
            def _(e):
                for f in L["pe"]:
                    f(e)

            @block.scalar
            def _(e):
                for f in L["act"]:
                    f(e)

            @block.vector
            def _(e):
                for f in L["dve"]:
                    f(e)

            @block.gpsimd
            def _(e):
                for f in L["pool"]:
                    f(e)

            @block.sync
            def _(e):
                for f in L["sp"]:
                    f(e)


class Tile:
    def __init__(self, t, n=1, name=""):
        self.t = t
        self.b = [Buf(name + str(i)) for i in range(n)]

    def __getitem__(self, idx):
        return self.t[idx]


def _rope_tables(n, nrows, row0):
    cos = np.ones((nrows, T), np.float32)
    sin = np.zeros((nrows, T), np.float32)
    perm = np.zeros((nrows, nrows), np.float32)
    t = np.arange(LAT)
    pos = [(t // GRID_W).astype(np.float32), (t % GRID_W).astype(np.float32)]
    half = n // 2
    m = half
    inv = (THETA ** (-np.arange(0, m, 2, dtype=np.float32) / np.float32(m))).astype(np.float32)
    for f in range(n):
        blk, j = f // half, f % half
        fr = j % (m // 2)
        ang = (pos[blk] * inv[fr]).astype(np.float32)
        cos[row0 + f, CTX:] = np.cos(ang).astype(np.float32)
        s = np.sin(ang).astype(np.float32)
        if j < m // 2:
            sin[row0 + f, CTX:] = -s
            src = f + m // 2
        else:
            sin[row0 + f, CTX:] = s
            src = f - m // 2
        perm[row0 + src, row0 + f] = 1.0
    for f in range(nrows):
        if not (row0 <= f < row0 + n):
            perm[f, f] = 1.0
    return cos, sin, perm


def _consts():
    c = {}
    c["ident"] = np.eye(128, dtype=np.float32)
    s = np.arange(128)
    c["maskf"] = (s[:, None] <= s[None, :]).astype(np.float32)
    c["maskb"] = (s[:, None] >= s[None, :]).astype(np.float32)
    c1, s1, p1 = _rope_tables(32, 96, 64)
    c["cos1"], c["sin1"], c["perm1"] = c1, s1, p1
    c["cos1k"], c["sin1k"] = np.ascontiguousarray(c1[64:96]), np.ascontiguousarray(s1[64:96])
    c["perm1k"] = np.ascontiguousarray(p1[64:96, 64:96])
    c2, s2, p2 = _rope_tables(128, 128, 0)
    c["cos2"], c["sin2"], c["perm2"] = c2, s2, p2
    c3, s3, p3 = _rope_tables(64, 64, 0)
    c["cos3"] = np.concatenate([c3, c3], 0)
    c["sin3"] = np.concatenate([s3, s3], 0)
    pp = np.zeros((128, 128), np.float32)
    pp[:64, :64] = p3
    pp[64:, 64:] = p3
    c["perm3"] = pp
    return c


CONST_SHAPES = {k: v.shape for k, v in _consts().items()}

IN_SHAPES = {
    "x": [LAT, D], "c": [1, D], "ctx": [CTX, D], "c_ctx": [1, D],
    "mod_w": [4, D, 6 * D], "mod_b": [24, D], "norm_g": [16, D],
    "a_w_in": [D, 5120], "a_lb": [10, D], "a_onorm_g": [1, 128], "a_w_out": [D, D],
    "b_w_in": [D, 672], "b_qnorm_g": [3, 128], "b_kvnorm_g": [2, 128], "b_w_qb": [384, 1536],
    "b_w_kvb": [256, 2048], "b_w_out": [D, D],
    "c_w_in": [D, 1536], "c_qnorm_g": [1, 128], "c_knorm_g": [1, 128], "c_w_out": [D, D],
    "d_w_in": [D, 3072], "d_lambda": [1, 256], "d_onorm_g": [1, 128], "d_w_out": [D, D],
    "ffn_w_gu": [2, FC, 128, KC * 256], "ffn_w_down": [2, KC, 128, FF], "moe_router": [2, D, NE],
    "moe_w_gu": [2, NE, FC, 128, KC * 256], "moe_w_down": [2, NE, KC, 128, FF],
}


class Builder:
    def __init__(self, n_layers=4, debug=False):
        self.n_layers = n_layers
        self.debug = debug
        nc = self.nc = bass.Bass("TRN2", target_bir_lowering=False)
        self.S = Sched(nc)
        self.I = {}
        for k, shp in IN_SHAPES.items():
            self.I[k] = nc.dram_tensor(k, list(shp), F32, kind="ExternalInput").ap()
        for k, shp in CONST_SHAPES.items():
            self.I[k] = nc.dram_tensor(k, list(shp), F32, kind="ExternalInput").ap()
        self.out = nc.dram_tensor("out", [LAT, D], F32, kind="ExternalOutput").ap()
        if debug:
            self.dbg = nc.dram_tensor("dbg", [D, T], F32, kind="ExternalOutput").ap()

        def scr(name, shape, dt):
            return nc.dram_tensor(name, shape, dt, kind="Internal").ap()
        self.hT = scr("hT", [D, T], F32)
        self.hb = [Buf("hT%d" % i) for i in range(NT)]
        self.sA = scr("sA", [1536, T], BF16)
        self.sB = scr("sB", [1024, T], BF16)
        self.sC = scr("sC", [1024, T], BF16)
        self.sKR = scr("sKR", [32, T], BF16)
        self.sG = scr("sG", [D, T], BF16)
        self.sV = scr("sV", [T, 1024], BF16)
        self.sF1 = scr("sF1", [D, T], F32)
        self.sF2 = scr("sF2", [D, T], F32)
        self.sF3 = scr("sF3", [D, T], F32)
        self.sF4 = scr("sF4", [D, T], F32)
        self.sOT = scr("sOT", [D, T], BF16)
        self.uid = 0

    def sb(self, es, name, shape, dt, n=1):
        self.uid += 1
        t = es.enter_context(self.nc.sbuf_tensor("s%d_%s" % (self.uid, name), list(shape), dt))
        return Tile(t, n, name)

    def ps(self, es, name, shape, dt=F32, n=1):
        self.uid += 1
        t = es.enter_context(self.nc.psum_tensor("p%d_%s" % (self.uid, name), list(shape), dt))
        tl = Tile(t, n, name)
        for b in tl.b:
            b.x = True
        return tl

    @staticmethod
    def view(tile, ap):
        v = Tile(ap, 0)
        v.b = tile.b
        return v

    def MM(self, out, lhsT, rhs, start, stop, r, w):
        self.S.op("pe", lambda e: e.matmul(out, lhsT=lhsT, rhs=rhs, start=start, stop=stop), r, w)

    def TR(self, out, in_, ident, r, w):
        self.S.op("pe", lambda e: e.transpose(out, in_, ident), r, w)

    def ACT(self, out, in_, func, r, w, bias=None, scale=None, eng="act"):
        kw = {}
        if bias is not None:
            kw["bias"] = bias
        if scale is not None:
            kw["scale"] = scale
        self.S.op(eng, lambda e: e.activation(out=out, in_=in_, func=func, **kw), r, w)

    def TT(self, out, a, b, op, r, w, eng="dve"):
        self.S.op(eng, lambda e: e.tensor_tensor(out=out, in0=a, in1=b, op=op), r, w)

    def TS(self, out, a, s1, s2, op0, op1, r, w, eng="dve"):
        if s2 is None:
            self.S.op(eng, lambda e: e.tensor_scalar(out=out, in0=a, scalar1=s1, scalar2=None, op0=op0), r, w)
        else:
            self.S.op(eng, lambda e: e.tensor_scalar(out=out, in0=a, scalar1=s1, scalar2=s2, op0=op0, op1=op1), r, w)

    def STT(self, out, a, s, b, op0, op1, r, w, eng="dve"):
        self.S.op(eng, lambda e: e.scalar_tensor_tensor(out=out, in0=a, scalar=s, in1=b, op0=op0, op1=op1), r, w)

    def CP(self, out, in_, r, w, eng="dve"):
        if eng == "act":
            self.S.op(eng, lambda e: e.copy(out=out, in_=in_), r, w)
        else:
            self.S.op(eng, lambda e: e.tensor_copy(out=out, in_=in_), r, w)

    def RCP(self, out, in_, r, w):
        self.S.op("dve", lambda e: e.reciprocal(out=out, in_=in_), r, w)

    def MEMSET(self, ap, val, w, eng="pool"):
        self.S.op(eng, lambda e: e.memset(ap, val), (), w)

    def DMA(self, q, key, out, in_, r=(), w=()):
        self.S.dma(q, key, out, in_, r, w)

    def rstd(self, out, ss, n, r, w, tmp, tmpb):
        self.ACT(tmp, ss, AF.Sqrt, r, tmpb, bias=self.epsc[0:ss.shape[0], :], scale=1.0 / n)
        self.RCP(out, tmp, tmpb, w)

    def setup(self, es):
        I = self.I
        self.ident = self.sb(es, "ident", [128, 128], F32)
        self.identb = self.sb(es, "identb", [128, 128], BF16)
        self.onesf = self.sb(es, "onesf", [128, 128], F32)
        self.onesb = self.sb(es, "onesb", [128, 128], BF16)
        self.epsc = self.sb(es, "epsc", [128, 1], F32)
        self.DMA("sp", "c0", self.ident[:], I["ident"][:, :], w=self.ident.b)
        self.CP(self.identb[:], self.ident[:], self.ident.b, self.identb.b)
        self.MEMSET(self.onesf[:], 1.0, self.onesf.b)
        self.MEMSET(self.onesb[:], 1.0, self.onesb.b)
        self.MEMSET(self.epsc[:], EPS, self.epsc.b)
        self.epsc = self.epsc
        epsT = self.epsc
        self.epsc = epsT.t
        self.epsb = epsT.b
        self.condT = self.sb(es, "condT", [128, KC, 2], F32)
        self.normg = self.sb(es, "normg", [128, KC, 16], F32)
        self.modb = self.sb(es, "modb", [128, KC, 24], F32)
        self.alb = self.sb(es, "alb", [128, KC, 10], F32)
        self.small = self.sb(es, "smallc", [128, 9], F32)
        self.modc = self.sb(es, "modc", [128, 6, KC, 2], F32)
        self.Acol = self.sb(es, "Acol", [128, 2, KC, 2], F32)
        self.Bcol = self.sb(es, "Bcol", [128, 2, KC, 2], F32)
        self.Gcol = self.sb(es, "Gcol", [128, 2, KC, 2], F32)
        self.lbc = self.sb(es, "lbc", [128, KC, 2, 2], F32)
        with ExitStack() as e2:
            rows = self.sb(e2, "rows", [24, D], F32)
            pt = self.ps(e2, "setup_ps", [128, 512], F32)

            def rows_to_cols(src_ap, R, dst, C, key):
                self.DMA("sp", key, rows[0:R, 0:C * 128], src_ap, w=rows.b)
                for c in range(C):
                    self.TR(pt[:, 0:R], rows[0:R, c * 128:(c + 1) * 128], self.ident[0:R, 0:R],
                            rows.b + self.ident.b, pt.b)
                    self.CP(dst(c), pt[:, 0:R], pt.b, self._colw)
            self._colw = self.condT.b
            rows_to_cols(I["c"][:, :], 1, lambda c: self.condT[:, c, 0:1], KC, "rw")
            rows_to_cols(I["c_ctx"][:, :], 1, lambda c: self.condT[:, c, 1:2], KC, "rw")
            sg = self.sb(e2, "sgc", [128, KC, 2], F32)
            self.ACT(sg[:], self.condT[:], AF.Sigmoid, self.condT.b, sg.b)
            self.TT(self.condT[:], self.condT[:], sg[:], ALU.mult, sg.b + self.condT.b, self.condT.b)
            self._colw = self.normg.b
            rows_to_cols(I["norm_g"][:, :], 16, lambda c: self.normg[:, c, :], KC, "rw")
            self._colw = self.modb.b
            rows_to_cols(I["mod_b"][:, :], 24, lambda c: self.modb[:, c, :], KC, "rw")
            self._colw = self.alb.b
            rows_to_cols(I["a_lb"][:, :], 10, lambda c: self.alb[:, c, :], KC, "rw")
            self._colw = self.small.b
            rows_to_cols(I["a_onorm_g"][:, :], 1, lambda c: self.small[:, 0:1], 1, "rw")
            rows_to_cols(I["b_qnorm_g"][:, :], 3, lambda c: self.small[:, 1:4], 1, "rw")
            rows_to_cols(I["b_kvnorm_g"][:, :], 2, lambda c: self.small[:, 4:6], 1, "rw")
            rows_to_cols(I["c_qnorm_g"][:, :], 1, lambda c: self.small[:, 6:7], 1, "rw")
            rows_to_cols(I["c_knorm_g"][:, :], 1, lambda c: self.small[:, 7:8], 1, "rw")
            rows_to_cols(I["d_onorm_g"][:, :], 1, lambda c: self.small[:, 8:9], 1, "rw")
            ex = self.sb(e2, "albx", [128, KC, 10], F32)
            sm = self.sb(e2, "albs", [128, KC, 2], F32)
            self.ACT(ex[:], self.alb[:], AF.Exp, self.alb.b, ex.b)
            for d in range(2):
                self.TT(sm[:, :, d], ex[:, :, 5 * d], ex[:, :, 5 * d + 1], ALU.add, ex.b, sm.b)
                for j in range(2, 5):
                    self.TT(sm[:, :, d], sm[:, :, d], ex[:, :, 5 * d + j], ALU.add, ex.b + sm.b, sm.b)
            self.RCP(sm[:], sm[:], sm.b, sm.b)
            for d in range(2):
                self.TT(self.lbc[:, :, d, 0], ex[:, :, 5 * d], sm[:, :, d], ALU.mult, ex.b + sm.b, self.lbc.b)
                self.TS(self.lbc[:, :, d, 1], self.lbc[:, :, d, 0], -1.0, 1.0, ALU.mult, ALU.add, self.lbc.b, self.lbc.b)
            xin = [self.sb(e2, "xin%d" % i, [128, D], F32) for i in range(2)]
            stg = [self.sb(e2, "xstg%d" % i, [128, KC, 512], F32) for i in range(2)]
            pts = [self.ps(e2, "xps%d" % i, [128, 512], F32) for i in range(2)]
            blocks = [(0, 2)] + [(2 + 4 * i, 4) for i in range(8)]
            for bi, (t0, nt) in enumerate(blocks):
                st = stg[bi % 2]
                for j in range(nt):
                    tt = t0 + j
                    xt = xin[tt % 2]
                    src = I["ctx"][tt * 128:(tt + 1) * 128, :] if tt < 2 else I["x"][(tt - 2) * 128:(tt - 1) * 128, :]
                    self.DMA("sp", "xin%d" % (tt % 2), xt[:], src, w=xt.b)
                    for k in range(KC):
                        p = pts[k % 2]
                        self.TR(p[:, 0:128], xt[:, k * 128:(k + 1) * 128], self.ident[:], xt.b + self.ident.b, p.b)
                        self.CP(st[:, k, j * 128:(j + 1) * 128], p[:, 0:128], p.b, st.b, eng=("act" if k % 2 else "dve"))
                self.DMA("pool", "xst%d" % (bi % 2), self.hT.rearrange("(k p) t -> p k t", p=128)[:, :, t0 * 128:(t0 + nt) * 128],
                         st[:, :, 0:nt * 128], r=st.b, w=[self.hb[t0 + j] for j in range(nt)])
            self.S.barrier()

    def mod_phase(self, i):
        I = self.I
        with ExitStack() as es:
            wb = [self.sb(es, "modw%d" % j, [128, KC, 512], F32) for j in range(2)]
            pm = self.ps(es, "modps", [128, 48, 2], F32)
            for cb in range(12):
                w = wb[cb % 2]
                self.DMA("sp", "modw%d" % (cb % 2), w[:], I["mod_w"][i].rearrange("(k p) n -> p k n", p=128)[:, :, cb * 512:(cb + 1) * 512], w=w.b)
                for f in range(4):
                    fc = cb * 4 + f
                    for k in range(KC):
                        self.MM(pm[:, fc, :], w[:, k, f * 128:(f + 1) * 128], self.condT[:, k, :], k == 0, k == KC - 1,
                                w.b + self.condT.b, pm.b)
            for s in range(2):
                self.TT(self.modc[:, :, :, s], pm[:, :, s].rearrange("p (j c) -> p j c", j=6),
                        self.modb[:, :, 6 * i:6 * i + 6].rearrange("p c j -> p j c"), ALU.add, pm.b + self.modb.b, self.modc.b)
            for sub in range(2):
                jb, ja, jg = 3 * sub, 3 * sub + 1, 3 * sub + 2
                gin, gout = 4 * i + 2 * sub, 4 * i + 2 * sub + 1
                for s in range(2):
                    self.STT(self.Acol[:, sub, :, s], self.modc[:, ja, :, s], 1.0, self.normg[:, :, gin], ALU.add, ALU.mult,
                             self.modc.b + self.normg.b, self.Acol.b)
                    self.CP(self.Bcol[:, sub, :, s], self.modc[:, jb, :, s], self.modc.b, self.Bcol.b)
                    self.TT(self.Gcol[:, sub, :, s], self.modc[:, jg, :, s], self.normg[:, :, gout], ALU.mult,
                            self.modc.b + self.normg.b, self.Gcol.b)
            self.S.barrier()

    @staticmethod
    def segs(t0, n):
        out = []
        if t0 < CTX:
            m = min(CTX, t0 + n) - t0
            out.append((0, m, 1))
            if n > m:
                out.append((m, n - m, 0))
        else:
            out.append((0, n, 0))
        return out

    def load_h(self, hblk, t0, n, key):
        self.DMA("sp", key, hblk[:, :, 0:n], self.hT.rearrange("(k p) t -> p k t", p=128)[:, :, t0:t0 + n],
                 r=[self.hb[t] for t in range(t0 // 128, (t0 + n) // 128)], w=hblk.b)

    def store_h(self, hblk, t0, n, key):
        self.DMA("pool", key, self.hT.rearrange("(k p) t -> p k t", p=128)[:, :, t0:t0 + n], hblk[:, :, 0:n],
                 r=hblk.b, w=[self.hb[t] for t in range(t0 // 128, (t0 + n) // 128)])

    def sumsq_rstd(self, src_fn, src_b, nchunks, n, ndim, W, out_rstd):
        for k in range(nchunks):
            sq = W["sq"][k % 2]
            self.ACT(sq[:, 0:n], src_fn(k), AF.Square, src_b, sq.b)
            self.MM(W["pss"][:, 0:n], self.onesb[:], sq[:, 0:n], k == 0, k == nchunks - 1, self.onesb.b + sq.b, W["pss"].b)
        self.rstd(out_rstd[:, 0:n], W["pss"][:, 0:n], ndim, W["pss"].b + self.epsb, out_rstd.b, W["rt"][:, 0:n], W["rt"].b)

    def norm_mod(self, hblk, t0, n, sub, uT, ucol0, W, u32=None):
        self.sumsq_rstd(lambda k: hblk[:, k, 0:n], hblk.b, KC, n, D, W, W["rs"])
        for k in range(KC):
            tmp = W["tmp"][k % 2]
            for (o, m, s) in self.segs(t0, n):
                self.STT(tmp[:, o:o + m], hblk[:, k, o:o + m], self.Acol[:, sub, k, s:s + 1], W["rs"][:, o:o + m], ALU.mult, ALU.mult,
                         hblk.b + self.Acol.b + W["rs"].b, tmp.b)
                dst = uT[:, k, ucol0 + o:ucol0 + o + m] if u32 is None else u32[k][:, o:o + m]
                dstb = [uT.b[k]] if u32 is None else u32[k].b
                self.ACT(dst, tmp[:, o:o + m], AF.Identity, tmp.b + self.Bcol.b, dstb, bias=self.Bcol[:, sub, k, s:s + 1], scale=1.0)

    def resid_update(self, hblk, t0, n, sub, y_fn, y_b, W):
        self.sumsq_rstd(y_fn, y_b, KC, n, D, W, W["rs"])
        for k in range(KC):
            tmp = W["tmp"][k % 2]
            for (o, m, s) in self.segs(t0, n):
                self.STT(tmp[:, o:o + m], y_fn(k)[:, o:o + m], self.Gcol[:, sub, k, s:s + 1], W["rs"][:, o:o + m], ALU.mult, ALU.mult,
                         y_b + self.Gcol.b + W["rs"].b, tmp.b)
                self.TT(hblk[:, k, o:o + m], hblk[:, k, o:o + m], tmp[:, o:o + m], ALU.add, hblk.b + tmp.b, hblk.b, eng="pool")

    def work_tiles(self, es, n=512):
        W = {}
        W["sq"] = [self.sb(es, "w_sq%d" % i, [128, n], BF16) for i in range(2)]
        W["pss"] = self.ps(es, "w_pss", [128, n], F32)
        W["rt"] = self.sb(es, "w_rt", [128, n], F32)
        W["rs"] = self.sb(es, "w_rs", [128, n], F32)
        W["tmp"] = [self.sb(es, "w_tmp%d" % i, [128, n], F32) for i in range(2)]
        return W

    def load_w(self, wt, src, key, kc=None):
        kc = kc or src.shape[0] // 128
        v = src.rearrange("(k p) n -> p k n", p=128)
        for k in range(kc):
            self.DMA("pool", key, wt[:, k, :], v[:, k, :], w=wt.b)

    def final_out(self):
        with ExitStack() as es:
            hblk = [self.sb(es, "fo_h%d" % i, [128, KC, 512], F32) for i in range(2)]
            ot = [self.sb(es, "fo_o%d" % i, [128, D], F32) for i in range(2)]
            pts = [self.ps(es, "fo_ps%d" % i, [128, 512], F32) for i in range(2)]
            for bi in range(8):
                t0 = CTX + bi * 512
                hb = hblk[bi % 2]
                self.load_h(hb, t0, 512, "foh%d" % (bi % 2))
                for j in range(4):
                    o = ot[j % 2]
                    for kk in range(2):
                        p = pts[kk]
                        for q in range(4):
                            k = kk * 4 + q
                            self.TR(p[:, q * 128:(q + 1) * 128], hb[:, k, j * 128:(j + 1) * 128], self.ident[:], hb.b + self.ident.b, p.b)
                        self.CP(o[:, kk * 512:(kk + 1) * 512], p[:], p.b, o.b, eng=("act" if kk else "dve"))
                    r0 = bi * 512 + j * 128
                    self.DMA("pool", "foo%d" % (j % 2), self.out[r0:r0 + 128, :], o[:], r=o.b)
            if self.debug:
                for bi in range(9):
                    t0, n = (0, 256) if bi == 0 else (CTX + (bi - 1) * 512, 512)
                    hb = hblk[bi % 2]
                    self.load_h(hb, t0, n, "foh%d" % (bi % 2))
                    self.DMA("pool", "dbg%d" % (bi % 2), self.dbg.rearrange("(k p) t -> p k t", p=128)[:, :, t0:t0 + n], hb[:, :, 0:n], r=hb.b)
            self.S.barrier()


def _blocks(t0, n, bs=512):
    out = []
    o = 0
    while o < n:
        m = min(bs, n - o)
        out.append((t0 + o, o, m))
        o += m
    return out


def ffn_phase(self, i, wout_ap):
    I = self.I
    moe = (i % 2 == 1) or FORCE_MOE
    li = i // 2
    E = NE if moe else 1
    NS = 4
    CS = FC // NS
    last = (i == 3)
    gt = [(2, 8), (10, 8), (18, 8), (26, 8)] if last else [(0, 9), (9, 9), (18, 8), (26, 8)]
    TGM = 9 * 128
    wgu_l = I["moe_w_gu"][li] if moe else I["ffn_w_gu"]
    wd_l = I["moe_w_down"][li] if moe else I["ffn_w_down"]
    if not moe:
        wgu_l = wgu_l[li:li + 1]
        wd_l = wd_l[li:li + 1]
    with ExitStack() as es:
        W = self.work_tiles(es)
        wout = self.sb(es, "f_wout", [128, KC, D], BF16)
        self.load_w(wout, wout_ap, "f_wout")
        uT = self.sb(es, "f_uT", [128, KC, TGM], BF16, n=KC)
        hid = [self.sb(es, "f_hid%d" % j, [128, CS, TGM], BF16, n=CS) for j in range(2)]
        yacc = self.sb(es, "f_yacc", [128, KC, TGM], F32, n=KC)
        hblk = self.sb(es, "f_hblk", [128, KC, 512], F32)
        otb = self.sb(es, "f_otb", [128, KC, 512], BF16)
        ysb = self.sb(es, "f_ysb", [128, KC, 512], F32, n=KC)
        wgu = [self.sb(es, "f_wgu%d" % j, [128, KC, 256], BF16) for j in range(3)]
        wdt = [self.sb(es, "f_wd%d" % j, [128, CS, 128], BF16) for j in range(3)]
        sgt = [self.sb(es, "f_sg%d" % j, [128, 512], F32) for j in range(2)]
        pG = [self.ps(es, "f_pG%d" % j, [128, 512]) for j in range(2)]
        pU = [self.ps(es, "f_pU%d" % j, [128, 512]) for j in range(2)]
        pY = [self.ps(es, "f_pY%d" % j, [128, 512]) for j in range(2)]
        pX = self.ps(es, "f_pX", [128, 512])
        if moe:
            rt_w = self.sb(es, "f_router", [128, KC, NE], F32)
            self.DMA("sp", "f_router", rt_w[:], I["moe_router"][li].rearrange("(k p) e -> p k e", p=128), w=rt_w.b)
            u32 = self.sb(es, "f_u32", [128, KC, 512], F32, n=KC)
            t1 = [self.sb(es, "f_t1%d" % j, [128, 512], F32) for j in range(2)]
            CB = self.sb(es, "f_CB", [128, TGM], BF16)
            comb = self.sb(es, "f_comb", [128, 9, NE], F32)
            dg = [self.sb(es, "f_dg%d" % j, [128, 128], F32) for j in range(2)]
            rsm = self.sb(es, "f_rsm", [128, 40], F32)
        cnt = {"wgu": 0, "wd": 0, "gu": 0, "y": 0}
        for (tile0, ntile) in gt:
            t0g, ng = tile0 * 128, ntile * 128
            blks = _blocks(t0g, ng)
            for (t0, o, n) in blks:
                self.load_h(hblk, t0, n, "f_hblk")
                self.DMA("sp", "f_otb", otb[:, :, 0:n], self.sOT.rearrange("(k p) t -> p k t", p=128)[:, :, t0:t0 + n], w=otb.b)
                for fo in range(KC):
                    p = pY[fo % 2]
                    for k in range(KC):
                        self.MM(p[:, 0:n], wout[:, k, fo * 128:(fo + 1) * 128], otb[:, k, 0:n], k == 0, k == KC - 1, wout.b + otb.b, p.b)
                    self.CP(ysb[:, fo, 0:n], p[:, 0:n], p.b, [ysb.b[fo]], eng=("act" if fo % 2 else "dve"))
                self.resid_update(hblk, t0, n, 0, lambda k: ysb[:, k, 0:n], ysb.b, W)
                self.store_h(hblk, t0, n, "f_hst")
                if not moe:
                    self.norm_mod(hblk, t0, n, 1, uT, o, W)
                else:
                    self.sumsq_rstd(lambda k: hblk[:, k, 0:n], hblk.b, KC, n, D, W, W["rs"])
                    nt_ = n // 128
                    for k in range(KC):
                        tmp = W["tmp"][k % 2]
                        for (oo, m, s) in self.segs(t0, n):
                            self.STT(tmp[:, oo:oo + m], hblk[:, k, oo:oo + m], self.Acol[:, 1, k, s:s + 1], W["rs"][:, oo:oo + m], ALU.mult, ALU.mult,
                                     hblk.b + self.Acol.b + W["rs"].b, tmp.b)
                            self.ACT(u32[:, k, oo:oo + m], tmp[:, oo:oo + m], AF.Identity, tmp.b + self.Bcol.b, [u32.b[k]], bias=self.Bcol[:, 1, k, s:s + 1], scale=1.0)
                        self.CP(uT[:, k, o:o + n], u32[:, k, 0:n], [u32.b[k]], [uT.b[k]], eng="pool")
                    for j in range(nt_):
                        for k in range(KC):
                            self.MM(pX[:, j * 8:(j + 1) * 8], u32[:, k, j * 128:(j + 1) * 128], rt_w[:, k, :], k == 0, k == KC - 1, [u32.b[k]] + rt_w.b, pX.b)
                    for j in range(nt_):
                        tt = o // 128 + j
                        lg, m8, mk, ex = rsm[:, 0:8], rsm[:, 8:16], rsm[:, 16:24], rsm[:, 24:32]
                        self.CP(lg, pX[:, j * 8:(j + 1) * 8], pX.b, rsm.b)
                        self.S.op("dve", lambda e, m8=m8, lg=lg: e.max(out=m8, in_=lg), rsm.b, rsm.b)
                        self.TS(mk, lg, rsm[:, 9:10], None, ALU.is_ge, None, rsm.b, rsm.b)
                        self.TS(rsm[:, 32:33], rsm[:, 8:9], -1.0, None, ALU.mult, None, rsm.b, rsm.b)
                        self.ACT(ex, lg, AF.Exp, rsm.b, rsm.b, bias=rsm[:, 32:33], scale=1.0)
                        self.TT(ex, ex, mk, ALU.mult, rsm.b, rsm.b)
                        self.S.op("dve", lambda e, ex=ex: e.reduce_sum(out=rsm[:, 33:34], in_=ex, axis=mybir.AxisListType.X), rsm.b, rsm.b)
                        self.RCP(rsm[:, 34:35], rsm[:, 33:34], rsm.b, rsm.b)
                        self.TS(comb[:, tt, :], ex, rsm[:, 34:35], None, ALU.mult, None, rsm.b, comb.b)
            jobs = [(e, s) for e in range(E) for s in range(NS)]

            def GU(j):
                e, s = jobs[j]
                hd = hid[j % 2]
                if moe and s == 0:
                    for (t0, o, n) in blks:
                        for q in range(n // 128):
                            tt = o // 128 + q
                            d_ = dg[tt % 2]
                            self.TS(d_[:], self.ident[:], comb[:, tt, e:e + 1], None, ALU.mult, None, self.ident.b + comb.b, d_.b)
                            self.MM(pX[:, q * 128:(q + 1) * 128], self.onesf[:], d_[:], True, True, self.onesf.b + d_.b, pX.b)
                        self.CP(CB[:, o:o + n], pX[:, 0:n], pX.b, CB.b, eng="act")
                for cc in range(CS):
                    c = s * CS + cc
                    w = wgu[cnt["wgu"] % 3]
                    cnt["wgu"] += 1
                    self.DMA("pool", "f_wgu%d" % ((cnt["wgu"] - 1) % 3), w[:].rearrange("p k n -> p (k n)"), wgu_l[e, c], w=w.b)
                    for (t0, o, n) in blks:
                        g_, u_ = pG[cnt["gu"] % 2], pU[cnt["gu"] % 2]
                        sg = sgt[cnt["gu"] % 2]
                        for k in range(KC):
                            self.MM(g_[:, 0:n], w[:, k, 0:128], uT[:, k, o:o + n], k == 0, k == KC - 1, w.b + [uT.b[k]], g_.b)
                        for k in range(KC):
                            self.MM(u_[:, 0:n], w[:, k, 128:256], uT[:, k, o:o + n], k == 0, k == KC - 1, w.b + [uT.b[k]], u_.b)
                        self.ACT(sg[:, 0:n], g_[:, 0:n], AF.Silu, g_.b, sg.b)
                        if moe:
                            tt1 = t1[cnt["gu"] % 2]
                            self.TT(tt1[:, 0:n], sg[:, 0:n], u_[:, 0:n], ALU.mult, sg.b + u_.b, tt1.b)
                            self.TT(hd[:, cc, o:o + n], tt1[:, 0:n], CB[:, o:o + n], ALU.mult, tt1.b + CB.b, [hd.b[cc]])
                        else:
                            self.TT(hd[:, cc, o:o + n], sg[:, 0:n], u_[:, 0:n], ALU.mult, sg.b + u_.b, [hd.b[cc]])
                        cnt["gu"] += 1

            def DN(j):
                e, s = jobs[j]
                hd = hid[j % 2]
                for fo in range(KC):
                    w = wdt[cnt["wd"] % 3]
                    cnt["wd"] += 1
                    self.DMA("pool", "f_wd%d" % ((cnt["wd"] - 1) % 3), w[:].rearrange("p c n -> p (c n)"), wd_l[e, fo][:, s * CS * 128:(s + 1) * CS * 128], w=w.b)
                    for (t0, o, n) in blks:
                        p = pY[cnt["y"] % 2]
                        cnt["y"] += 1
                        for cc in range(CS):
                            self.MM(p[:, 0:n], w[:, cc, :], hd[:, cc, o:o + n], cc == 0, cc == CS - 1, w.b + [hd.b[cc]], p.b)
                        if j == 0:
                            self.CP(yacc[:, fo, o:o + n], p[:, 0:n], p.b, [yacc.b[fo]], eng="act")
                        else:
                            self.TT(yacc[:, fo, o:o + n], yacc[:, fo, o:o + n], p[:, 0:n], ALU.add, p.b + [yacc.b[fo]], [yacc.b[fo]])
            GU(0)
            for j in range(len(jobs)):
                if j + 1 < len(jobs):
                    GU(j + 1)
                DN(j)
            for (t0, o, n) in blks:
                self.load_h(hblk, t0, n, "f_hblk")
                self.resid_update(hblk, t0, n, 1, lambda k: yacc[:, k, o:o + n], yacc.b, W)
                self.store_h(hblk, t0, n, "f_hst")
        self.S.barrier()


Builder.ffn_phase = ffn_phase


P1_BLOCKS = [(0, 256)] + [(CTX + 512 * b, 512) for b in range(8)]


def attn_tiles(self, es, nS=3):
    A = {}
    A["nS"] = nS
    A["pS"] = [self.ps(es, "a_pS%d" % j, [128, 512]) for j in range(nS)]
    A["pT"] = [self.sb(es, "a_pT%d" % j, [128, 512], BF16) for j in range(4)]
    A["pO"] = [self.ps(es, "a_pO%d" % j, [128, 512]) for j in range(2)]
    A["pD"] = [self.ps(es, "a_pD%d" % j, [128, 512]) for j in range(2)]
    A["pB"] = self.ps(es, "a_pB", [128, 512])
    A["rd"] = [self.sb(es, "a_rd%d" % j, [1, 512], F32) for j in range(2)]
    A["osb"] = [self.sb(es, "a_osb%d" % j, [128, 512], F32) for j in range(2)]
    A["n"] = 0
    A["ns"] = 0
    return A


def attn_block(self, A, kfn, q_ap, vfn, kts, nq, dv, scale, rb, out_ap, out_b):
    i0 = A["n"]
    A["n"] += 1
    pO, pD, rd, osb = A["pO"][i0 % 2], A["pD"][i0 % 2], A["rd"][i0 % 2], A["osb"][i0 % 2]
    for i, kt in enumerate(kts):
        ps_ = A["pS"][A["ns"] % A["nS"]]
        pt = A["pT"][A["ns"] % 4]
        A["ns"] += 1
        self.MM(ps_[:, 0:nq], kfn(kt), q_ap, True, True, rb, ps_.b)
        self.ACT(pt[:, 0:nq], ps_[:, 0:nq], AF.Exp, ps_.b, pt.b, scale=scale)
        self.MM(pO[0:dv, 0:nq], vfn(kt), pt[:, 0:nq], i == 0, i == len(kts) - 1, rb + pt.b, pO.b)
        self.MM(pD[0:1, 0:nq], self.onesb[:, 0:1], pt[:, 0:nq], i == 0, i == len(kts) - 1, self.onesb.b + pt.b, pD.b)
    self.RCP(rd[0:1, 0:nq], pD[0:1, 0:nq], pD.b, rd.b)
    self.MM(A["pB"][0:dv, 0:nq], self.onesf[0:1, 0:dv], rd[0:1, 0:nq], True, True, self.onesf.b + rd.b, A["pB"].b)
    self.CP(osb[0:dv, 0:nq], pO[0:dv, 0:nq], pO.b, osb.b)
    self.TT(out_ap, osb[0:dv, 0:nq], A["pB"][0:dv, 0:nq], ALU.mult, osb.b + A["pB"].b, out_b)


Builder.attn_tiles = attn_tiles
Builder.attn_block = attn_block


def rope_chunk(self, R, pq, nrow, n, qn_fn, cosb, sinb, permb, out_ap, out_b, rs=None):
    qn, pr, t1, t2 = R["qn"][R["i"] % 2], R["pr"], R["t1"][R["i"] % 2], R["t2"][R["i"] % 2]
    R["i"] += 1
    qn_fn(qn)
    self.MM(pr[0:nrow, 0:n], permb[0:nrow, 0:nrow], qn[0:nrow, 0:n], True, True, permb.b + qn.b, pr.b)
    self.TT(t1[0:nrow, 0:n], qn[0:nrow, 0:n], cosb[0:nrow, 0:n], ALU.mult, qn.b + cosb.b, t1.b, eng="pool")
    self.TT(t2[0:nrow, 0:n], pr[0:nrow, 0:n], sinb[0:nrow, 0:n], ALU.mult, pr.b + sinb.b, t2.b)
    if rs is None:
        self.TT(out_ap, t1[0:nrow, 0:n], t2[0:nrow, 0:n], ALU.add, t1.b + t2.b, out_b, eng="pool")
    else:
        self.TT(t1[0:nrow, 0:n], t1[0:nrow, 0:n], t2[0:nrow, 0:n], ALU.add, t1.b + t2.b, t1.b, eng="pool")
        self.TT(out_ap, t1[0:nrow, 0:n], rs[0:nrow, 0:n], ALU.mult, t1.b + rs.b, out_b)


def rope_tiles(self, es, cos_name, sin_name, perm_name, nrow):
    R = {"i": 0}
    R["qn"] = [self.sb(es, "r_qn%d" % j, [128, 512], BF16) for j in range(2)]
    R["t1"] = [self.sb(es, "r_t1%d" % j, [128, 512], F32) for j in range(2)]
    R["t2"] = [self.sb(es, "r_t2%d" % j, [128, 512], F32) for j in range(2)]
    R["pr"] = self.ps(es, "r_pr", [128, 512])
    R["cos"] = [self.sb(es, "r_cos%d" % j, [128, 512], F32) for j in range(2)]
    R["sin"] = [self.sb(es, "r_sin%d" % j, [128, 512], F32) for j in range(2)]
    pf = self.sb(es, "r_permf", [128, 128], F32)
    R["perm"] = self.sb(es, "r_perm", [128, 128], BF16)
    self.DMA("sp", "r_perm", pf[0:nrow, 0:nrow], self.I[perm_name][:, :], w=pf.b)
    self.CP(R["perm"][0:nrow, 0:nrow], pf[0:nrow, 0:nrow], pf.b, R["perm"].b)
    R["names"] = (cos_name, sin_name, nrow)
    return R


def rope_load(self, R, bi, t0, n):
    cn, sn, nrow = R["names"]
    cb, sbb = R["cos"][bi % 2], R["sin"][bi % 2]
    self.DMA("sp", "r_cos%d" % (bi % 2), cb[0:nrow, 0:n], self.I[cn][:, t0:t0 + n], w=cb.b)
    self.DMA("sp", "r_sin%d" % (bi % 2), sbb[0:nrow, 0:n], self.I[sn][:, t0:t0 + n], w=sbb.b)
    return cb, sbb


Builder.rope_chunk = rope_chunk
Builder.rope_tiles = rope_tiles
Builder.rope_load = rope_load


def gqa_p1(self):
    I = self.I
    with ExitStack() as es:
        W = self.work_tiles(es)
        R = self.rope_tiles(es, "cos2", "sin2", "perm2", 128)
        win = self.sb(es, "g_win", [128, KC, 1536], BF16)
        self.load_w(win, I["c_w_in"], "g_win")
        uT = self.sb(es, "g_uT", [128, KC, 512], BF16, n=KC)
        hblk = [self.sb(es, "g_h%d" % j, [128, KC, 512], F32) for j in range(2)]
        pq = [self.ps(es, "g_pq%d" % j, [128, 512]) for j in range(2)]
        pv = self.ps(es, "g_pv", [128, 512])
        rs2 = self.sb(es, "g_rs2", [128, 512], F32)
        qo = [self.sb(es, "g_qo%d" % j, [128, 512], BF16) for j in range(3)]
        vt = [self.sb(es, "g_vt%d" % j, [128, 256], BF16) for j in range(2)]
        for bi, (t0, n) in enumerate(P1_BLOCKS):
            hb = hblk[bi % 2]
            self.load_h(hb, t0, n, "g_h%d" % (bi % 2))
            cb, sbb = self.rope_load(R, bi, t0, n)
            self.norm_mod(hb, t0, n, 0, uT, 0, W)
            for ch in range(10):
                p = pq[ch % 2]
                for k in range(KC):
                    self.MM(p[:, 0:n], win[:, k, ch * 128:(ch + 1) * 128], uT[:, k, 0:n], k == 0, k == KC - 1, win.b + [uT.b[k]], p.b)
                self.sumsq_rstd(lambda k: p[:, 0:n], p.b, 1, n, 128, W, rs2)
                gcol = self.small[:, 6:7] if ch < 8 else self.small[:, 7:8]
                o_ = qo[ch % 3]
                self.rope_chunk(R, p, 128, n, lambda dst: self.TS(dst[:, 0:n], p[:, 0:n], gcol, None, ALU.mult, None, p.b + self.small.b, dst.b),
                                cb, sbb, R["perm"], o_[:, 0:n], o_.b, rs=rs2)
                dst = self.sA[ch * 128:(ch + 1) * 128, t0:t0 + n] if ch < 8 else self.sB[(ch - 8) * 128:(ch - 7) * 128, t0:t0 + n]
                self.DMA("pool", "g_qo%d" % (ch % 3), dst, o_[:, 0:n], r=o_.b)
            for j in range(n // 128):
                for k in range(KC):
                    self.MM(pv[:, 0:256], uT[:, k, j * 128:(j + 1) * 128], win[:, k, 1280:1536], k == 0, k == KC - 1, win.b + [uT.b[k]], pv.b)
                v_ = vt[j % 2]
                self.CP(v_[:], pv[:, 0:256], pv.b, v_.b, eng="act")
                self.DMA("pool", "g_vt%d" % (j % 2), self.sV[t0 + j * 128:t0 + (j + 1) * 128, 0:256], v_[:], r=v_.b)
        self.S.barrier()


def attn_qblocks(do_ctx=True):
    out = []
    if do_ctx:
        out.append((0, 256, [0, 1]))
    for b in range(8):
        out.append((CTX + 512 * b, 512, list(range(NT))))
    return out


def gqa_p2(self):
    with ExitStack() as es:
        A = self.attn_tiles(es)
        KT = self.sb(es, "g2_K", [128, T], BF16)
        V = self.sb(es, "g2_V", [128, NT, 128], BF16)
        QT = [self.sb(es, "g2_Q%d" % j, [128, T], BF16) for j in range(2)]
        ob = [self.sb(es, "g2_o%d" % j, [128, 512], BF16) for j in range(2)]
        scale = 128 ** -0.5
        cnt = 0
        for kvh in range(2):
            self.DMA("sp", "g2_K", KT[:], self.sB[kvh * 128:(kvh + 1) * 128, :], w=KT.b)
            self.DMA("sp", "g2_V", V[:], self.sV.rearrange("(t p) d -> p t d", p=128)[:, :, kvh * 128:(kvh + 1) * 128], w=V.b)
            for g in range(4):
                h = kvh * 4 + g
                Q = QT[h % 2]
                self.DMA("sp", "g2_Q%d" % (h % 2), Q[:], self.sA[h * 128:(h + 1) * 128, :], w=Q.b)
                for (q0, nq, kts) in attn_qblocks(True):
                    o_ = ob[cnt % 2]
                    self.attn_block(A, lambda kt: KT[:, kt * 128:(kt + 1) * 128], Q[:, q0:q0 + nq], lambda kt: V[:, kt, :], kts, nq, 128, scale,
                                    KT.b + V.b + Q.b, o_[:, 0:nq], o_.b)
                    self.DMA("pool", "g2_o%d" % (cnt % 2), self.sOT[h * 128:(h + 1) * 128, q0:q0 + nq], o_[:, 0:nq], r=o_.b)
                    cnt += 1
        self.S.barrier()


Builder.gqa_p1 = gqa_p1
Builder.gqa_p2 = gqa_p2


def build(layers=(0, 1, 2, 3), debug=False):
    B = Builder(len(layers), debug)
    I = B.I
    es = ExitStack()
    B.setup(es)
    for i in layers:
        B.mod_phase(i)
        if i == 0:
            B.hgrn_p1()
            B.hgrn_p2()
            B.hgrn_p2b()
            wout = I["a_w_out"]
        elif i == 1:
            B.mla_p1()
            B.mla_p2()
            wout = I["b_w_out"]
        elif i == 2:
            B.gqa_p1()
            B.gqa_p2()
            wout = I["c_w_out"]
        else:
            B.diff_p1()
            B.diff_p2()
            wout = I["d_w_out"]
        B.ffn_phase(i, wout)
    B.final_out()
    B.S.barrier()
    B.S.emit()
    return B.nc


def prep_shared(inp):
    f = lambda a: np.ascontiguousarray(a, dtype=np.float32)
    sh = {}
    sh["c_ctx"] = f(inp["c_ctx"]).reshape(1, D)
    sh["mod_w"] = f(inp["mod_w"])
    sh["mod_b"] = f(inp["mod_b"]).reshape(24, D)
    sh["norm_g"] = f(inp["norm_g"]).reshape(16, D)
    sh["a_w_in"] = f(inp["a_w_in"][0])
    sh["a_lb"] = f(inp["a_lb"]).reshape(10, D)
    sh["a_onorm_g"] = f(inp["a_onorm_g"]).reshape(1, 128)
    sh["a_w_out"] = f(inp["a_w_out"][0])
    sh["b_w_in"] = f(inp["b_w_in"][0])
    sh["b_qnorm_g"] = f(inp["b_qnorm_g"]).reshape(3, 128)
    sh["b_kvnorm_g"] = f(inp["b_kvnorm_g"]).reshape(2, 128)
    sh["b_w_qb"] = f(inp["b_w_qb"][0])
    kvb = f(inp["b_w_kvb"][0]).reshape(256, 16, 2, 64)
    sh["b_w_kvb"] = np.ascontiguousarray(kvb.transpose(0, 2, 1, 3).reshape(256, 2048))
    sh["b_w_out"] = f(inp["b_w_out"][0])
    sh["c_w_in"] = f(inp["c_w_in"][0])
    sh["c_qnorm_g"] = f(inp["c_qnorm_g"]).reshape(1, 128)
    sh["c_knorm_g"] = f(inp["c_knorm_g"]).reshape(1, 128)
    sh["c_w_out"] = f(inp["c_w_out"][0])
    sh["d_w_in"] = f(inp["d_w_in"][0])
    sh["d_lambda"] = f(inp["d_lambda"]).reshape(1, 256)
    sh["d_onorm_g"] = f(inp["d_onorm_g"]).reshape(1, 128)
    sh["d_w_out"] = f(inp["d_w_out"][0])
    g = f(inp["ffn_w_gu"]).reshape(2, KC, 128, 2, FC, 128)
    sh["ffn_w_gu"] = np.ascontiguousarray(g.transpose(0, 4, 2, 1, 3, 5).reshape(2, FC, 128, KC * 256))
    d_ = f(inp["ffn_w_down"]).reshape(2, FC, 128, KC, 128)
    sh["ffn_w_down"] = np.ascontiguousarray(d_.transpose(0, 3, 2, 1, 4).reshape(2, KC, 128, FF))
    sh["moe_router"] = f(inp["moe_router"])
    g = f(inp["moe_w_gu"]).reshape(2, NE, KC, 128, 2, FC, 128)
    sh["moe_w_gu"] = np.ascontiguousarray(g.transpose(0, 1, 5, 3, 2, 4, 6).reshape(2, NE, FC, 128, KC * 256))
    d_ = f(inp["moe_w_down"]).reshape(2, NE, FC, 128, KC, 128)
    sh["moe_w_down"] = np.ascontiguousarray(d_.transpose(0, 1, 4, 3, 2, 5).reshape(2, NE, KC, 128, FF))
    sh.update(_consts())
    return sh


def kernel(**inp):
    nc = build()
    sh = prep_shared(inp)
    in_maps = []
    for b in range(8):
        m = dict(sh)
        m["x"] = np.ascontiguousarray(inp["x"][b], dtype=np.float32)
        m["c"] = np.ascontiguousarray(inp["c"][b:b + 1], dtype=np.float32)
        m["ctx"] = np.ascontiguousarray(inp["ctx"][b], dtype=np.float32)
        in_maps.append(m)
    res = run_bass_kernel_spmd(nc, in_maps, core_ids=list(range(8)))
    return np.stack([np.asarray(r["out"], dtype=np.float32) for r in res.results], axis=0)


def diff_p1(self):
    I = self.I
    with ExitStack() as es:
        W = self.work_tiles(es)
        R = self.rope_tiles(es, "cos3", "sin3", "perm3", 128)
        win = self.sb(es, "d_win", [128, KC, 3072], BF16)
        self.load_w(win, I["d_w_in"], "d_win")
        uT = self.sb(es, "d_uT", [128, KC, 512], BF16, n=KC)
        hblk = [self.sb(es, "d_h%d" % j, [128, KC, 512], F32) for j in range(2)]
        pq = [self.ps(es, "d_pq%d" % j, [128, 512]) for j in range(2)]
        pv = [self.ps(es, "d_pv%d" % j, [128, 512]) for j in range(2)]
        qo = [self.sb(es, "d_qo%d" % j, [128, 512], BF16) for j in range(3)]
        vt = [self.sb(es, "d_vt%d" % j, [128, 1024], BF16) for j in range(2)]
        for bi, (t0, n) in enumerate(P1_BLOCKS):
            hb = hblk[bi % 2]
            self.load_h(hb, t0, n, "d_h%d" % (bi % 2))
            cb, sbb = self.rope_load(R, bi, t0, n)
            self.norm_mod(hb, t0, n, 0, uT, 0, W)
            for ch in range(16):
                p = pq[ch % 2]
                for k in range(KC):
                    self.MM(p[:, 0:n], win[:, k, ch * 128:(ch + 1) * 128], uT[:, k, 0:n], k == 0, k == KC - 1, win.b + [uT.b[k]], p.b)
                o_ = qo[ch % 3]
                self.rope_chunk(R, p, 128, n, lambda dst: self.CP(dst[:, 0:n], p[:, 0:n], p.b, dst.b, eng="act"),
                                cb, sbb, R["perm"], o_[:, 0:n], o_.b)
                dst = self.sA[ch * 128:(ch + 1) * 128, t0:t0 + n] if ch < 8 else self.sB[(ch - 8) * 128:(ch - 7) * 128, t0:t0 + n]
                self.DMA("pool", "d_qo%d" % (ch % 3), dst, o_[:, 0:n], r=o_.b)
            for j in range(n // 128):
                v_ = vt[j % 2]
                for hh in range(2):
                    p = pv[hh]
                    for k in range(KC):
                        self.MM(p[:, :], uT[:, k, j * 128:(j + 1) * 128], win[:, k, 2048 + hh * 512:2048 + (hh + 1) * 512], k == 0, k == KC - 1,
                                win.b + [uT.b[k]], p.b)
                    self.CP(v_[:, hh * 512:(hh + 1) * 512], p[:, :], p.b, v_.b, eng=("act" if hh else "dve"))
                self.DMA("pool", "d_vt%d" % (j % 2), self.sV[t0 + j * 128:t0 + (j + 1) * 128, :], v_[:], r=v_.b)
        self.S.barrier()


def diff_p2(self, i=3):
    I = self.I
    lam_init = 0.8 - 0.6 * math.exp(-0.3 * i)
    with ExitStack() as es:
        A = self.attn_tiles(es, 2)
        W = self.work_tiles(es)
        KT = [self.sb(es, "d2_K%d" % j, [128, T], BF16) for j in range(2)]
        V = [self.sb(es, "d2_V%d" % j, [128, NT, 128], BF16) for j in range(2)]
        QT = [self.sb(es, "d2_Q%d" % j, [128, T], BF16) for j in range(2)]
        am = [self.sb(es, "d2_a%d" % j, [128, 512], F32) for j in range(2)]
        od = self.sb(es, "d2_od", [128, 512], F32)
        ob = [self.sb(es, "d2_o%d" % j, [128, 512], BF16) for j in range(2)]
        lt = self.sb(es, "d2_lam", [128, 256], F32)
        lc = self.sb(es, "d2_lc", [128, 8], F32)
        self.DMA("sp", "d2_lam", lt[:], I["d_lambda"][0:1, :].to_broadcast([128, 256]), w=lt.b)
        for q in range(2):
            self.TT(lt[:, q * 128:q * 128 + 64], lt[:, q * 128:q * 128 + 64], lt[:, q * 128 + 64:q * 128 + 128], ALU.mult, lt.b, lt.b)
            self.S.op("dve", lambda e, q=q: e.reduce_sum(out=lc[:, q:q + 1], in_=lt[:, q * 128:q * 128 + 64], axis=mybir.AxisListType.X), lt.b, lc.b)
        self.ACT(lc[:, 2:4], lc[:, 0:2], AF.Exp, lc.b, lc.b)
        self.TT(lc[:, 4:5], lc[:, 3:4], lc[:, 2:3], ALU.subtract, lc.b, lc.b)
        self.TS(lc[:, 5:6], lc[:, 4:5], -lam_init, None, ALU.add, None, lc.b, lc.b)
        self.TS(lc[:, 6:7], self.small[:, 8:9], 1.0 - lam_init, None, ALU.mult, None, lc.b + self.small.b, lc.b)
        scale = 64 ** -0.5
        cnt = 0
        for h in range(8):
            K_, V_, Q_ = KT[h % 2], V[h % 2], QT[h % 2]
            self.DMA("sp", "d2_K%d" % (h % 2), K_[:], self.sB[h * 128:(h + 1) * 128, :], w=K_.b)
            self.DMA("sp", "d2_V%d" % (h % 2), V_[:], self.sV.rearrange("(t p) d -> p t d", p=128)[:, :, h * 128:(h + 1) * 128], w=V_.b)
            self.DMA("sp", "d2_Q%d" % (h % 2), Q_[:], self.sA[h * 128:(h + 1) * 128, :], w=Q_.b)
            for (q0, nq, kts) in attn_qblocks(False):
                for m in range(2):
                    lo, hi = 64 * m, 64 * m + 64
                    self.attn_block(A, lambda kt: K_[lo:hi, kt * 128:(kt + 1) * 128], Q_[lo:hi, q0:q0 + nq], lambda kt: V_[:, kt, :], kts, nq, 128, scale,
                                    K_.b + V_.b + Q_.b, am[m][:, 0:nq], am[m].b)
                self.STT(od[:, 0:nq], am[1][:, 0:nq], lc[:, 5:6], am[0][:, 0:nq], ALU.mult, ALU.add, am[0].b + am[1].b + lc.b, od.b)
                self.sumsq_rstd(lambda k: od[:, 0:nq], od.b, 1, nq, 128, W, W["rs"])
                o_ = ob[cnt % 2]
                self.STT(o_[:, 0:nq], od[:, 0:nq], lc[:, 6:7], W["rs"][:, 0:nq], ALU.mult, ALU.mult, od.b + lc.b + W["rs"].b, o_.b)
                self.DMA("pool", "d2_o%d" % (cnt % 2), self.sOT[h * 128:(h + 1) * 128, q0:q0 + nq], o_[:, 0:nq], r=o_.b)
                cnt += 1
        self.S.barrier()


Builder.diff_p1 = diff_p1
Builder.diff_p2 = diff_p2


def mla_p1(self):
    I = self.I
    with ExitStack() as es:
        W = self.work_tiles(es)
        R = self.rope_tiles(es, "cos1", "sin1", "perm1", 96)
        ck = self.sb(es, "m_cosk", [32, T], F32)
        sk = self.sb(es, "m_sink", [32, T], F32)
        pkf = self.sb(es, "m_permkf", [32, 32], F32)
        pkb = self.sb(es, "m_permk", [32, 32], BF16)
        self.DMA("sp", "m_ck", ck[:], I["cos1k"][:, :], w=ck.b)
        self.DMA("sp", "m_sk", sk[:], I["sin1k"][:, :], w=sk.b)
        self.DMA("sp", "m_pk", pkf[:], I["perm1k"][:, :], w=pkf.b)
        self.CP(pkb[:], pkf[:], pkf.b, pkb.b)
        win = self.sb(es, "m_win", [128, KC, 768], BF16)
        wqb = self.sb(es, "m_wqb", [128, 3, 1536], BF16)
        wkvb = self.sb(es, "m_wkvb", [128, 2, 2048], BF16)
        for k in range(KC):
            self.DMA("pool", "m_win", win[:, k, 0:672], I["b_w_in"].rearrange("(k p) n -> p k n", p=128)[:, k, :], w=win.b)
        self.load_w(wqb, I["b_w_qb"], "m_wqb")
        self.load_w(wkvb, I["b_w_kvb"], "m_wkvb")
        for k in range(3 if 'scale' in MLA_PARTS else 0):
            self.TS(wqb[:, k, :], wqb[:, k, :], self.small[:, 1 + k:2 + k], None, ALU.mult, None, wqb.b + self.small.b, wqb.b)
        for k in range(2 if 'scale' in MLA_PARTS else 0):
            self.TS(wkvb[:, k, :], wkvb[:, k, :], self.small[:, 4 + k:5 + k], None, ALU.mult, None, wkvb.b + self.small.b, wkvb.b)
        uT = self.sb(es, "m_uT", [128, KC, 512], BF16, n=KC)
        hblk = [self.sb(es, "m_h%d" % j, [128, KC, 512], F32) for j in range(2)]
        pq = [self.ps(es, "m_pq%d" % j, [128, 512]) for j in range(2)]
        pv = [self.ps(es, "m_pv%d" % j, [128, 512]) for j in range(2)]
        pss2 = self.ps(es, "m_pss2", [128, 512])
        cqT = self.sb(es, "m_cqT", [128, 3, 512], BF16)
        ckvT = self.sb(es, "m_ckvT", [128, 2, 512], BF16)
        sqq = [self.sb(es, "m_sqq%d" % j, [128, 512], BF16) for j in range(3)]
        sqkv = [self.sb(es, "m_sqkv%d" % j, [128, 512], BF16) for j in range(2)]
        rsq = self.sb(es, "m_rsq", [128, 512], F32)
        rskv = self.sb(es, "m_rskv", [128, 512], F32)
        rcol = self.sb(es, "m_rcol", [128, 4], F32)
        qo = [self.sb(es, "m_qo%d" % j, [128, 512], BF16) for j in range(3)]
        vt = [self.sb(es, "m_vt%d" % j, [128, 1024], BF16) for j in range(2)]
        for bi, (t0, n) in enumerate(P1_BLOCKS):
            hb = hblk[bi % 2]
            self.load_h(hb, t0, n, "m_h%d" % (bi % 2))
            cb, sbb = self.rope_load(R, bi, t0, n)
            self.norm_mod(hb, t0, n, 0, uT, 0, W)
            if 'lat' not in MLA_PARTS:
                continue
            for ch in range(5):
                p = pq[ch % 2]
                for k in range(KC):
                    self.MM(p[:, 0:n], win[:, k, ch * 128:(ch + 1) * 128], uT[:, k, 0:n], k == 0, k == KC - 1, win.b + [uT.b[k]], p.b)
                if 'nosq' in MLA_PARTS:
                    continue
                if ch < 3:
                    sq = sqq[ch]
                    self.ACT(sq[:, 0:n], p[:, 0:n], AF.Square, p.b, sq.b)
                    self.CP(cqT[:, ch, 0:n], p[:, 0:n], p.b, cqT.b)
                else:
                    sq = sqkv[ch - 3]
                    self.ACT(sq[:, 0:n], p[:, 0:n], AF.Square, p.b, sq.b)
                    self.CP(ckvT[:, ch - 3, 0:n], p[:, 0:n], p.b, ckvT.b)
            if 'nosq' in MLA_PARTS or 'noones' in MLA_PARTS:
                continue
            for ch in range(3):
                self.MM(W["pss"][:, 0:n], self.onesb[:], sqq[ch][:, 0:n], ch == 0, ch == 2, self.onesb.b + sqq[ch].b, W["pss"].b)
            for ch in range(2):
                self.MM(pss2[:, 0:n], self.onesb[:], sqkv[ch][:, 0:n], ch == 0, ch == 1, self.onesb.b + sqkv[ch].b, pss2.b)
            self.rstd(rsq[:, 0:n], W["pss"][:, 0:n], 384, W["pss"].b + self.epsb, rsq.b, W["rt"][:, 0:n], W["rt"].b)
            self.rstd(rskv[:, 0:n], pss2[:, 0:n], 256, pss2.b + self.epsb, rskv.b, W["rt"][:, 0:n], W["rt"].b)
            p = pq[1]
            if 'kr' not in MLA_PARTS:
                continue
            for k in range(KC):
                self.MM(p[0:32, 0:n], win[:, k, 640:672], uT[:, k, 0:n], k == 0, k == KC - 1, win.b + [uT.b[k]], p.b)
            o_ = qo[0]
            self.rope_chunk(R, p, 32, n, lambda dst: self.CP(dst[0:32, 0:n], p[0:32, 0:n], p.b, dst.b, eng="act"),
                            self.view(ck, ck.t[:, t0:t0 + n]), self.view(sk, sk.t[:, t0:t0 + n]), pkb, o_[0:32, 0:n], o_.b)
            self.DMA("pool", "m_qo0", self.sKR[:, t0:t0 + n], o_[0:32, 0:n], r=o_.b)
            for h in range(16 if 'q' in MLA_PARTS else 0):
                p = pq[h % 2]
                for k in range(3):
                    self.MM(p[0:96, 0:n], wqb[:, k, h * 96:(h + 1) * 96], cqT[:, k, 0:n], k == 0, k == 2, wqb.b + cqT.b, p.b)
                o_ = qo[(h + 1) % 3]
                self.rope_chunk(R, p, 96, n, lambda dst: self.TT(dst[0:96, 0:n], p[0:96, 0:n], rsq[0:96, 0:n], ALU.mult, p.b + rsq.b, dst.b),
                                cb, sbb, R["perm"], o_[0:96, 0:n], o_.b)
                self.DMA("pool", "m_qo%d" % ((h + 1) % 3), self.sA[h * 96:(h + 1) * 96, t0:t0 + n], o_[0:96, 0:n], r=o_.b)
            for j in range(8 if 'kn' in MLA_PARTS else 0):
                p = pq[j % 2]
                for k in range(2):
                    self.MM(p[:, 0:n], wkvb[:, k, j * 128:(j + 1) * 128], ckvT[:, k, 0:n], k == 0, k == 1, wkvb.b + ckvT.b, p.b)
                o_ = qo[j % 3]
                self.TT(o_[:, 0:n], p[:, 0:n], rskv[:, 0:n], ALU.mult, p.b + rskv.b, o_.b)
                self.DMA("pool", "m_qo%d" % (j % 3), self.sB[j * 128:(j + 1) * 128, t0:t0 + n], o_[:, 0:n], r=o_.b)
            for j in range(n // 128 if 'v' in MLA_PARTS else 0):
                v_ = vt[j % 2]
                for k in range(2):
                    self.MM(pss2[:, 0:8], sqkv[k][:, j * 128:(j + 1) * 128], self.onesb[:, 0:8], k == 0, k == 1, sqkv[k].b + self.onesb.b, pss2.b)
                self.ACT(rcol[:, 0:1], pss2[:, 0:1], AF.Sqrt, pss2.b + self.epsb, rcol.b, bias=self.epsc[:, :], scale=1.0 / 256)
                self.RCP(rcol[:, 1:2], rcol[:, 0:1], rcol.b, rcol.b)
                for hh in range(2):
                    p = pv[hh]
                    for k in range(2):
                        self.MM(p[:, :], ckvT[:, k, j * 128:(j + 1) * 128], wkvb[:, k, 1024 + hh * 512:1024 + (hh + 1) * 512], k == 0, k == 1,
                                wkvb.b + ckvT.b, p.b)
                    self.TS(v_[:, hh * 512:(hh + 1) * 512], p[:, :], rcol[:, 1:2], None, ALU.mult, None, p.b + rcol.b, v_.b)
                self.DMA("pool", "m_vt%d" % (j % 2), self.sV[t0 + j * 128:t0 + (j + 1) * 128, :], v_[:], r=v_.b)
        self.S.barrier()


def mla_p2(self):
    with ExitStack() as es:
        A = self.attn_tiles(es)
        KT = [self.sb(es, "m2_K%d" % j, [96, T], BF16) for j in range(2)]
        V = [self.sb(es, "m2_V%d" % j, [128, NT, 64], BF16) for j in range(2)]
        QT = [self.sb(es, "m2_Q%d" % j, [96, T], BF16) for j in range(2)]
        ob = [self.sb(es, "m2_o%d" % j, [64, 512], BF16) for j in range(2)]
        scale = 96 ** -0.5
        cnt = 0
        for h in range(16):
            K_, V_, Q_ = KT[h % 2], V[h % 2], QT[h % 2]
            self.DMA("sp", "m2_K%d" % (h % 2), K_[0:64, :], self.sB[h * 64:(h + 1) * 64, :], w=K_.b)
            self.DMA("sp", "m2_K%d" % (h % 2), K_[64:96, :], self.sKR[:, :], w=K_.b)
            self.DMA("sp", "m2_V%d" % (h % 2), V_[:], self.sV.rearrange("(t p) d -> p t d", p=128)[:, :, h * 64:(h + 1) * 64], w=V_.b)
            self.DMA("sp", "m2_Q%d" % (h % 2), Q_[:], self.sA[h * 96:(h + 1) * 96, :], w=Q_.b)
            for (q0, nq, kts) in attn_qblocks(True):
                o_ = ob[cnt % 2]
                self.attn_block(A, lambda kt: K_[:, kt * 128:(kt + 1) * 128], Q_[:, q0:q0 + nq], lambda kt: V_[:, kt, :], kts, nq, 64, scale,
                                K_.b + V_.b + Q_.b, o_[:, 0:nq], o_.b)
                self.DMA("pool", "m2_o%d" % (cnt % 2), self.sOT[h * 64:(h + 1) * 64, q0:q0 + nq], o_[:, 0:nq], r=o_.b)
                cnt += 1
        self.S.barrier()


Builder.mla_p1 = mla_p1
Builder.mla_p2 = mla_p2


def hgrn_p1(self):
    I = self.I
    with ExitStack() as es:
        W = self.work_tiles(es)
        win = self.sb(es, "h_win", [128, KC, 5120], BF16)
        self.load_w(win, I["a_w_in"], "h_win")
        uT = self.sb(es, "h_uT", [128, KC, 512], BF16, n=KC)
        hblk = [self.sb(es, "h_h%d" % j, [128, KC, 512], F32) for j in range(2)]
        pq = [self.ps(es, "h_pq%d" % j, [128, 512]) for j in range(3)]
        pv = [self.ps(es, "h_pv%d" % j, [128, 512]) for j in range(2)]
        ob = [self.sb(es, "h_ob%d" % j, [128, 512], BF16) for j in range(3)]
        sg = [self.sb(es, "h_sg%d" % j, [128, 512], F32) for j in range(2)]
        ff = [self.sb(es, "h_ff%d" % j, [128, 512], F32) for j in range(2)]
        lf = [self.sb(es, "h_lf%d" % j, [128, 512], F32) for j in range(2)]
        kk = [self.sb(es, "h_kk%d" % j, [128, 512], BF16) for j in range(2)]
        vt = [self.sb(es, "h_vt%d" % j, [128, 1024], BF16) for j in range(2)]
        c = 0
        for bi, (t0, n) in enumerate(P1_BLOCKS):
            hb = hblk[bi % 2]
            self.load_h(hb, t0, n, "h_h%d" % (bi % 2))
            self.norm_mod(hb, t0, n, 0, uT, 0, W)
            for ch in range(32):
                p = pq[c % 3]
                for k in range(KC):
                    self.MM(p[:, 0:n], win[:, k, ch * 128:(ch + 1) * 128] if ch < 24 else win[:, k, 4096 + (ch - 24) * 128:4096 + (ch - 23) * 128],
                            uT[:, k, 0:n], k == 0, k == KC - 1, win.b + [uT.b[k]], p.b)
                if ch < 8:
                    o_ = ob[c % 3]
                    self.TS(o_[:, 0:n], p[:, 0:n], 128 ** -0.5, None, ALU.mult, None, p.b, o_.b)
                    self.DMA("pool", "h_ob%d" % (c % 3), self.sA[ch * 128:(ch + 1) * 128, t0:t0 + n], o_[:, 0:n], r=o_.b)
                elif ch < 24:
                    d, hh = (ch - 8) // 8, (ch - 8) % 8
                    s_, f_, l_, k_ = sg[c % 2], ff[c % 2], lf[c % 2], kk[c % 2]
                    self.ACT(s_[:, 0:n], p[:, 0:n], AF.Sigmoid, p.b, s_.b)
                    self.TS(f_[:, 0:n], s_[:, 0:n], self.lbc[:, hh, d, 1:2], self.lbc[:, hh, d, 0:1], ALU.mult, ALU.add, s_.b + self.lbc.b, f_.b)
                    self.ACT(l_[:, 0:n], f_[:, 0:n], AF.Ln, f_.b, l_.b)
                    self.TS(k_[:, 0:n], f_[:, 0:n], -1.0, 1.0, ALU.mult, ALU.add, f_.b, k_.b)
                    self.DMA("pool", "h_lf%d" % (c % 2), (self.sF1 if d == 0 else self.sF2)[hh * 128:(hh + 1) * 128, t0:t0 + n], l_[:, 0:n], r=l_.b)
                    self.DMA("pool", "h_kk%d" % (c % 2), (self.sB if d == 0 else self.sC)[hh * 128:(hh + 1) * 128, t0:t0 + n], k_[:, 0:n], r=k_.b)
                else:
                    hh = ch - 24
                    o_ = ob[c % 3]
                    self.ACT(o_[:, 0:n], p[:, 0:n], AF.Silu, p.b, o_.b)
                    self.DMA("pool", "h_ob%d" % (c % 3), self.sG[hh * 128:(hh + 1) * 128, t0:t0 + n], o_[:, 0:n], r=o_.b)
                c += 1
            for j in range(n // 128):
                v_ = vt[j % 2]
                for hh in range(2):
                    p = pv[hh]
                    for k in range(KC):
                        self.MM(p[:, :], uT[:, k, j * 128:(j + 1) * 128], win[:, k, 3072 + hh * 512:3072 + (hh + 1) * 512], k == 0, k == KC - 1,
                                win.b + [uT.b[k]], p.b)
                    self.CP(v_[:, hh * 512:(hh + 1) * 512], p[:, :], p.b, v_.b, eng=("act" if hh else "dve"))
                self.DMA("pool", "h_vt%d" % (j % 2), self.sV[t0 + j * 128:t0 + (j + 1) * 128, :], v_[:], r=v_.b)
        self.S.barrier()


def hgrn_p2(self):
    I = self.I
    NU = 16
    with ExitStack() as es:
        mk = [self.sb(es, "h2_mask%d" % d, [128, 128], F32) for d in range(2)]
        self.DMA("sp", "h2_m0", mk[0][:], I["maskf"][:, :], w=mk[0].b)
        self.DMA("sp", "h2_m1", mk[1][:], I["maskb"][:, :], w=mk[1].b)
        St = self.sb(es, "h2_S", [128, NU, 128], F32, n=NU)
        self.MEMSET(St[:], 0.0, St.b)
        onesr = self.sb(es, "h2_ones", [128, 128], F32)
        self.MEMSET(onesr[:], 1.0, onesr.b)
        f32t = {nm: self.sb(es, "h2_" + nm, [128, NU, 128], F32, n=NU) for nm in ("cpre", "base", "e1", "e2", "e3")}
        bft = {nm: self.sb(es, "h2_" + nm, [128, NU, 128], BF16, n=NU) for nm in ("qd", "qi", "attm", "ketm", "Sbf")}
        ekt = [self.sb(es, "h2_ek%d" % j, [128, NU, 128], F32, n=NU) for j in range(4)]
        kdt = [self.sb(es, "h2_kd%d" % j, [128, NU, 128], BF16, n=NU) for j in range(4)]
        for j in range(4):
            self.MEMSET(kdt[j][:], 0.0, kdt[j].b)
        bft["ke"] = self.sb(es, "h2_ke", [128, NU, 128], F32, n=NU)
        cols = self.sb(es, "h2_cols", [128, NU, 16], F32, n=NU)
        self.MEMSET(cols[:], 0.0, cols.b)
        ld = {}
        for d in range(2):
            for j in range(2):
                ld[(d, j)] = dict(q=self.sb(es, "h2_q%d%d" % (d, j), [128, 8, 128], BF16), k=self.sb(es, "h2_k%d%d" % (d, j), [128, 8, 128], BF16),
                                  lf=self.sb(es, "h2_lf%d%d" % (d, j), [128, 8, 128], F32), v=self.sb(es, "h2_v%d%d" % (d, j), [128, 1024], BF16),
                                  o=self.sb(es, "h2_o%d%d" % (d, j), [128, 8, 128], F32, n=8))
        pA = [self.ps(es, "h2_pA%d" % j, [128, 4, 128], F32) for j in range(2)]
        pO = [self.ps(es, "h2_pO%d" % j, [128, 4, 128], F32) for j in range(2)]
        pS = [self.ps(es, "h2_pS%d" % j, [128, 4, 128], F32) for j in range(2)]
        pT = [self.ps(es, "h2_pT%d" % j, [128, 4, 128], F32) for j in range(2)]
        order = [list(range(NT)), [1, 0] + list(range(NT - 1, 1, -1))]
        srcK = [self.sB, self.sC]
        srcL = [self.sF1, self.sF2]
        dstO = [self.sF3, self.sF4]
        for step in range(min(NT, HG_STEPS)):
            L = []
            for d in range(2):
                tl = order[d][step]
                c0 = tl * 128
                b = ld[(d, step % 2)]
                key = "h2_%d%d" % (d, step % 2)
                self.DMA("sp", key + "q", b["q"][:], self.sA[0:1024, :].rearrange("(h p) t -> p h t", p=128)[:, :, c0:c0 + 128], w=b["q"].b)
                self.DMA("sp", key + "k", b["k"][:], srcK[d].rearrange("(h p) t -> p h t", p=128)[:, :, c0:c0 + 128], w=b["k"].b)
                self.DMA("sp", key + "l", b["lf"][:], srcL[d].rearrange("(h p) t -> p h t", p=128)[:, :, c0:c0 + 128], w=b["lf"].b)
                self.DMA("sp", key + "v", b["v"][:], self.sV[c0:c0 + 128, :], w=b["v"].b)
                L.append(b)
            units = [(d, h) for d in range(2) for h in range(8)]
            for u, (d, h) in enumerate(units):
                if HG_STOP < 1:
                    break
                b = L[d]
                cp, ba, e1, e2, e3 = (f32t[nm] for nm in ("cpre", "base", "e1", "e2", "e3"))
                cl = cols[:, u, :]
                cb_ = [cols.b[u]]
                self.S.op("dve", lambda e, o=cp[:, u, :], x=b["lf"][:, h, :]: e.tensor_tensor_scan(out=o, data0=onesr[:], data1=x, initial=0.0,
                                                                                            op0=ALU.mult, op1=ALU.add),
                          b["lf"].b + onesr.b, [cp.b[u]])
                tot = cp[:, u, 127:128]
                if d == 0:
                    base, baseb = cp[:, u, :], [cp.b[u]]
                    self.CP(cl[:, 1:4], cp[:, u, 31:96:32], [cp.b[u]], cb_)
                    self.TS(cl[:, 5:8], cp[:, u, 31:96:32], -1.0, None, ALU.mult, None, [cp.b[u]], cb_)
                else:
                    self.TT(ba[:, u, :], b["lf"][:, h, :], cp[:, u, :], ALU.subtract, b["lf"].b + [cp.b[u]], [ba.b[u]])
                    base, baseb = ba[:, u, :], [ba.b[u]]
                    self.CP(cl[:, 0:3], ba[:, u, 32:128:32], baseb, cb_)
                    self.TS(cl[:, 4:7], ba[:, u, 32:128:32], -1.0, None, ALU.mult, None, baseb, cb_)
                    self.TS(cl[:, 3:4], tot, -1.0, None, ALU.mult, None, [cp.b[u]], cb_)
                    self.CP(cl[:, 7:8], tot, [cp.b[u]], cb_)
                rb_ = baseb + cb_ + [cp.b[u]]
                for j in range(4):
                    sl = slice(32 * j, 32 * j + 32)
                    rng = slice(0, 32 * j + 32) if d == 0 else slice(32 * j, 128)
                    if d == 0 and j == 0:
                        self.ACT(e1[:, u, sl], base[:, sl], AF.Exp, rb_, [e1.b[u]])
                        self.ACT(ekt[j][:, u, rng], base[:, rng], AF.Exp, rb_, [ekt[j].b[u]], scale=-1.0)
                    else:
                        self.ACT(e1[:, u, sl], base[:, sl], AF.Exp, rb_, [e1.b[u]], bias=cl[:, 4 + j:5 + j], scale=1.0)
                        self.ACT(ekt[j][:, u, rng], base[:, rng], AF.Exp, rb_, [ekt[j].b[u]], bias=cl[:, j:j + 1], scale=-1.0)
                    self.TT(kdt[j][:, u, rng], b["k"][:, h, rng], ekt[j][:, u, rng], ALU.mult, b["k"].b + [ekt[j].b[u]], [kdt[j].b[u]], eng="pool")
                if d == 0:
                    self.ACT(e2[:, u, :], base, AF.Exp, rb_, [e2.b[u]])
                    self.ACT(e3[:, u, :], base, AF.Exp, rb_, [e3.b[u]], bias=tot, scale=-1.0)
                else:
                    self.ACT(e2[:, u, :], base, AF.Exp, rb_, [e2.b[u]], bias=tot, scale=1.0)
                    self.ACT(e3[:, u, :], base, AF.Exp, rb_, [e3.b[u]], scale=-1.0)
                self.ACT(cl[:, 8:9], tot, AF.Exp, [cp.b[u]], cb_)
                self.TT(bft["qd"][:, u, :], b["q"][:, h, :], e1[:, u, :], ALU.mult, b["q"].b + [e1.b[u]], [bft["qd"].b[u]])
                self.TT(bft["qi"][:, u, :], b["q"][:, h, :], e2[:, u, :], ALU.mult, b["q"].b + [e2.b[u]], [bft["qi"].b[u]])
                self.TT(bft["ke"][:, u, :], b["k"][:, h, :], e3[:, u, :], ALU.mult, b["k"].b + [e3.b[u]], [bft["ke"].b[u]], eng="pool")
                self.CP(bft["Sbf"][:, u, :], St[:, u, :], [St.b[u]], [bft["Sbf"].b[u]])
            for g0 in range(0, NU, 4):
                if HG_STOP < 2:
                    break
                pa, pt_ = pA[(g0 // 4) % 2], pT[(g0 // 4) % 2]
                for u in range(g0, g0 + 4):
                    for j in range(4):
                        self.MM(pa[:, u % 4, 32 * j:32 * j + 32], kdt[j][:, u, :], bft["qd"][:, u, 32 * j:32 * j + 32], True, True,
                                [kdt[j].b[u], bft["qd"].b[u]], pa.b)
                    self.TR(pt_[:, u % 4, :], bft["ke"][:, u, :], self.ident[:], [bft["ke"].b[u]] + self.ident.b, pt_.b)
                for u in range(g0, g0 + 4):
                    d = units[u][0]
                    self.TT(bft["attm"][:, u, :], pa[:, u % 4, :], mk[d][:], ALU.mult, pa.b + mk[d].b, [bft["attm"].b[u]])
                    self.CP(bft["ketm"][:, u, :], pt_[:, u % 4, :], pt_.b, [bft["ketm"].b[u]], eng="act")
            for g0 in range(0, NU, 4):
                if HG_STOP < 3:
                    break
                po, ps_ = pO[(g0 // 4) % 2], pS[(g0 // 4) % 2]
                for u in range(g0, g0 + 4):
                    d, h = units[u]
                    b = L[d]
                    vh = b["v"][:, h * 128:(h + 1) * 128]
                    self.MM(po[:, u % 4, :], vh, bft["attm"][:, u, :], True, False, b["v"].b + [bft["attm"].b[u]], po.b)
                    self.MM(po[:, u % 4, :], bft["Sbf"][:, u, :], bft["qi"][:, u, :], False, True, [bft["Sbf"].b[u], bft["qi"].b[u]], po.b)
                    self.MM(ps_[:, u % 4, :], bft["ketm"][:, u, :], vh, True, True, b["v"].b + [bft["ketm"].b[u]], ps_.b)
                for u in range(g0, g0 + 4):
                    d, h = units[u]
                    b = L[d]
                    self.CP(b["o"][:, h, :], po[:, u % 4, :], po.b, [b["o"].b[h]], eng="act")
                    self.STT(St[:, u, :], St[:, u, :], cols[:, u, 8:9], ps_[:, u % 4, :], ALU.mult, ALU.add, [St.b[u], cols.b[u]] + ps_.b, [St.b[u]])
            for d in range(2):
                c0 = order[d][step] * 128
                self.DMA("pool", "h2_o%d%d" % (d, step % 2), dstO[d].rearrange("(h p) t -> p h t", p=128)[:, :, c0:c0 + 128], L[d]["o"][:], r=L[d]["o"].b)
        self.S.barrier()


def hgrn_p2b(self):
    with ExitStack() as es:
        W = self.work_tiles(es)
        of = [self.sb(es, "h3_of%d" % j, [128, 8, 512], F32) for j in range(2)]
        obk = [self.sb(es, "h3_ob%d" % j, [128, 8, 512], F32) for j in range(2)]
        gt = [self.sb(es, "h3_g%d" % j, [128, 8, 512], BF16) for j in range(2)]
        ot = [self.sb(es, "h3_ot%d" % j, [128, 8, 512], BF16) for j in range(2)]
        od = [self.sb(es, "h3_od%d" % j, [128, 512], F32) for j in range(2)]
        rsh = self.sb(es, "h3_rs", [128, 512], F32)
        for bi, (t0, n) in enumerate(P1_BLOCKS):
            j = bi % 2
            v = lambda a: a.rearrange("(h p) t -> p h t", p=128)[:, :, t0:t0 + n]
            self.DMA("sp", "h3_of%d" % j, of[j][:, :, 0:n], v(self.sF3), w=of[j].b)
            self.DMA("sp", "h3_ob%d" % j, obk[j][:, :, 0:n], v(self.sF4), w=obk[j].b)
            self.DMA("sp", "h3_g%d" % j, gt[j][:, :, 0:n], v(self.sG), w=gt[j].b)
            for h in range(8):
                o_ = od[h % 2]
                self.TT(o_[:, 0:n], of[j][:, h, 0:n], obk[j][:, h, 0:n], ALU.add, of[j].b + obk[j].b, o_.b, eng="pool")
                self.sumsq_rstd(lambda k: o_[:, 0:n], o_.b, 1, n, 128, W, rsh)
                self.STT(o_[:, 0:n], o_[:, 0:n], self.small[:, 0:1], rsh[:, 0:n], ALU.mult, ALU.mult, o_.b + self.small.b + rsh.b, o_.b)
                self.TT(ot[j][:, h, 0:n], o_[:, 0:n], gt[j][:, h, 0:n], ALU.mult, o_.b + gt[j].b, ot[j].b, eng="pool")
            self.DMA("pool", "h3_ot%d" % j, v(self.sOT), ot[j][:, :, 0:n], r=ot[j].b)
        self.S.barrier()


Builder.hgrn_p1 = hgrn_p1
Builder.hgrn_p2 = hgrn_p2
Builder.hgrn_p2b = hgrn_p2b
```

```python
import math
from contextlib import ExitStack
import numpy as np
import concourse.bass as bass
import concourse.mybir as mybir
from concourse.bass_utils import run_bass_kernel_spmd

F32 = mybir.dt.float32
BF16 = mybir.dt.bfloat16
AF = mybir.ActivationFunctionType
ALU = mybir.AluOpType

D = 1024
KC = 8
CTX = 256
LAT = 4096
T = CTX + LAT
NT = T // 128
GRID_W = 64
EPS = 1e-6
FF = 3584
FC = FF // 128
NE = 8
THETA = 10000.0
HG_STOP = 3
FORCE_MOE = False
MLA_PARTS = ('scale', 'lat', 'kr', 'q', 'kn', 'v')
HG_STEPS = 1000


class Buf:
    __slots__ = ("name", "w", "r", "x")

    def __init__(self, name=""):
        self.name = name
        self.w = None
        self.r = {}
        self.x = False


class Sched:
    ENGS = ("pe", "act", "dve", "pool", "sp")

    def __init__(self, nc):
        self.nc = nc
        self.lists = {e: [] for e in self.ENGS}
        self.cnt = {e: 0 for e in self.ENGS}
        self.semh = {}
        self.dcnt = {}
        self.waited = {e: {} for e in self.ENGS}
        self.keymap = {}
        self._ctx = []
        for e in self.ENGS:
            self._sem("E_" + e)

    def _sem(self, key):
        if key not in self.semh:
            cm = self.nc.semaphore(key)
            self.semh[key] = cm.__enter__()
            self._ctx.append(cm)
        return self.semh[key]

    def _deps(self, reads, writes):
        deps = {}

        def add(k, v):
            if deps.get(k, 0) < v:
                deps[k] = v
        for t in reads:
            if t.w is not None:
                add(*t.w)
        for t in writes:
            if t.w is not None:
                add(*t.w)
            for k, v in t.r.items():
                add(k, v)
        return deps

    def _emit_waits(self, eng, deps):
        wd = self.waited[eng]
        own = "E_" + eng
        for k, v in deps.items():
            if eng == "pe" and k == own:
                continue
            if wd.get(k, 0) >= v:
                continue
            wd[k] = v
            h = self.semh[k]
            self.lists[eng].append(lambda e, h=h, v=v: e.wait_ge(h, v))

    def _mark(self, ev, reads, writes):
        k, v = ev
        for t in reads:
            if t.r.get(k, 0) < v:
                t.r[k] = v
        for t in writes:
            t.w = ev
            t.r = {}

    def op(self, eng, fn, reads=(), writes=()):
        xs = [t for t in reads if t.x]
        if xs:
            writes = list(writes) + xs
        self._emit_waits(eng, self._deps(reads, writes))
        self.cnt[eng] += 1
        h = self.semh["E_" + eng]
        self.lists[eng].append(lambda e, fn=fn, h=h: fn(e).then_inc(h, 1))
        self._mark(("E_" + eng, self.cnt[eng]), reads, writes)

    def dma(self, q, key, out, in_, reads=(), writes=()):
        if key not in self.keymap:
            idx = len(self.keymap)
            self.keymap[key] = "D_%d" % idx
        key = self.keymap[key]
        h = self._sem(key)
        deps = self._deps(reads, writes)
        prev = self.dcnt.get(key, 0)
        if prev and deps.get(key, 0) < prev:
            deps[key] = prev
        self._emit_waits(q, deps)
        self.dcnt[key] = prev + 16
        self.lists[q].append(lambda e, out=out, in_=in_, h=h: e.dma_start(out=out, in_=in_).then_inc(h, 16))
        self._mark((key, prev + 16), reads, writes)

    def barrier(self):
        cur = {("E_" + e): self.cnt[e] for e in self.ENGS if self.cnt[e]}
        cur.update({k: v for k, v in self.dcnt.items() if v})
        for e in self.ENGS:
            self._emit_waits(e, dict(cur))
        self.keymap = {}

    def emit(self):
        nc = self.nc
        L = self.lists
        with nc.Block() as block:
            @block.tensor
            def _(e):
                for f in L["pe"]:
                    f(e)

            @block.scalar
            def _(e):
                for f in L["act"]:
                    f(e)

            @block.vector
            def _(e):
                for f in L["dve"]:
                    f(e)

            @block.gpsimd
            def _(e):
                for f in L["pool"]:
                    f(e)

            @block.sync
            def _(e):
                for f in L["sp"]:
                    f(e)


class Tile:
    def __init__(self, t, n=1, name=""):
        self.t = t
        self.b = [Buf(name + str(i)) for i in range(n)]

    def __getitem__(self, idx):
        return self.t[idx]


def _rope_tables(n, nrows, row0):
    cos = np.ones((nrows, T), np.float32)
    sin = np.zeros((nrows, T), np.float32)
    perm = np.zeros((nrows, nrows), np.float32)
    t = np.arange(LAT)
    pos = [(t // GRID_W).astype(np.float32), (t % GRID_W).astype(np.float32)]
    half = n // 2
    m = half
    inv = (THETA ** (-np.arange(0, m, 2, dtype=np.float32) / np.float32(m))).astype(np.float32)
    for f in range(n):
        blk, j = f // half, f % half
        fr = j % (m // 2)
        ang = (pos[blk] * inv[fr]).astype(np.float32)
        cos[row0 + f, CTX:] = np.cos(ang).astype(np.float32)
        s = np.sin(ang).astype(np.float32)
        if j < m // 2:
            sin[row0 + f, CTX:] = -s
            src = f + m // 2
        else:
            sin[row0 + f, CTX:] = s
            src = f - m // 2
        perm[row0 + src, row0 + f] = 1.0
    for f in range(nrows):
        if not (row0 <= f < row0 + n):
            perm[f, f] = 1.0
    return cos, sin, perm


def _consts():
    c = {}
    c["ident"] = np.eye(128, dtype=np.float32)
    s = np.arange(128)
    c["maskf"] = (s[:, None] <= s[None, :]).astype(np.float32)
    c["maskb"] = (s[:, None] >= s[None, :]).astype(np.float32)
    c1, s1, p1 = _rope_tables(32, 96, 64)
    c["cos1"], c["sin1"], c["perm1"] = c1, s1, p1
    c["cos1k"], c["sin1k"] = np.ascontiguousarray(c1[64:96]), np.ascontiguousarray(s1[64:96])
    c["perm1k"] = np.ascontiguousarray(p1[64:96, 64:96])
    c2, s2, p2 = _rope_tables(128, 128, 0)
    c["cos2"], c["sin2"], c["perm2"] = c2, s2, p2
    c3, s3, p3 = _rope_tables(64, 64, 0)
    c["cos3"] = np.concatenate([c3, c3], 0)
    c["sin3"] = np.concatenate([s3, s3], 0)
    pp = np.zeros((128, 128), np.float32)
    pp[:64, :64] = p3
    pp[64:, 64:] = p3
    c["perm3"] = pp
    return c


CONST_SHAPES = {k: v.shape for k, v in _consts().items()}

IN_SHAPES = {
    "x": [LAT, D], "c": [1, D], "ctx": [CTX, D], "c_ctx": [1, D],
    "mod_w": [4, D, 6 * D], "mod_b": [24, D], "norm_g": [16, D],
    "a_w_in": [D, 5120], "a_lb": [10, D], "a_onorm_g": [1, 128], "a_w_out": [D, D],
    "b_w_in": [D, 672], "b_qnorm_g": [3, 128], "b_kvnorm_g": [2, 128], "b_w_qb": [384, 1536],
    "b_w_kvb": [256, 2048], "b_w_out": [D, D],
    "c_w_in": [D, 1536], "c_qnorm_g": [1, 128], "c_knorm_g": [1, 128], "c_w_out": [D, D],
    "d_w_in": [D, 3072], "d_lambda": [1, 256], "d_onorm_g": [1, 128], "d_w_out": [D, D],
    "ffn_w_gu": [2, FC, 128, KC * 256], "ffn_w_down": [2, KC, 128, FF], "moe_router": [2, D, NE],
    "moe_w_gu": [2, NE, FC, 128, KC * 256], "moe_w_down": [2, NE, KC, 128, FF],
}


class Builder:
    def __init__(self, n_layers=4, debug=False):
        self.n_layers = n_layers
        self.debug = debug
        nc = self.nc = bass.Bass("TRN2", target_bir_lowering=False)
        self.S = Sched(nc)
        self.I = {}
        for k, shp in IN_SHAPES.items():
            self.I[k] = nc.dram_tensor(k, list(shp), F32, kind="ExternalInput").ap()
        for k, shp in CONST_SHAPES.items():
            self.I[k] = nc.dram_tensor(k, list(shp), F32, kind="ExternalInput").ap()
        self.out = nc.dram_tensor("out", [LAT, D], F32, kind="ExternalOutput").ap()
        if debug:
            self.dbg = nc.dram_tensor("dbg", [D, T], F32, kind="ExternalOutput").ap()

        def scr(name, shape, dt):
            return nc.dram_tensor(name, shape, dt, kind="Internal").ap()
        self.hT = scr("hT", [D, T], F32)
        self.hb = [Buf("hT%d" % i) for i in range(NT)]
        self.sA = scr("sA", [1536, T], BF16)
        self.sB = scr("sB", [1024, T], BF16)
        self.sC = scr("sC", [1024, T], BF16)
        self.sKR = scr("sKR", [32, T], BF16)
        self.sG = scr("sG", [D, T], BF16)
        self.sV = scr("sV", [T, 1024], BF16)
        self.sF1 = scr("sF1", [D, T], F32)
        self.sF2 = scr("sF2", [D, T], F32)
        self.sF3 = scr("sF3", [D, T], F32)
        self.sF4 = scr("sF4", [D, T], F32)
        self.sOT = scr("sOT", [D, T], BF16)
        self.uid = 0

    def sb(self, es, name, shape, dt, n=1):
        self.uid += 1
        t = es.enter_context(self.nc.sbuf_tensor("s%d_%s" % (self.uid, name), list(shape), dt))
        return Tile(t, n, name)

    def ps(self, es, name, shape, dt=F32, n=1):
        self.uid += 1
        t = es.enter_context(self.nc.psum_tensor("p%d_%s" % (self.uid, name), list(shape), dt))
        tl = Tile(t, n, name)
        for b in tl.b:
            b.x = True
        return tl

    @staticmethod
    def view(tile, ap):
        v = Tile(ap, 0)
        v.b = tile.b
        return v

    def MM(self, out, lhsT, rhs, start, stop, r, w):
        self.S.op("pe", lambda e: e.matmul(out, lhsT=lhsT, rhs=rhs, start=start, stop=stop), r, w)

    def TR(self, out, in_, ident, r, w):
        self.S.op("pe", lambda e: e.transpose(out, in_, ident), r, w)

    def ACT(self, out, in_, func, r, w, bias=None, scale=None, eng="act"):
        kw = {}
        if bias is not None:
            kw["bias"] = bias
        if scale is not None:
            kw["scale"] = scale
        self.S.op(eng, lambda e: e.activation(out=out, in_=in_, func=func, **kw), r, w)

    def TT(self, out, a, b, op, r, w, eng="dve"):
        self.S.op(eng, lambda e: e.tensor_tensor(out=out, in0=a, in1=b, op=op), r, w)

    def TS(self, out, a, s1, s2, op0, op1, r, w, eng="dve"):
        if s2 is None:
            self.S.op(eng, lambda e: e.tensor_scalar(out=out, in0=a, scalar1=s1, scalar2=None, op0=op0), r, w)
        else:
            self.S.op(eng, lambda e: e.tensor_scalar(out=out, in0=a, scalar1=s1, scalar2=s2, op0=op0, op1=op1), r, w)

    def STT(self, out, a, s, b, op0, op1, r, w, eng="dve"):
        self.S.op(eng, lambda e: e.scalar_tensor_tensor(out=out, in0=a, scalar=s, in1=b, op0=op0, op1=op1), r, w)

    def CP(self, out, in_, r, w, eng="dve"):
        if eng == "act":
            self.S.op(eng, lambda e: e.copy(out=out, in_=in_), r, w)
        else:
            self.S.op(eng, lambda e: e.tensor_copy(out=out, in_=in_), r, w)

    def RCP(self, out, in_, r, w):
        self.S.op("dve", lambda e: e.reciprocal(out=out, in_=in_), r, w)

    def MEMSET(self, ap, val, w, eng="pool"):
        self.S.op(eng, lambda e: e.memset(ap, val), (), w)

    def DMA(self, q, key, out, in_, r=(), w=()):
        self.S.dma(q, key, out, in_, r, w)

    def rstd(self, out, ss, n, r, w, tmp, tmpb):
        self.ACT(tmp, ss, AF.Sqrt, r, tmpb, bias=self.epsc[0:ss.shape[0], :], scale=1.0 / n)
        self.RCP(out, tmp, tmpb, w)

    def setup(self, es):
        I = self.I
        self.ident = self.sb(es, "ident", [128, 128], F32)
        self.identb = self.sb(es, "identb", [128, 128], BF16)
        self.onesf = self.sb(es, "onesf", [128, 128], F32)
        self.onesb = self.sb(es, "onesb", [128, 128], BF16)
        self.epsc = self.sb(es, "epsc", [128, 1], F32)
        self.DMA("sp", "c0", self.ident[:], I["ident"][:, :], w=self.ident.b)
        self.CP(self.identb[:], self.ident[:], self.ident.b, self.identb.b)
        self.MEMSET(self.onesf[:], 1.0, self.onesf.b)
        self.MEMSET(self.onesb[:], 1.0, self.onesb.b)
        self.MEMSET(self.epsc[:], EPS, self.epsc.b)
        self.epsc = self.epsc
        epsT = self.epsc
        self.epsc = epsT.t
        self.epsb = epsT.b
        self.condT = self.sb(es, "condT", [128, KC, 2], F32)
        self.normg = self.sb(es, "normg", [128, KC, 16], F32)
        self.modb = self.sb(es, "modb", [128, KC, 24], F32)
        self.alb = self.sb(es, "alb", [128, KC, 10], F32)
        self.small = self.sb(es, "smallc", [128, 9], F32)
        self.modc = self.sb(es, "modc", [128, 6, KC, 2], F32)
        self.Acol = self.sb(es, "Acol", [128, 2, KC, 2], F32)
        self.Bcol = self.sb(es, "Bcol", [128, 2, KC, 2], F32)
        self.Gcol = self.sb(es, "Gcol", [128, 2, KC, 2], F32)
        self.lbc = self.sb(es, "lbc", [128, KC, 2, 2], F32)
        with ExitStack() as e2:
            rows = self.sb(e2, "rows", [24, D], F32)
            pt = self.ps(e2, "setup_ps", [128, 512], F32)

            def rows_to_cols(src_ap, R, dst, C, key):
                self.DMA("sp", key, rows[0:R, 0:C * 128], src_ap, w=rows.b)
                for c in range(C):
                    self.TR(pt[:, 0:R], rows[0:R, c * 128:(c + 1) * 128], self.ident[0:R, 0:R],
                            rows.b + self.ident.b, pt.b)
                    self.CP(dst(c), pt[:, 0:R], pt.b, self._colw)
            self._colw = self.condT.b
            rows_to_cols(I["c"][:, :], 1, lambda c: self.condT[:, c, 0:1], KC, "rw")
            rows_to_cols(I["c_ctx"][:, :], 1, lambda c: self.condT[:, c, 1:2], KC, "rw")
            sg = self.sb(e2, "sgc", [128, KC, 2], F32)
            self.ACT(sg[:], self.condT[:], AF.Sigmoid, self.condT.b, sg.b)
            self.TT(self.condT[:], self.condT[:], sg[:], ALU.mult, sg.b + self.condT.b, self.condT.b)
            self._colw = self.normg.b
            rows_to_cols(I["norm_g"][:, :], 16, lambda c: self.normg[:, c, :], KC, "rw")
            self._colw = self.modb.b
            rows_to_cols(I["mod_b"][:, :], 24, lambda c: self.modb[:, c, :], KC, "rw")
            self._colw = self.alb.b
            rows_to_cols(I["a_lb"][:, :], 10, lambda c: self.alb[:, c, :], KC, "rw")
            self._colw = self.small.b
            rows_to_cols(I["a_onorm_g"][:, :], 1, lambda c: self.small[:, 0:1], 1, "rw")
            rows_to_cols(I["b_qnorm_g"][:, :], 3, lambda c: self.small[:, 1:4], 1, "rw")
            rows_to_cols(I["b_kvnorm_g"][:, :], 2, lambda c: self.small[:, 4:6], 1, "rw")
            rows_to_cols(I["c_qnorm_g"][:, :], 1, lambda c: self.small[:, 6:7], 1, "rw")
            rows_to_cols(I["c_knorm_g"][:, :], 1, lambda c: self.small[:, 7:8], 1, "rw")
            rows_to_cols(I["d_onorm_g"][:, :], 1, lambda c: self.small[:, 8:9], 1, "rw")
            ex = self.sb(e2, "albx", [128, KC, 10], F32)
            sm = self.sb(e2, "albs", [128, KC, 2], F32)
            self.ACT(ex[:], self.alb[:], AF.Exp, self.alb.b, ex.b)
            for d in range(2):
                self.TT(sm[:, :, d], ex[:, :, 5 * d], ex[:, :, 5 * d + 1], ALU.add, ex.b, sm.b)
                for j in range(2, 5):
                    self.TT(sm[:, :, d], sm[:, :, d], ex[:, :, 5 * d + j], ALU.add, ex.b + sm.b, sm.b)
            self.RCP(sm[:], sm[:], sm.b, sm.b)
            for d in range(2):
                self.TT(self.lbc[:, :, d, 0], ex[:, :, 5 * d], sm[:, :, d], ALU.mult, ex.b + sm.b, self.lbc.b)
                self.TS(self.lbc[:, :, d, 1], self.lbc[:, :, d, 0], -1.0, 1.0, ALU.mult, ALU.add, self.lbc.b, self.lbc.b)
            xin = [self.sb(e2, "xin%d" % i, [128, D], F32) for i in range(2)]
            stg = [self.sb(e2, "xstg%d" % i, [128, KC, 512], F32) for i in range(2)]
            pts = [self.ps(e2, "xps%d" % i, [128, 512], F32) for i in range(2)]
            blocks = [(0, 2)] + [(2 + 4 * i, 4) for i in range(8)]
            for bi, (t0, nt) in enumerate(blocks):
                st = stg[bi % 2]
                for j in range(nt):
                    tt = t0 + j
                    xt = xin[tt % 2]
                    src = I["ctx"][tt * 128:(tt + 1) * 128, :] if tt < 2 else I["x"][(tt - 2) * 128:(tt - 1) * 128, :]
                    self.DMA("sp", "xin%d" % (tt % 2), xt[:], src, w=xt.b)
                    for k in range(KC):
                        p = pts[k % 2]
                        self.TR(p[:, 0:128], xt[:, k * 128:(k + 1) * 128], self.ident[:], xt.b + self.ident.b, p.b)
                        self.CP(st[:, k, j * 128:(j + 1) * 128], p[:, 0:128], p.b, st.b, eng=("act" if k % 2 else "dve"))
                self.DMA("pool", "xst%d" % (bi % 2), self.hT.rearrange("(k p) t -> p k t", p=128)[:, :, t0 * 128:(t0 + nt) * 128],
                         st[:, :, 0:nt * 128], r=st.b, w=[self.hb[t0 + j] for j in range(nt)])
            self.S.barrier()

    def mod_phase(self, i):
        I = self.I
        with ExitStack() as es:
            wb = [self.sb(es, "modw%d" % j, [128, KC, 512], F32) for j in range(2)]
            pm = self.ps(es, "modps", [128, 48, 2], F32)
            for cb in range(12):
                w = wb[cb % 2]
                self.DMA("sp", "modw%d" % (cb % 2), w[:], I["mod_w"][i].rearrange("(k p) n -> p k n", p=128)[:, :, cb * 512:(cb + 1) * 512], w=w.b)
                for f in range(4):
                    fc = cb * 4 + f
                    for k in range(KC):
                        self.MM(pm[:, fc, :], w[:, k, f * 128:(f + 1) * 128], self.condT[:, k, :], k == 0, k == KC - 1,
                                w.b + self.condT.b, pm.b)
            for s in range(2):
                self.TT(self.modc[:, :, :, s], pm[:, :, s].rearrange("p (j c) -> p j c", j=6),
                        self.modb[:, :, 6 * i:6 * i + 6].rearrange("p c j -> p j c"), ALU.add, pm.b + self.modb.b, self.modc.b)
            for sub in range(2):
                jb, ja, jg = 3 * sub, 3 * sub + 1, 3 * sub + 2
                gin, gout = 4 * i + 2 * sub, 4 * i + 2 * sub + 1
                for s in range(2):
                    self.STT(self.Acol[:, sub, :, s], self.modc[:, ja, :, s], 1.0, self.normg[:, :, gin], ALU.add, ALU.mult,
                             self.modc.b + self.normg.b, self.Acol.b)
                    self.CP(self.Bcol[:, sub, :, s], self.modc[:, jb, :, s], self.modc.b, self.Bcol.b)
                    self.TT(self.Gcol[:, sub, :, s], self.modc[:, jg, :, s], self.normg[:, :, gout], ALU.mult,
                            self.modc.b + self.normg.b, self.Gcol.b)
            self.S.barrier()

    @staticmethod
    def segs(t0, n):
        out = []
        if t0 < CTX:
            m = min(CTX, t0 + n) - t0
            out.append((0, m, 1))
            if n > m:
                out.append((m, n - m, 0))
        else:
            out.append((0, n, 0))
        return out

    def load_h(self, hblk, t0, n, key):
        self.DMA("sp", key, hblk[:, :, 0:n], self.hT.rearrange("(k p) t -> p k t", p=128)[:, :, t0:t0 + n],
                 r=[self.hb[t] for t in range(t0 // 128, (t0 + n) // 128)], w=hblk.b)

    def store_h(self, hblk, t0, n, key):
        self.DMA("pool", key, self.hT.rearrange("(k p) t -> p k t", p=128)[:, :, t0:t0 + n], hblk[:, :, 0:n],
                 r=hblk.b, w=[self.hb[t] for t in range(t0 // 128, (t0 + n) // 128)])

    def sumsq_rstd(self, src_fn, src_b, nchunks, n, ndim, W, out_rstd):
        for k in range(nchunks):
            sq = W["sq"][k % 2]
            self.ACT(sq[:, 0:n], src_fn(k), AF.Square, src_b, sq.b)
            self.MM(W["pss"][:, 0:n], self.onesb[:], sq[:, 0:n], k == 0, k == nchunks - 1, self.onesb.b + sq.b, W["pss"].b)
        self.rstd(out_rstd[:, 0:n], W["pss"][:, 0:n], ndim, W["pss"].b + self.epsb, out_rstd.b, W["rt"][:, 0:n], W["rt"].b)

    def norm_mod(self, hblk, t0, n, sub, uT, ucol0, W, u32=None):
        self.sumsq_rstd(lambda k: hblk[:, k, 0:n], hblk.b, KC, n, D, W, W["rs"])
        for k in range(KC):
            tmp = W["tmp"][k % 2]
            for (o, m, s) in self.segs(t0, n):
                self.STT(tmp[:, o:o + m], hblk[:, k, o:o + m], self.Acol[:, sub, k, s:s + 1], W["rs"][:, o:o + m], ALU.mult, ALU.mult,
                         hblk.b + self.Acol.b + W["rs"].b, tmp.b)
                dst = uT[:, k, ucol0 + o:ucol0 + o + m] if u32 is None else u32[k][:, o:o + m]
                dstb = [uT.b[k]] if u32 is None else u32[k].b
                self.ACT(dst, tmp[:, o:o + m], AF.Identity, tmp.b + self.Bcol.b, dstb, bias=self.Bcol[:, sub, k, s:s + 1], scale=1.0)

    def resid_update(self, hblk, t0, n, sub, y_fn, y_b, W):
        self.sumsq_rstd(y_fn, y_b, KC, n, D, W, W["rs"])
        for k in range(KC):
            tmp = W["tmp"][k % 2]
            for (o, m, s) in self.segs(t0, n):
                self.STT(tmp[:, o:o + m], y_fn(k)[:, o:o + m], self.Gcol[:, sub, k, s:s + 1], W["rs"][:, o:o + m], ALU.mult, ALU.mult,
                         y_b + self.Gcol.b + W["rs"].b, tmp.b)
                self.TT(hblk[:, k, o:o + m], hblk[:, k, o:o + m], tmp[:, o:o + m], ALU.add, hblk.b + tmp.b, hblk.b, eng="pool")

    def work_tiles(self, es, n=512):
        W = {}
        W["sq"] = [self.sb(es, "w_sq%d" % i, [128, n], BF16) for i in range(2)]
        W["pss"] = self.ps(es, "w_pss", [128, n], F32)
        W["rt"] = self.sb(es, "w_rt", [128, n], F32)
        W["rs"] = self.sb(es, "w_rs", [128, n], F32)
        W["tmp"] = [self.sb(es, "w_tmp%d" % i, [128, n], F32) for i in range(2)]
        return W

    def load_w(self, wt, src, key, kc=None):
        kc = kc or src.shape[0] // 128
        v = src.rearrange("(k p) n -> p k n", p=128)
        for k in range(kc):
            self.DMA("pool", key, wt[:, k, :], v[:, k, :], w=wt.b)

    def final_out(self):
        with ExitStack() as es:
            hblk = [self.sb(es, "fo_h%d" % i, [128, KC, 512], F32) for i in range(2)]
            ot = [self.sb(es, "fo_o%d" % i, [128, D], F32) for i in range(2)]
            pts = [self.ps(es, "fo_ps%d" % i, [128, 512], F32) for i in range(2)]
            for bi in range(8):
                t0 = CTX + bi * 512
                hb = hblk[bi % 2]
                self.load_h(hb, t0, 512, "foh%d" % (bi % 2))
                for j in range(4):
                    o = ot[j % 2]
                    for kk in range(2):
                        p = pts[kk]
                        for q in range(4):
                            k = kk * 4 + q
                            self.TR(p[:, q * 128:(q + 1) * 128], hb[:, k, j * 128:(j + 1) * 128], self.ident[:], hb.b + self.ident.b, p.b)
                        self.CP(o[:, kk * 512:(kk + 1) * 512], p[:], p.b, o.b, eng=("act" if kk else "dve"))
                    r0 = bi * 512 + j * 128
                    self.DMA("pool", "foo%d" % (j % 2), self.out[r0:r0 + 128, :], o[:], r=o.b)
            if self.debug:
                for bi in range(9):
                    t0, n = (0, 256) if bi == 0 else (CTX + (bi - 1) * 512, 512)
                    hb = hblk[bi % 2]
                    self.load_h(hb, t0, n, "foh%d" % (bi % 2))
                    self.DMA("pool", "dbg%d" % (bi % 2), self.dbg.rearrange("(k p) t -> p k t", p=128)[:, :, t0:t0 + n], hb[:, :, 0:n], r=hb.b)
            self.S.barrier()


def _blocks(t0, n, bs=512):
    out = []
    o = 0
    while o < n:
        m = min(bs, n - o)
        out.append((t0 + o, o, m))
        o += m
    return out


def ffn_phase(self, i, wout_ap):
    I = self.I
    moe = (i % 2 == 1) or FORCE_MOE
    li = i // 2
    E = NE if moe else 1
    NS = 4
    CS = FC // NS
    last = (i == 3)
    gt = [(2, 8), (10, 8), (18, 8), (26, 8)] if last else [(0, 9), (9, 9), (18, 8), (26, 8)]
    TGM = 9 * 128
    wgu_l = I["moe_w_gu"][li] if moe else I["ffn_w_gu"]
    wd_l = I["moe_w_down"][li] if moe else I["ffn_w_down"]
    if not moe:
        wgu_l = wgu_l[li:li + 1]
        wd_l = wd_l[li:li + 1]
    with ExitStack() as es:
        W = self.work_tiles(es)
        wout = self.sb(es, "f_wout", [128, KC, D], BF16)
        self.load_w(wout, wout_ap, "f_wout")
        uT = self.sb(es, "f_uT", [128, KC, TGM], BF16, n=KC)
        hid = [self.sb(es, "f_hid%d" % j, [128, CS, TGM], BF16, n=CS) for j in range(2)]
        yacc = self.sb(es, "f_yacc", [128, KC, TGM], F32, n=KC)
        hblk = self.sb(es, "f_hblk", [128, KC, 512], F32)
        otb = self.sb(es, "f_otb", [128, KC, 512], BF16)
        ysb = self.sb(es, "f_ysb", [128, KC, 512], F32, n=KC)
        wgu = [self.sb(es, "f_wgu%d" % j, [128, KC, 256], BF16) for j in range(3)]
        wdt = [self.sb(es, "f_wd%d" % j, [128, CS, 128], BF16) for j in range(3)]
        sgt = [self.sb(es, "f_sg%d" % j, [128, 512], F32) for j in range(2)]
        pG = [self.ps(es, "f_pG%d" % j, [128, 512]) for j in range(2)]
        pU = [self.ps(es, "f_pU%d" % j, [128, 512]) for j in range(2)]
        pY = [self.ps(es, "f_pY%d" % j, [128, 512]) for j in range(2)]
        pX = self.ps(es, "f_pX", [128, 512])
        if moe:
            rt_w = self.sb(es, "f_router", [128, KC, NE], F32)
            self.DMA("sp", "f_router", rt_w[:], I["moe_router"][li].rearrange("(k p) e -> p k e", p=128), w=rt_w.b)
            u32 = self.sb(es, "f_u32", [128, KC, 512], F32, n=KC)
            t1 = [self.sb(es, "f_t1%d" % j, [128, 512], F32) for j in range(2)]
            CB = self.sb(es, "f_CB", [128, TGM], BF16)
            comb = self.sb(es, "f_comb", [128, 9, NE], F32)
            dg = [self.sb(es, "f_dg%d" % j, [128, 128], F32) for j in range(2)]
            rsm = self.sb(es, "f_rsm", [128, 40], F32)
        cnt = {"wgu": 0, "wd": 0, "gu": 0, "y": 0}
        for (tile0, ntile) in gt:
            t0g, ng = tile0 * 128, ntile * 128
            blks = _blocks(t0g, ng)
            for (t0, o, n) in blks:
                self.load_h(hblk, t0, n, "f_hblk")
                self.DMA("sp", "f_otb", otb[:, :, 0:n], self.sOT.rearrange("(k p) t -> p k t", p=128)[:, :, t0:t0 + n], w=otb.b)
                for fo in range(KC):
                    p = pY[fo % 2]
                    for k in range(KC):
                        self.MM(p[:, 0:n], wout[:, k, fo * 128:(fo + 1) * 128], otb[:, k, 0:n], k == 0, k == KC - 1, wout.b + otb.b, p.b)
                    self.CP(ysb[:, fo, 0:n], p[:, 0:n], p.b, [ysb.b[fo]], eng=("act" if fo % 2 else "dve"))
                self.resid_update(hblk, t0, n, 0, lambda k: ysb[:, k, 0:n], ysb.b, W)
                self.store_h(hblk, t0, n, "f_hst")
                if not moe:
                    self.norm_mod(hblk, t0, n, 1, uT, o, W)
                else:
                    self.sumsq_rstd(lambda k: hblk[:, k, 0:n], hblk.b, KC, n, D, W, W["rs"])
                    nt_ = n // 128
                    for k in range(KC):
                        tmp = W["tmp"][k % 2]
                        for (oo, m, s) in self.segs(t0, n):
                            self.STT(tmp[:, oo:oo + m], hblk[:, k, oo:oo + m], self.Acol[:, 1, k, s:s + 1], W["rs"][:, oo:oo + m], ALU.mult, ALU.mult,
                                     hblk.b + self.Acol.b + W["rs"].b, tmp.b)
                            self.ACT(u32[:, k, oo:oo + m], tmp[:, oo:oo + m], AF.Identity, tmp.b + self.Bcol.b, [u32.b[k]], bias=self.Bcol[:, 1, k, s:s + 1], scale=1.0)
                        self.CP(uT[:, k, o:o + n], u32[:, k, 0:n], [u32.b[k]], [uT.b[k]], eng="pool")
                    for j in range(nt_):
                        for k in range(KC):
                            self.MM(pX[:, j * 8:(j + 1) * 8], u32[:, k, j * 128:(j + 1) * 128], rt_w[:, k, :], k == 0, k == KC - 1, [u32.b[k]] + rt_w.b, pX.b)
                    for j in range(nt_):
                        tt = o // 128 + j
                        lg, m8, mk, ex = rsm[:, 0:8], rsm[:, 8:16], rsm[:, 16:24], rsm[:, 24:32]
                        self.CP(lg, pX[:, j * 8:(j + 1) * 8], pX.b, rsm.b)
                        self.S.op("dve", lambda e, m8=m8, lg=lg: e.max(out=m8, in_=lg), rsm.b, rsm.b)
                        self.TS(mk, lg, rsm[:, 9:10], None, ALU.is_ge, None, rsm.b, rsm.b)
                        self.TS(rsm[:, 32:33], rsm[:, 8:9], -1.0, None, ALU.mult, None, rsm.b, rsm.b)
                        self.ACT(ex, lg, AF.Exp, rsm.b, rsm.b, bias=rsm[:, 32:33], scale=1.0)
                        self.TT(ex, ex, mk, ALU.mult, rsm.b, rsm.b)
                        self.S.op("dve", lambda e, ex=ex: e.reduce_sum(out=rsm[:, 33:34], in_=ex, axis=mybir.AxisListType.X), rsm.b, rsm.b)
                        self.RCP(rsm[:, 34:35], rsm[:, 33:34], rsm.b, rsm.b)
                        self.TS(comb[:, tt, :], ex, rsm[:, 34:35], None, ALU.mult, None, rsm.b, comb.b)
            jobs = [(e, s) for e in range(E) for s in range(NS)]

            def GU(j):
                e, s = jobs[j]
                hd = hid[j % 2]
                if moe and s == 0:
                    for (t0, o, n) in blks:
                        for q in range(n // 128):
                            tt = o // 128 + q
                            d_ = dg[tt % 2]
                            self.TS(d_[:], self.ident[:], comb[:, tt, e:e + 1], None, ALU.mult, None, self.ident.b + comb.b, d_.b)
                            self.MM(pX[:, q * 128:(q + 1) * 128], self.onesf[:], d_[:], True, True, self.onesf.b + d_.b, pX.b)
                        self.CP(CB[:, o:o + n], pX[:, 0:n], pX.b, CB.b, eng="act")
                for cc in range(CS):
                    c = s * CS + cc
                    w = wgu[cnt["wgu"] % 3]
                    cnt["wgu"] += 1
                    self.DMA("pool", "f_wgu%d" % ((cnt["wgu"] - 1) % 3), w[:].rearrange("p k n -> p (k n)"), wgu_l[e, c], w=w.b)
                    for (t0, o, n) in blks:
                        g_, u_ = pG[cnt["gu"] % 2], pU[cnt["gu"] % 2]
                        sg = sgt[cnt["gu"] % 2]
                        for k in range(KC):
                            self.MM(g_[:, 0:n], w[:, k, 0:128], uT[:, k, o:o + n], k == 0, k == KC - 1, w.b + [uT.b[k]], g_.b)
                        for k in range(KC):
                            self.MM(u_[:, 0:n], w[:, k, 128:256], uT[:, k, o:o + n], k == 0, k == KC - 1, w.b + [uT.b[k]], u_.b)
                        self.ACT(sg[:, 0:n], g_[:, 0:n], AF.Silu, g_.b, sg.b)
                        if moe:
                            tt1 = t1[cnt["gu"] % 2]
                            self.TT(tt1[:, 0:n], sg[:, 0:n], u_[:, 0:n], ALU.mult, sg.b + u_.b, tt1.b)
                            self.TT(hd[:, cc, o:o + n], tt1[:, 0:n], CB[:, o:o + n], ALU.mult, tt1.b + CB.b, [hd.b[cc]])
                        else:
                            self.TT(hd[:, cc, o:o + n], sg[:, 0:n], u_[:, 0:n], ALU.mult, sg.b + u_.b, [hd.b[cc]])
                        cnt["gu"] += 1

            def DN(j):
                e, s = jobs[j]
                hd = hid[j % 2]
                for fo in range(KC):
                    w = wdt[cnt["wd"] % 3]
                    cnt["wd"] += 1
                    self.DMA("pool", "f_wd%d" % ((cnt["wd"] - 1) % 3), w[:].rearrange("p c n -> p (c n)"), wd_l[e, fo][:, s * CS * 128:(s + 1) * CS * 128], w=w.b)
                    for (t0, o, n) in blks:
                        p = pY[cnt["y"] % 2]
                        cnt["y"] += 1
                        for cc in range(CS):
                            self.MM(p[:, 0:n], w[:, cc, :], hd[:, cc, o:o + n], cc == 0, cc == CS - 1, w.b + [hd.b[cc]], p.b)
                        if j == 0:
                            self.CP(yacc[:, fo, o:o + n], p[:, 0:n], p.b, [yacc.b[fo]], eng="act")
                        else:
                            self.TT(yacc[:, fo, o:o + n], yacc[:, fo, o:o + n], p[:, 0:n], ALU.add, p.b + [yacc.b[fo]], [yacc.b[fo]])
            GU(0)
            for j in range(len(jobs)):
                if j + 1 < len(jobs):
                    GU(j + 1)
                DN(j)
            for (t0, o, n) in blks:
                self.load_h(hblk, t0, n, "f_hblk")
                self.resid_update(hblk, t0, n, 1, lambda k: yacc[:, k, o:o + n], yacc.b, W)
                self.store_h(hblk, t0, n, "f_hst")
        self.S.barrier()


Builder.ffn_phase = ffn_phase


P1_BLOCKS = [(0, 256)] + [(CTX + 512 * b, 512) for b in range(8)]


def attn_tiles(self, es, nS=5):
    A = {}
    A["nS"] = nS
    A["pS"] = [self.ps(es, "a_pS%d" % j, [128, 512]) for j in range(nS)]
    A["pT"] = [self.sb(es, "a_pT%d" % j, [128, 512], BF16) for j in range(6)]
    A["pO"] = [self.ps(es, "a_pO%d" % j, [128, 512]) for j in range(2)]
    A["pB"] = self.ps(es, "a_pB", [128, 512])
    A["acc"] = [[self.sb(es, "a_acc%d%d" % (i, j), [128, 512], F32) for j in range(2)] for i in range(2)]
    A["osb"] = [self.sb(es, "a_osb%d" % j, [128, 512], F32) for j in range(2)]
    A["n"] = 0
    A["ns"] = 0
    return A


def attn_block(self, A, kfn, q_ap, vfn, kts, nq, dv, scale, rb, out_ap, out_b, after=None):
    i0 = A["n"]
    A["n"] += 1
    pO, osb, acc = A["pO"][i0 % 2], A["osb"][i0 % 2], A["acc"][i0 % 2]
    n = len(kts)
    LA = A["nS"] - 1
    slots = {}

    def qk(i):
        ps_ = A["pS"][A["ns"] % A["nS"]]
        pt = A["pT"][A["ns"] % 6]
        A["ns"] += 1
        slots[i] = (ps_, pt)
        self.MM(ps_[:, 0:nq], kfn(kts[i]), q_ap, True, True, rb, ps_.b)
        self.ACT(pt[:, 0:nq], ps_[:, 0:nq], AF.Exp, ps_.b, pt.b, scale=scale)

    def pv(i):
        ps_, pt = slots.pop(i)
        self.MM(pO[0:dv, 0:nq], vfn(kts[i]), pt[:, 0:nq], i == 0, i == n - 1, rb + pt.b, pO.b)
        eng = "dve" if i % 2 == 0 else "pool"
        a_ = acc[i % 2]
        if i < 2:
            self.CP(a_[:, 0:nq], pt[:, 0:nq], pt.b, a_.b, eng=eng)
        else:
            self.TT(a_[:, 0:nq], a_[:, 0:nq], pt[:, 0:nq], ALU.add, a_.b + pt.b, a_.b, eng=eng)

    for i in range(min(LA, n)):
        qk(i)
    if A.get("pending") is not None:
        A["pending"]()
        A["pending"] = None
    for i in range(n):
        if i + LA < n:
            qk(i + LA)
        pv(i)

    def fin():
        self.MM(A["pB"][0:dv, 0:nq], self.onesf[:, 0:dv], acc[0][:, 0:nq], True, False, self.onesf.b + acc[0].b, A["pB"].b)
        self.MM(A["pB"][0:dv, 0:nq], self.onesf[:, 0:dv], acc[1][:, 0:nq], False, True, self.onesf.b + acc[1].b, A["pB"].b)
        self.RCP(osb[0:dv, 0:nq], A["pB"][0:dv, 0:nq], A["pB"].b, osb.b)
        self.TT(out_ap, pO[0:dv, 0:nq], osb[0:dv, 0:nq], ALU.mult, pO.b + osb.b, out_b)
        if after is not None:
            after()
    A["pending"] = fin


def attn_flush(self, A):
    if A.get("pending") is not None:
        A["pending"]()
        A["pending"] = None


Builder.attn_flush = attn_flush
Builder.attn_tiles = attn_tiles
Builder.attn_block = attn_block


def rope_chunk(self, R, pq, nrow, n, qn_fn, cosb, sinb, permb, out_ap, out_b, rs=None):
    qn, pr, t1, t2 = R["qn"][R["i"] % 2], R["pr"], R["t1"][R["i"] % 2], R["t2"][R["i"] % 2]
    R["i"] += 1
    qn_fn(qn)
    self.MM(pr[0:nrow, 0:n], permb[0:nrow, 0:nrow], qn[0:nrow, 0:n], True, True, permb.b + qn.b, pr.b)
    self.TT(t1[0:nrow, 0:n], qn[0:nrow, 0:n], cosb[0:nrow, 0:n], ALU.mult, qn.b + cosb.b, t1.b, eng="pool")
    self.TT(t2[0:nrow, 0:n], pr[0:nrow, 0:n], sinb[0:nrow, 0:n], ALU.mult, pr.b + sinb.b, t2.b)
    if rs is None:
        self.TT(out_ap, t1[0:nrow, 0:n], t2[0:nrow, 0:n], ALU.add, t1.b + t2.b, out_b, eng="pool")
    else:
        self.TT(t1[0:nrow, 0:n], t1[0:nrow, 0:n], t2[0:nrow, 0:n], ALU.add, t1.b + t2.b, t1.b, eng="pool")
        self.TT(out_ap, t1[0:nrow, 0:n], rs[0:nrow, 0:n], ALU.mult, t1.b + rs.b, out_b)


def rope_tiles(self, es, cos_name, sin_name, perm_name, nrow):
    R = {"i": 0}
    R["qn"] = [self.sb(es, "r_qn%d" % j, [128, 512], BF16) for j in range(2)]
    R["t1"] = [self.sb(es, "r_t1%d" % j, [128, 512], F32) for j in range(2)]
    R["t2"] = [self.sb(es, "r_t2%d" % j, [128, 512], F32) for j in range(2)]
    R["pr"] = self.ps(es, "r_pr", [128, 512])
    R["cos"] = [self.sb(es, "r_cos%d" % j, [128, 512], F32) for j in range(2)]
    R["sin"] = [self.sb(es, "r_sin%d" % j, [128, 512], F32) for j in range(2)]
    pf = self.sb(es, "r_permf", [128, 128], F32)
    R["perm"] = self.sb(es, "r_perm", [128, 128], BF16)
    self.DMA("sp", "r_perm", pf[0:nrow, 0:nrow], self.I[perm_name][:, :], w=pf.b)
    self.CP(R["perm"][0:nrow, 0:nrow], pf[0:nrow, 0:nrow], pf.b, R["perm"].b)
    R["names"] = (cos_name, sin_name, nrow)
    return R


def rope_load(self, R, bi, t0, n):
    cn, sn, nrow = R["names"]
    cb, sbb = R["cos"][bi % 2], R["sin"][bi % 2]
    self.DMA("sp", "r_cos%d" % (bi % 2), cb[0:nrow, 0:n], self.I[cn][:, t0:t0 + n], w=cb.b)
    self.DMA("sp", "r_sin%d" % (bi % 2), sbb[0:nrow, 0:n], self.I[sn][:, t0:t0 + n], w=sbb.b)
    return cb, sbb


Builder.rope_chunk = rope_chunk
Builder.rope_tiles = rope_tiles
Builder.rope_load = rope_load


def gqa_p1(self):
    I = self.I
    with ExitStack() as es:
        W = self.work_tiles(es)
        R = self.rope_tiles(es, "cos2", "sin2", "perm2", 128)
        win = self.sb(es, "g_win", [128, KC, 1536], BF16)
        self.load_w(win, I["c_w_in"], "g_win")
        uT = self.sb(es, "g_uT", [128, KC, 512], BF16, n=KC)
        hblk = [self.sb(es, "g_h%d" % j, [128, KC, 512], F32) for j in range(2)]
        pq = [self.ps(es, "g_pq%d" % j, [128, 512]) for j in range(2)]
        pv = self.ps(es, "g_pv", [128, 512])
        rs2 = self.sb(es, "g_rs2", [128, 512], F32)
        qo = [self.sb(es, "g_qo%d" % j, [128, 512], BF16) for j in range(3)]
        vt = [self.sb(es, "g_vt%d" % j, [128, 256], BF16) for j in range(2)]
        for bi, (t0, n) in enumerate(P1_BLOCKS):
            hb = hblk[bi % 2]
            self.load_h(hb, t0, n, "g_h%d" % (bi % 2))
            cb, sbb = self.rope_load(R, bi, t0, n)
            self.norm_mod(hb, t0, n, 0, uT, 0, W)
            for ch in range(10):
                p = pq[ch % 2]
                for k in range(KC):
                    self.MM(p[:, 0:n], win[:, k, ch * 128:(ch + 1) * 128], uT[:, k, 0:n], k == 0, k == KC - 1, win.b + [uT.b[k]], p.b)
                self.sumsq_rstd(lambda k: p[:, 0:n], p.b, 1, n, 128, W, rs2)
                gcol = self.small[:, 6:7] if ch < 8 else self.small[:, 7:8]
                o_ = qo[ch % 3]
                self.rope_chunk(R, p, 128, n, lambda dst: self.TS(dst[:, 0:n], p[:, 0:n], gcol, None, ALU.mult, None, p.b + self.small.b, dst.b),
                                cb, sbb, R["perm"], o_[:, 0:n], o_.b, rs=rs2)
                dst = self.sA[ch * 128:(ch + 1) * 128, t0:t0 + n] if ch < 8 else self.sB[(ch - 8) * 128:(ch - 7) * 128, t0:t0 + n]
                self.DMA("pool", "g_qo%d" % (ch % 3), dst, o_[:, 0:n], r=o_.b)
            for j in range(n // 128):
                for k in range(KC):
                    self.MM(pv[:, 0:256], uT[:, k, j * 128:(j + 1) * 128], win[:, k, 1280:1536], k == 0, k == KC - 1, win.b + [uT.b[k]], pv.b)
                v_ = vt[j % 2]
                self.CP(v_[:], pv[:, 0:256], pv.b, v_.b, eng="act")
                self.DMA("pool", "g_vt%d" % (j % 2), self.sV[t0 + j * 128:t0 + (j + 1) * 128, 0:256], v_[:], r=v_.b)
        self.S.barrier()


def attn_qblocks(do_ctx=True):
    out = []
    if do_ctx:
        out.append((0, 256, [0, 1]))
    for b in range(8):
        out.append((CTX + 512 * b, 512, list(range(NT))))
    return out


def gqa_p2(self):
    with ExitStack() as es:
        A = self.attn_tiles(es)
        KT = self.sb(es, "g2_K", [128, T], BF16)
        V = self.sb(es, "g2_V", [128, NT, 128], BF16)
        QT = [self.sb(es, "g2_Q%d" % j, [128, T], BF16) for j in range(2)]
        ob = [self.sb(es, "g2_o%d" % j, [128, 512], BF16) for j in range(2)]
        scale = 128 ** -0.5
        cnt = 0
        for kvh in range(2):
            self.DMA("sp", "g2_K", KT[:], self.sB[kvh * 128:(kvh + 1) * 128, :], w=KT.b)
            self.DMA("sp", "g2_V", V[:], self.sV.rearrange("(t p) d -> p t d", p=128)[:, :, kvh * 128:(kvh + 1) * 128], w=V.b)
            for g in range(4):
                h = kvh * 4 + g
                Q = QT[h % 2]
                self.DMA("sp", "g2_Q%d" % (h % 2), Q[:], self.sA[h * 128:(h + 1) * 128, :], w=Q.b)
                for (q0, nq, kts) in attn_qblocks(True):
                    o_ = ob[cnt % 2]
                    def after(o_=o_, c=cnt % 2, h=h, q0=q0, nq=nq):
                        self.DMA("sp", "g2_o%d" % c, self.sOT[h * 128:(h + 1) * 128, q0:q0 + nq], o_[:, 0:nq], r=o_.b)
                    self.attn_block(A, lambda kt: KT[:, kt * 128:(kt + 1) * 128], Q[:, q0:q0 + nq], lambda kt: V[:, kt, :], kts, nq, 128, scale,
                                    KT.b + V.b + Q.b, o_[:, 0:nq], o_.b, after=after)
                    cnt += 1
        self.attn_flush(A)
        self.S.barrier()


Builder.gqa_p1 = gqa_p1
Builder.gqa_p2 = gqa_p2


def build(layers=(0, 1, 2, 3), debug=False):
    B = Builder(len(layers), debug)
    I = B.I
    es = ExitStack()
    B.setup(es)
    for i in layers:
        B.mod_phase(i)
        if i == 0:
            B.hgrn_p1()
            B.hgrn_p2()
            B.hgrn_p2b()
            wout = I["a_w_out"]
        elif i == 1:
            B.mla_p1()
            B.mla_p2()
            wout = I["b_w_out"]
        elif i == 2:
            B.gqa_p1()
            B.gqa_p2()
            wout = I["c_w_out"]
        else:
            B.diff_p1()
            B.diff_p2()
            wout = I["d_w_out"]
        B.ffn_phase(i, wout)
    B.final_out()
    B.S.barrier()
    B.S.emit()
    return B.nc


def prep_shared(inp):
    f = lambda a: np.ascontiguousarray(a, dtype=np.float32)
    sh = {}
    sh["c_ctx"] = f(inp["c_ctx"]).reshape(1, D)
    sh["mod_w"] = f(inp["mod_w"])
    sh["mod_b"] = f(inp["mod_b"]).reshape(24, D)
    sh["norm_g"] = f(inp["norm_g"]).reshape(16, D)
    sh["a_w_in"] = f(inp["a_w_in"][0])
    sh["a_lb"] = f(inp["a_lb"]).reshape(10, D)
    sh["a_onorm_g"] = f(inp["a_onorm_g"]).reshape(1, 128)
    sh["a_w_out"] = f(inp["a_w_out"][0])
    sh["b_w_in"] = f(inp["b_w_in"][0])
    sh["b_qnorm_g"] = f(inp["b_qnorm_g"]).reshape(3, 128)
    sh["b_kvnorm_g"] = f(inp["b_kvnorm_g"]).reshape(2, 128)
    sh["b_w_qb"] = f(inp["b_w_qb"][0])
    kvb = f(inp["b_w_kvb"][0]).reshape(256, 16, 2, 64)
    sh["b_w_kvb"] = np.ascontiguousarray(kvb.transpose(0, 2, 1, 3).reshape(256, 2048))
    sh["b_w_out"] = f(inp["b_w_out"][0])
    sh["c_w_in"] = f(inp["c_w_in"][0])
    sh["c_qnorm_g"] = f(inp["c_qnorm_g"]).reshape(1, 128)
    sh["c_knorm_g"] = f(inp["c_knorm_g"]).reshape(1, 128)
    sh["c_w_out"] = f(inp["c_w_out"][0])
    sh["d_w_in"] = f(inp["d_w_in"][0])
    sh["d_lambda"] = f(inp["d_lambda"]).reshape(1, 256)
    sh["d_onorm_g"] = f(inp["d_onorm_g"]).reshape(1, 128)
    sh["d_w_out"] = f(inp["d_w_out"][0])
    g = f(inp["ffn_w_gu"]).reshape(2, KC, 128, 2, FC, 128)
    sh["ffn_w_gu"] = np.ascontiguousarray(g.transpose(0, 4, 2, 1, 3, 5).reshape(2, FC, 128, KC * 256))
    d_ = f(inp["ffn_w_down"]).reshape(2, FC, 128, KC, 128)
    sh["ffn_w_down"] = np.ascontiguousarray(d_.transpose(0, 3, 2, 1, 4).reshape(2, KC, 128, FF))
    sh["moe_router"] = f(inp["moe_router"])
    g = f(inp["moe_w_gu"]).reshape(2, NE, KC, 128, 2, FC, 128)
    sh["moe_w_gu"] = np.ascontiguousarray(g.transpose(0, 1, 5, 3, 2, 4, 6).reshape(2, NE, FC, 128, KC * 256))
    d_ = f(inp["moe_w_down"]).reshape(2, NE, FC, 128, KC, 128)
    sh["moe_w_down"] = np.ascontiguousarray(d_.transpose(0, 1, 4, 3, 2, 5).reshape(2, NE, KC, 128, FF))
    sh.update(_consts())
    return sh


def kernel(**inp):
    nc = build()
    sh = prep_shared(inp)
    in_maps = []
    for b in range(8):
        m = dict(sh)
        m["x"] = np.ascontiguousarray(inp["x"][b], dtype=np.float32)
        m["c"] = np.ascontiguousarray(inp["c"][b:b + 1], dtype=np.float32)
        m["ctx"] = np.ascontiguousarray(inp["ctx"][b], dtype=np.float32)
        in_maps.append(m)
    res = run_bass_kernel_spmd(nc, in_maps, core_ids=list(range(8)))
    return np.stack([np.asarray(r["out"], dtype=np.float32) for r in res.results], axis=0)


def diff_p1(self):
    I = self.I
    with ExitStack() as es:
        W = self.work_tiles(es)
        R = self.rope_tiles(es, "cos3", "sin3", "perm3", 128)
        win = self.sb(es, "d_win", [128, KC, 3072], BF16)
        self.load_w(win, I["d_w_in"], "d_win")
        uT = self.sb(es, "d_uT", [128, KC, 512], BF16, n=KC)
        hblk = [self.sb(es, "d_h%d" % j, [128, KC, 512], F32) for j in range(2)]
        pq = [self.ps(es, "d_pq%d" % j, [128, 512]) for j in range(2)]
        pv = [self.ps(es, "d_pv%d" % j, [128, 512]) for j in range(2)]
        qo = [self.sb(es, "d_qo%d" % j, [128, 512], BF16) for j in range(3)]
        vt = [self.sb(es, "d_vt%d" % j, [128, 1024], BF16) for j in range(2)]
        for bi, (t0, n) in enumerate(P1_BLOCKS):
            hb = hblk[bi % 2]
            self.load_h(hb, t0, n, "d_h%d" % (bi % 2))
            cb, sbb = self.rope_load(R, bi, t0, n)
            self.norm_mod(hb, t0, n, 0, uT, 0, W)
            for ch in range(16):
                p = pq[ch % 2]
                for k in range(KC):
                    self.MM(p[:, 0:n], win[:, k, ch * 128:(ch + 1) * 128], uT[:, k, 0:n], k == 0, k == KC - 1, win.b + [uT.b[k]], p.b)
                o_ = qo[ch % 3]
                self.rope_chunk(R, p, 128, n, lambda dst: self.CP(dst[:, 0:n], p[:, 0:n], p.b, dst.b, eng="act"),
                                cb, sbb, R["perm"], o_[:, 0:n], o_.b)
                dst = self.sA[ch * 128:(ch + 1) * 128, t0:t0 + n] if ch < 8 else self.sB[(ch - 8) * 128:(ch - 7) * 128, t0:t0 + n]
                self.DMA("pool", "d_qo%d" % (ch % 3), dst, o_[:, 0:n], r=o_.b)
            for j in range(n // 128):
                v_ = vt[j % 2]
                for hh in range(2):
                    p = pv[hh]
                    for k in range(KC):
                        self.MM(p[:, :], uT[:, k, j * 128:(j + 1) * 128], win[:, k, 2048 + hh * 512:2048 + (hh + 1) * 512], k == 0, k == KC - 1,
                                win.b + [uT.b[k]], p.b)
                    self.CP(v_[:, hh * 512:(hh + 1) * 512], p[:, :], p.b, v_.b, eng=("act" if hh else "dve"))
                self.DMA("pool", "d_vt%d" % (j % 2), self.sV[t0 + j * 128:t0 + (j + 1) * 128, :], v_[:], r=v_.b)
        self.S.barrier()


def diff_p2(self, i=3):
    I = self.I
    lam_init = 0.8 - 0.6 * math.exp(-0.3 * i)
    with ExitStack() as es:
        A = self.attn_tiles(es, 4)
        W = self.work_tiles(es)
        KT = [self.sb(es, "d2_K%d" % j, [128, T], BF16) for j in range(2)]
        V = [self.sb(es, "d2_V%d" % j, [128, NT, 128], BF16) for j in range(2)]
        QT = [self.sb(es, "d2_Q%d" % j, [128, T], BF16) for j in range(2)]
        ams = [[self.sb(es, "d2_a%d%d" % (i, j), [128, 512], F32) for j in range(2)] for i in range(2)]
        od = self.sb(es, "d2_od", [128, 512], F32)
        ob = [self.sb(es, "d2_o%d" % j, [128, 512], BF16) for j in range(2)]
        lt = self.sb(es, "d2_lam", [128, 256], F32)
        lc = self.sb(es, "d2_lc", [128, 8], F32)
        self.DMA("sp", "d2_lam", lt[:], I["d_lambda"][0:1, :].to_broadcast([128, 256]), w=lt.b)
        for q in range(2):
            self.TT(lt[:, q * 128:q * 128 + 64], lt[:, q * 128:q * 128 + 64], lt[:, q * 128 + 64:q * 128 + 128], ALU.mult, lt.b, lt.b)
            self.S.op("dve", lambda e, q=q: e.reduce_sum(out=lc[:, q:q + 1], in_=lt[:, q * 128:q * 128 + 64], axis=mybir.AxisListType.X), lt.b, lc.b)
        self.ACT(lc[:, 2:4], lc[:, 0:2], AF.Exp, lc.b, lc.b)
        self.TT(lc[:, 4:5], lc[:, 3:4], lc[:, 2:3], ALU.subtract, lc.b, lc.b)
        self.TS(lc[:, 5:6], lc[:, 4:5], -lam_init, None, ALU.add, None, lc.b, lc.b)
        self.TS(lc[:, 6:7], self.small[:, 8:9], 1.0 - lam_init, None, ALU.mult, None, lc.b + self.small.b, lc.b)
        scale = 64 ** -0.5
        cnt = 0
        for h in range(8):
            K_, V_, Q_ = KT[h % 2], V[h % 2], QT[h % 2]
            self.DMA("sp", "d2_K%d" % (h % 2), K_[:], self.sB[h * 128:(h + 1) * 128, :], w=K_.b)
            self.DMA("sp", "d2_V%d" % (h % 2), V_[:], self.sV.rearrange("(t p) d -> p t d", p=128)[:, :, h * 128:(h + 1) * 128], w=V_.b)
            self.DMA("sp", "d2_Q%d" % (h % 2), Q_[:], self.sA[h * 128:(h + 1) * 128, :], w=Q_.b)
            for (q0, nq, kts) in attn_qblocks(False):
                am = ams[cnt % 2]
                o_ = ob[cnt % 2]

                def combine(am=am, o_=o_, c=cnt % 2, h=h, q0=q0, nq=nq):
                    self.STT(od[:, 0:nq], am[1][:, 0:nq], lc[:, 5:6], am[0][:, 0:nq], ALU.mult, ALU.add, am[0].b + am[1].b + lc.b, od.b)
                    self.sumsq_rstd(lambda k: od[:, 0:nq], od.b, 1, nq, 128, W, W["rs"])
                    self.STT(o_[:, 0:nq], od[:, 0:nq], lc[:, 6:7], W["rs"][:, 0:nq], ALU.mult, ALU.mult, od.b + lc.b + W["rs"].b, o_.b)
                    self.DMA("sp", "d2_o%d" % c, self.sOT[h * 128:(h + 1) * 128, q0:q0 + nq], o_[:, 0:nq], r=o_.b)
                for m in range(2):
                    lo, hi = 64 * m, 64 * m + 64
                    self.attn_block(A, lambda kt, K_=K_, lo=lo, hi=hi: K_[lo:hi, kt * 128:(kt + 1) * 128], Q_[lo:hi, q0:q0 + nq],
                                    lambda kt, V_=V_: V_[:, kt, :], kts, nq, 128, scale,
                                    K_.b + V_.b + Q_.b, am[m][:, 0:nq], am[m].b, after=(combine if m == 1 else None))
                cnt += 1
        self.attn_flush(A)
        self.S.barrier()


Builder.diff_p1 = diff_p1
Builder.diff_p2 = diff_p2


def mla_p1(self):
    I = self.I
    with ExitStack() as es:
        W = self.work_tiles(es)
        R = self.rope_tiles(es, "cos1", "sin1", "perm1", 96)
        ck = self.sb(es, "m_cosk", [32, T], F32)
        sk = self.sb(es, "m_sink", [32, T], F32)
        pkf = self.sb(es, "m_permkf", [32, 32], F32)
        pkb = self.sb(es, "m_permk", [32, 32], BF16)
        self.DMA("sp", "m_ck", ck[:], I["cos1k"][:, :], w=ck.b)
        self.DMA("sp", "m_sk", sk[:], I["sin1k"][:, :], w=sk.b)
        self.DMA("sp", "m_pk", pkf[:], I["perm1k"][:, :], w=pkf.b)
        self.CP(pkb[:], pkf[:], pkf.b, pkb.b)
        win = self.sb(es, "m_win", [128, KC, 768], BF16)
        wqb = self.sb(es, "m_wqb", [128, 3, 1536], BF16)
        wkvb = self.sb(es, "m_wkvb", [128, 2, 2048], BF16)
        for k in range(KC):
            self.DMA("pool", "m_win", win[:, k, 0:672], I["b_w_in"].rearrange("(k p) n -> p k n", p=128)[:, k, :], w=win.b)
        self.load_w(wqb, I["b_w_qb"], "m_wqb")
        self.load_w(wkvb, I["b_w_kvb"], "m_wkvb")
        for k in range(3 if 'scale' in MLA_PARTS else 0):
            self.TS(wqb[:, k, :], wqb[:, k, :], self.small[:, 1 + k:2 + k], None, ALU.mult, None, wqb.b + self.small.b, wqb.b)
        for k in range(2 if 'scale' in MLA_PARTS else 0):
            self.TS(wkvb[:, k, :], wkvb[:, k, :], self.small[:, 4 + k:5 + k], None, ALU.mult, None, wkvb.b + self.small.b, wkvb.b)
        uT = self.sb(es, "m_uT", [128, KC, 512], BF16, n=KC)
        hblk = [self.sb(es, "m_h%d" % j, [128, KC, 512], F32) for j in range(2)]
        pq = [self.ps(es, "m_pq%d" % j, [128, 512]) for j in range(2)]
        pv = [self.ps(es, "m_pv%d" % j, [128, 512]) for j in range(2)]
        pss2 = self.ps(es, "m_pss2", [128, 512])
        cqT = self.sb(es, "m_cqT", [128, 3, 512], BF16)
        ckvT = self.sb(es, "m_ckvT", [128, 2, 512], BF16)
        sqq = [self.sb(es, "m_sqq%d" % j, [128, 512], BF16) for j in range(3)]
        sqkv = [self.sb(es, "m_sqkv%d" % j, [128, 512], BF16) for j in range(2)]
        rsq = self.sb(es, "m_rsq", [128, 512], F32)
        rskv = self.sb(es, "m_rskv", [128, 512], F32)
        rcol = self.sb(es, "m_rcol", [128, 4], F32)
        qo = [self.sb(es, "m_qo%d" % j, [128, 512], BF16) for j in range(3)]
        vt = [self.sb(es, "m_vt%d" % j, [128, 1024], BF16) for j in range(2)]
        for bi, (t0, n) in enumerate(P1_BLOCKS):
            hb = hblk[bi % 2]
            self.load_h(hb, t0, n, "m_h%d" % (bi % 2))
            cb, sbb = self.rope_load(R, bi, t0, n)
            self.norm_mod(hb, t0, n, 0, uT, 0, W)
            if 'lat' not in MLA_PARTS:
                continue
            for ch in range(5):
                p = pq[ch % 2]
                for k in range(KC):
                    self.MM(p[:, 0:n], win[:, k, ch * 128:(ch + 1) * 128], uT[:, k, 0:n], k == 0, k == KC - 1, win.b + [uT.b[k]], p.b)
                if 'nosq' in MLA_PARTS:
                    continue
                if ch < 3:
                    sq = sqq[ch]
                    self.ACT(sq[:, 0:n], p[:, 0:n], AF.Square, p.b, sq.b)
                    self.CP(cqT[:, ch, 0:n], p[:, 0:n], p.b, cqT.b)
                else:
                    sq = sqkv[ch - 3]
                    self.ACT(sq[:, 0:n], p[:, 0:n], AF.Square, p.b, sq.b)
                    self.CP(ckvT[:, ch - 3, 0:n], p[:, 0:n], p.b, ckvT.b)
            if 'nosq' in MLA_PARTS or 'noones' in MLA_PARTS:
                continue
            for ch in range(3):
                self.MM(W["pss"][:, 0:n], self.onesb[:], sqq[ch][:, 0:n], ch == 0, ch == 2, self.onesb.b + sqq[ch].b, W["pss"].b)
            for ch in range(2):
                self.MM(pss2[:, 0:n], self.onesb[:], sqkv[ch][:, 0:n], ch == 0, ch == 1, self.onesb.b + sqkv[ch].b, pss2.b)
            self.rstd(rsq[:, 0:n], W["pss"][:, 0:n], 384, W["pss"].b + self.epsb, rsq.b, W["rt"][:, 0:n], W["rt"].b)
            self.rstd(rskv[:, 0:n], pss2[:, 0:n], 256, pss2.b + self.epsb, rskv.b, W["rt"][:, 0:n], W["rt"].b)
            p = pq[1]
            if 'kr' not in MLA_PARTS:
                continue
            for k in range(KC):
                self.MM(p[0:32, 0:n], win[:, k, 640:672], uT[:, k, 0:n], k == 0, k == KC - 1, win.b + [uT.b[k]], p.b)
            o_ = qo[0]
            self.rope_chunk(R, p, 32, n, lambda dst: self.CP(dst[0:32, 0:n], p[0:32, 0:n], p.b, dst.b, eng="act"),
                            self.view(ck, ck.t[:, t0:t0 + n]), self.view(sk, sk.t[:, t0:t0 + n]), pkb, o_[0:32, 0:n], o_.b)
            self.DMA("pool", "m_qo0", self.sKR[:, t0:t0 + n], o_[0:32, 0:n], r=o_.b)
            for h in range(16 if 'q' in MLA_PARTS else 0):
                p = pq[h % 2]
                for k in range(3):
                    self.MM(p[0:96, 0:n], wqb[:, k, h * 96:(h + 1) * 96], cqT[:, k, 0:n], k == 0, k == 2, wqb.b + cqT.b, p.b)
                o_ = qo[(h + 1) % 3]
                self.rope_chunk(R, p, 96, n, lambda dst: self.TT(dst[0:96, 0:n], p[0:96, 0:n], rsq[0:96, 0:n], ALU.mult, p.b + rsq.b, dst.b),
                                cb, sbb, R["perm"], o_[0:96, 0:n], o_.b)
                self.DMA("pool", "m_qo%d" % ((h + 1) % 3), self.sA[h * 96:(h + 1) * 96, t0:t0 + n], o_[0:96, 0:n], r=o_.b)
            for j in range(8 if 'kn' in MLA_PARTS else 0):
                p = pq[j % 2]
                for k in range(2):
                    self.MM(p[:, 0:n], wkvb[:, k, j * 128:(j + 1) * 128], ckvT[:, k, 0:n], k == 0, k == 1, wkvb.b + ckvT.b, p.b)
                o_ = qo[j % 3]
                self.TT(o_[:, 0:n], p[:, 0:n], rskv[:, 0:n], ALU.mult, p.b + rskv.b, o_.b)
                self.DMA("pool", "m_qo%d" % (j % 3), self.sB[j * 128:(j + 1) * 128, t0:t0 + n], o_[:, 0:n], r=o_.b)
            for j in range(n // 128 if 'v' in MLA_PARTS else 0):
                v_ = vt[j % 2]
                for k in range(2):
                    self.MM(pss2[:, 0:8], sqkv[k][:, j * 128:(j + 1) * 128], self.onesb[:, 0:8], k == 0, k == 1, sqkv[k].b + self.onesb.b, pss2.b)
                self.ACT(rcol[:, 0:1], pss2[:, 0:1], AF.Sqrt, pss2.b + self.epsb, rcol.b, bias=self.epsc[:, :], scale=1.0 / 256)
                self.RCP(rcol[:, 1:2], rcol[:, 0:1], rcol.b, rcol.b)
                for hh in range(2):
                    p = pv[hh]
                    for k in range(2):
                        self.MM(p[:, :], ckvT[:, k, j * 128:(j + 1) * 128], wkvb[:, k, 1024 + hh * 512:1024 + (hh + 1) * 512], k == 0, k == 1,
                                wkvb.b + ckvT.b, p.b)
                    self.TS(v_[:, hh * 512:(hh + 1) * 512], p[:, :], rcol[:, 1:2], None, ALU.mult, None, p.b + rcol.b, v_.b)
                self.DMA("pool", "m_vt%d" % (j % 2), self.sV[t0 + j * 128:t0 + (j + 1) * 128, :], v_[:], r=v_.b)
        self.S.barrier()


def mla_p2(self):
    with ExitStack() as es:
        A = self.attn_tiles(es)
        KT = [self.sb(es, "m2_K%d" % j, [96, T], BF16) for j in range(2)]
        V = [self.sb(es, "m2_V%d" % j, [128, NT, 64], BF16) for j in range(2)]
        QT = [self.sb(es, "m2_Q%d" % j, [96, T], BF16) for j in range(2)]
        ob = [self.sb(es, "m2_o%d" % j, [64, 512], BF16) for j in range(2)]
        scale = 96 ** -0.5
        cnt = 0
        for h in range(16):
            K_, V_, Q_ = KT[h % 2], V[h % 2], QT[h % 2]
            self.DMA("sp", "m2_K%d" % (h % 2), K_[0:64, :], self.sB[h * 64:(h + 1) * 64, :], w=K_.b)
            self.DMA("sp", "m2_K%d" % (h % 2), K_[64:96, :], self.sKR[:, :], w=K_.b)
            self.DMA("sp", "m2_V%d" % (h % 2), V_[:], self.sV.rearrange("(t p) d -> p t d", p=128)[:, :, h * 64:(h + 1) * 64], w=V_.b)
            self.DMA("sp", "m2_Q%d" % (h % 2), Q_[:], self.sA[h * 96:(h + 1) * 96, :], w=Q_.b)
            for (q0, nq, kts) in attn_qblocks(True):
                o_ = ob[cnt % 2]
                def after(o_=o_, c=cnt % 2, h=h, q0=q0, nq=nq):
                    self.DMA("sp", "m2_o%d" % c, self.sOT[h * 64:(h + 1) * 64, q0:q0 + nq], o_[:, 0:nq], r=o_.b)
                self.attn_block(A, lambda kt, K_=K_: K_[:, kt * 128:(kt + 1) * 128], Q_[:, q0:q0 + nq], lambda kt, V_=V_: V_[:, kt, :], kts, nq, 64, scale,
                                K_.b + V_.b + Q_.b, o_[:, 0:nq], o_.b, after=after)
                cnt += 1
        self.attn_flush(A)
        self.S.barrier()


Builder.mla_p1 = mla_p1
Builder.mla_p2 = mla_p2


def hgrn_p1(self):
    I = self.I
    with ExitStack() as es:
        W = self.work_tiles(es)
        win = self.sb(es, "h_win", [128, KC, 5120], BF16)
        self.load_w(win, I["a_w_in"], "h_win")
        uT = self.sb(es, "h_uT", [128, KC, 512], BF16, n=KC)
        hblk = [self.sb(es, "h_h%d" % j, [128, KC, 512], F32) for j in range(2)]
        pq = [self.ps(es, "h_pq%d" % j, [128, 512]) for j in range(3)]
        pv = [self.ps(es, "h_pv%d" % j, [128, 512]) for j in range(2)]
        ob = [self.sb(es, "h_ob%d" % j, [128, 512], BF16) for j in range(3)]
        sg = [self.sb(es, "h_sg%d" % j, [128, 512], F32) for j in range(2)]
        ff = [self.sb(es, "h_ff%d" % j, [128, 512], F32) for j in range(2)]
        lf = [self.sb(es, "h_lf%d" % j, [128, 512], F32) for j in range(2)]
        kk = [self.sb(es, "h_kk%d" % j, [128, 512], BF16) for j in range(2)]
        vt = [self.sb(es, "h_vt%d" % j, [128, 1024], BF16) for j in range(2)]
        c = 0
        for bi, (t0, n) in enumerate(P1_BLOCKS):
            hb = hblk[bi % 2]
            self.load_h(hb, t0, n, "h_h%d" % (bi % 2))
            self.norm_mod(hb, t0, n, 0, uT, 0, W)
            for ch in range(32):
                p = pq[c % 3]
                for k in range(KC):
                    self.MM(p[:, 0:n], win[:, k, ch * 128:(ch + 1) * 128] if ch < 24 else win[:, k, 4096 + (ch - 24) * 128:4096 + (ch - 23) * 128],
                            uT[:, k, 0:n], k == 0, k == KC - 1, win.b + [uT.b[k]], p.b)
                if ch < 8:
                    o_ = ob[c % 3]
                    self.TS(o_[:, 0:n], p[:, 0:n], 128 ** -0.5, None, ALU.mult, None, p.b, o_.b)
                    self.DMA("pool", "h_ob%d" % (c % 3), self.sA[ch * 128:(ch + 1) * 128, t0:t0 + n], o_[:, 0:n], r=o_.b)
                elif ch < 24:
                    d, hh = (ch - 8) // 8, (ch - 8) % 8
                    s_, f_, l_, k_ = sg[c % 2], ff[c % 2], lf[c % 2], kk[c % 2]
                    self.ACT(s_[:, 0:n], p[:, 0:n], AF.Sigmoid, p.b, s_.b)
                    self.TS(f_[:, 0:n], s_[:, 0:n], self.lbc[:, hh, d, 1:2], self.lbc[:, hh, d, 0:1], ALU.mult, ALU.add, s_.b + self.lbc.b, f_.b)
                    self.ACT(l_[:, 0:n], f_[:, 0:n], AF.Ln, f_.b, l_.b)
                    self.TS(k_[:, 0:n], f_[:, 0:n], -1.0, 1.0, ALU.mult, ALU.add, f_.b, k_.b)
                    self.DMA("pool", "h_lf%d" % (c % 2), (self.sF1 if d == 0 else self.sF2)[hh * 128:(hh + 1) * 128, t0:t0 + n], l_[:, 0:n], r=l_.b)
                    self.DMA("pool", "h_kk%d" % (c % 2), (self.sB if d == 0 else self.sC)[hh * 128:(hh + 1) * 128, t0:t0 + n], k_[:, 0:n], r=k_.b)
                else:
                    hh = ch - 24
                    o_ = ob[c % 3]
                    self.ACT(o_[:, 0:n], p[:, 0:n], AF.Silu, p.b, o_.b)
                    self.DMA("pool", "h_ob%d" % (c % 3), self.sG[hh * 128:(hh + 1) * 128, t0:t0 + n], o_[:, 0:n], r=o_.b)
                c += 1
            for j in range(n // 128):
                v_ = vt[j % 2]
                for hh in range(2):
                    p = pv[hh]
                    for k in range(KC):
                        self.MM(p[:, :], uT[:, k, j * 128:(j + 1) * 128], win[:, k, 3072 + hh * 512:3072 + (hh + 1) * 512], k == 0, k == KC - 1,
                                win.b + [uT.b[k]], p.b)
                    self.CP(v_[:, hh * 512:(hh + 1) * 512], p[:, :], p.b, v_.b, eng=("act" if hh else "dve"))
                self.DMA("pool", "h_vt%d" % (j % 2), self.sV[t0 + j * 128:t0 + (j + 1) * 128, :], v_[:], r=v_.b)
        self.S.barrier()


def hgrn_p2(self):
    I = self.I
    NU = 16
    with ExitStack() as es:
        mk = [self.sb(es, "h2_mask%d" % d, [128, 128], F32) for d in range(2)]
        self.DMA("sp", "h2_m0", mk[0][:], I["maskf"][:, :], w=mk[0].b)
        self.DMA("sp", "h2_m1", mk[1][:], I["maskb"][:, :], w=mk[1].b)
        St = self.sb(es, "h2_S", [128, NU, 128], F32, n=NU)
        self.MEMSET(St[:], 0.0, St.b)
        onesr = self.sb(es, "h2_ones", [128, 128], F32)
        self.MEMSET(onesr[:], 1.0, onesr.b)
        f32t = {nm: self.sb(es, "h2_" + nm, [128, NU, 128], F32, n=NU) for nm in ("cpre", "base", "e1", "e2", "e3")}
        bft = {nm: self.sb(es, "h2_" + nm, [128, NU, 128], BF16, n=NU) for nm in ("qd", "qi", "attm", "ketm", "Sbf")}
        ekt = [self.sb(es, "h2_ek%d" % j, [128, NU, 128], F32, n=NU) for j in range(4)]
        kdt = [self.sb(es, "h2_kd%d" % j, [128, NU, 128], BF16, n=NU) for j in range(4)]
        for j in range(4):
            self.MEMSET(kdt[j][:], 0.0, kdt[j].b)
        bft["ke"] = self.sb(es, "h2_ke", [128, NU, 128], F32, n=NU)
        cols = self.sb(es, "h2_cols", [128, NU, 16], F32, n=NU)
        self.MEMSET(cols[:], 0.0, cols.b)
        ld = {}
        for d in range(2):
            for j in range(2):
                ld[(d, j)] = dict(q=self.sb(es, "h2_q%d%d" % (d, j), [128, 8, 128], BF16), k=self.sb(es, "h2_k%d%d" % (d, j), [128, 8, 128], BF16),
                                  lf=self.sb(es, "h2_lf%d%d" % (d, j), [128, 8, 128], F32), v=self.sb(es, "h2_v%d%d" % (d, j), [128, 1024], BF16),
                                  o=self.sb(es, "h2_o%d%d" % (d, j), [128, 8, 128], F32, n=8))
        pA = [self.ps(es, "h2_pA%d" % j, [128, 4, 128], F32) for j in range(2)]
        pO = [self.ps(es, "h2_pO%d" % j, [128, 4, 128], F32) for j in range(2)]
        pS = [self.ps(es, "h2_pS%d" % j, [128, 4, 128], F32) for j in range(2)]
        pT = [self.ps(es, "h2_pT%d" % j, [128, 4, 128], F32) for j in range(2)]
        order = [list(range(NT)), [1, 0] + list(range(NT - 1, 1, -1))]
        srcK = [self.sB, self.sC]
        srcL = [self.sF1, self.sF2]
        dstO = [self.sF3, self.sF4]
        for step in range(min(NT, HG_STEPS)):
            L = []
            for d in range(2):
                tl = order[d][step]
                c0 = tl * 128
                b = ld[(d, step % 2)]
                key = "h2_%d%d" % (d, step % 2)
                self.DMA("sp", key + "q", b["q"][:], self.sA[0:1024, :].rearrange("(h p) t -> p h t", p=128)[:, :, c0:c0 + 128], w=b["q"].b)
                self.DMA("sp", key + "k", b["k"][:], srcK[d].rearrange("(h p) t -> p h t", p=128)[:, :, c0:c0 + 128], w=b["k"].b)
                self.DMA("sp", key + "l", b["lf"][:], srcL[d].rearrange("(h p) t -> p h t", p=128)[:, :, c0:c0 + 128], w=b["lf"].b)
                self.DMA("sp", key + "v", b["v"][:], self.sV[c0:c0 + 128, :], w=b["v"].b)
                L.append(b)
            units = [(d, h) for d in range(2) for h in range(8)]
            for u, (d, h) in enumerate(units):
                if HG_STOP < 1:
                    break
                b = L[d]
                cp, ba, e1, e2, e3 = (f32t[nm] for nm in ("cpre", "base", "e1", "e2", "e3"))
                cl = cols[:, u, :]
                cb_ = [cols.b[u]]
                self.S.op("dve", lambda e, o=cp[:, u, :], x=b["lf"][:, h, :]: e.tensor_tensor_scan(out=o, data0=onesr[:], data1=x, initial=0.0,
                                                                                            op0=ALU.mult, op1=ALU.add),
                          b["lf"].b + onesr.b, [cp.b[u]])
                tot = cp[:, u, 127:128]
                if d == 0:
                    base, baseb = cp[:, u, :], [cp.b[u]]
                    self.CP(cl[:, 1:4], cp[:, u, 31:96:32], [cp.b[u]], cb_)
                    self.TS(cl[:, 5:8], cp[:, u, 31:96:32], -1.0, None, ALU.mult, None, [cp.b[u]], cb_)
                else:
                    self.TT(ba[:, u, :], b["lf"][:, h, :], cp[:, u, :], ALU.subtract, b["lf"].b + [cp.b[u]], [ba.b[u]])
                    base, baseb = ba[:, u, :], [ba.b[u]]
                    self.CP(cl[:, 0:3], ba[:, u, 32:128:32], baseb, cb_)
                    self.TS(cl[:, 4:7], ba[:, u, 32:128:32], -1.0, None, ALU.mult, None, baseb, cb_)
                    self.TS(cl[:, 3:4], tot, -1.0, None, ALU.mult, None, [cp.b[u]], cb_)
                    self.CP(cl[:, 7:8], tot, [cp.b[u]], cb_)
                rb_ = baseb + cb_ + [cp.b[u]]
                for j in range(4):
                    sl = slice(32 * j, 32 * j + 32)
                    rng = slice(0, 32 * j + 32) if d == 0 else slice(32 * j, 128)
                    if d == 0 and j == 0:
                        self.ACT(e1[:, u, sl], base[:, sl], AF.Exp, rb_, [e1.b[u]])
                        self.ACT(ekt[j][:, u, rng], base[:, rng], AF.Exp, rb_, [ekt[j].b[u]], scale=-1.0)
                    else:
                        self.ACT(e1[:, u, sl], base[:, sl], AF.Exp, rb_, [e1.b[u]], bias=cl[:, 4 + j:5 + j], scale=1.0)
                        self.ACT(ekt[j][:, u, rng], base[:, rng], AF.Exp, rb_, [ekt[j].b[u]], bias=cl[:, j:j + 1], scale=-1.0)
                    self.TT(kdt[j][:, u, rng], b["k"][:, h, rng], ekt[j][:, u, rng], ALU.mult, b["k"].b + [ekt[j].b[u]], [kdt[j].b[u]], eng="pool")
                if d == 0:
                    self.ACT(e2[:, u, :], base, AF.Exp, rb_, [e2.b[u]])
                    self.ACT(e3[:, u, :], base, AF.Exp, rb_, [e3.b[u]], bias=tot, scale=-1.0)
                else:
                    self.ACT(e2[:, u, :], base, AF.Exp, rb_, [e2.b[u]], bias=tot, scale=1.0)
                    self.ACT(e3[:, u, :], base, AF.Exp, rb_, [e3.b[u]], scale=-1.0)
                self.ACT(cl[:, 8:9], tot, AF.Exp, [cp.b[u]], cb_)
                self.TT(bft["qd"][:, u, :], b["q"][:, h, :], e1[:, u, :], ALU.mult, b["q"].b + [e1.b[u]], [bft["qd"].b[u]])
                self.TT(bft["qi"][:, u, :], b["q"][:, h, :], e2[:, u, :], ALU.mult, b["q"].b + [e2.b[u]], [bft["qi"].b[u]])
                self.TT(bft["ke"][:, u, :], b["k"][:, h, :], e3[:, u, :], ALU.mult, b["k"].b + [e3.b[u]], [bft["ke"].b[u]], eng="pool")
                self.CP(bft["Sbf"][:, u, :], St[:, u, :], [St.b[u]], [bft["Sbf"].b[u]])
            for g0 in range(0, NU, 4):
                if HG_STOP < 2:
                    break
                pa, pt_ = pA[(g0 // 4) % 2], pT[(g0 // 4) % 2]
                for u in range(g0, g0 + 4):
                    for j in range(4):
                        self.MM(pa[:, u % 4, 32 * j:32 * j + 32], kdt[j][:, u, :], bft["qd"][:, u, 32 * j:32 * j + 32], True, True,
                                [kdt[j].b[u], bft["qd"].b[u]], pa.b)
                    self.TR(pt_[:, u % 4, :], bft["ke"][:, u, :], self.ident[:], [bft["ke"].b[u]] + self.ident.b, pt_.b)
                for u in range(g0, g0 + 4):
                    d = units[u][0]
                    self.TT(bft["attm"][:, u, :], pa[:, u % 4, :], mk[d][:], ALU.mult, pa.b + mk[d].b, [bft["attm"].b[u]])
                    self.CP(bft["ketm"][:, u, :], pt_[:, u % 4, :], pt_.b, [bft["ketm"].b[u]], eng="act")
            for g0 in range(0, NU, 4):
                if HG_STOP < 3:
                    break
                po, ps_ = pO[(g0 // 4) % 2], pS[(g0 // 4) % 2]
                for u in range(g0, g0 + 4):
                    d, h = units[u]
                    b = L[d]
                    vh = b["v"][:, h * 128:(h + 1) * 128]
                    self.MM(po[:, u % 4, :], vh, bft["attm"][:, u, :], True, False, b["v"].b + [bft["attm"].b[u]], po.b)
                    self.MM(po[:, u % 4, :], bft["Sbf"][:, u, :], bft["qi"][:, u, :], False, True, [bft["Sbf"].b[u], bft["qi"].b[u]], po.b)
                    self.MM(ps_[:, u % 4, :], bft["ketm"][:, u, :], vh, True, True, b["v"].b + [bft["ketm"].b[u]], ps_.b)
                for u in range(g0, g0 + 4):
                    d, h = units[u]
                    b = L[d]
                    self.CP(b["o"][:, h, :], po[:, u % 4, :], po.b, [b["o"].b[h]], eng="act")
                    self.STT(St[:, u, :], St[:, u, :], cols[:, u, 8:9], ps_[:, u % 4, :], ALU.mult, ALU.add, [St.b[u], cols.b[u]] + ps_.b, [St.b[u]])
            for d in range(2):
                c0 = order[d][step] * 128
                self.DMA("pool", "h2_o%d%d" % (d, step % 2), dstO[d].rearrange("(h p) t -> p h t", p=128)[:, :, c0:c0 + 128], L[d]["o"][:], r=L[d]["o"].b)
        self.S.barrier()


def hgrn_p2b(self):
    with ExitStack() as es:
        W = self.work_tiles(es)
        of = [self.sb(es, "h3_of%d" % j, [128, 8, 512], F32) for j in range(2)]
        obk = [self.sb(es, "h3_ob%d" % j, [128, 8, 512], F32) for j in range(2)]
        gt = [self.sb(es, "h3_g%d" % j, [128, 8, 512], BF16) for j in range(2)]
        ot = [self.sb(es, "h3_ot%d" % j, [128, 8, 512], BF16) for j in range(2)]
        od = [self.sb(es, "h3_od%d" % j, [128, 512], F32) for j in range(2)]
        rsh = self.sb(es, "h3_rs", [128, 512], F32)
        for bi, (t0, n) in enumerate(P1_BLOCKS):
            j = bi % 2
            v = lambda a: a.rearrange("(h p) t -> p h t", p=128)[:, :, t0:t0 + n]
            self.DMA("sp", "h3_of%d" % j, of[j][:, :, 0:n], v(self.sF3), w=of[j].b)
            self.DMA("sp", "h3_ob%d" % j, obk[j][:, :, 0:n], v(self.sF4), w=obk[j].b)
            self.DMA("sp", "h3_g%d" % j, gt[j][:, :, 0:n], v(self.sG), w=gt[j].b)
            for h in range(8):
                o_ = od[h % 2]
                self.TT(o_[:, 0:n], of[j][:, h, 0:n], obk[j][:, h, 0:n], ALU.add, of[j].b + obk[j].b, o_.b, eng="pool")
                self.sumsq_rstd(lambda k: o_[:, 0:n], o_.b, 1, n, 128, W, rsh)
                self.STT(o_[:, 0:n], o_[:, 0:n], self.small[:, 0:1], rsh[:, 0:n], ALU.mult, ALU.mult, o_.b + self.small.b + rsh.b, o_.b)
                self.TT(ot[j][:, h, 0:n], o_[:, 0:n], gt[j][:, h, 0:n], ALU.mult, o_.b + gt[j].b, ot[j].b, eng="pool")
            self.DMA("pool", "h3_ot%d" % j, v(self.sOT), ot[j][:, :, 0:n], r=ot[j].b)
        self.S.barrier()


Builder.hgrn_p1 = hgrn_p1
Builder.hgrn_p2 = hgrn_p2
Builder.hgrn_p2b = hgrn_p2b
```

```python
import math
from contextlib import ExitStack
import numpy as np
import concourse.bass as bass
import concourse.mybir as mybir
from concourse.bass_utils import run_bass_kernel_spmd

F32 = mybir.dt.float32
BF16 = mybir.dt.bfloat16
AF = mybir.ActivationFunctionType
ALU = mybir.AluOpType

D = 1024
KC = 8
CTX = 256
LAT = 4096
T = CTX + LAT
NT = T // 128
GRID_W = 64
EPS = 1e-6
FF = 3584
FC = FF // 128
NE = 8
THETA = 10000.0
HG_STOP = 3
FORCE_MOE = False
MLA_PARTS = ('scale', 'lat', 'kr', 'q', 'kn', 'v')
HG_STEPS = 1000


class Buf:
    __slots__ = ("name", "w", "r", "x")

    def __init__(self, name=""):
        self.name = name
        self.w = None
        self.r = {}
        self.x = False


class Sched:
    ENGS = ("pe", "act", "dve", "pool", "sp")

    def __init__(self, nc):
        self.nc = nc
        self.lists = {e: [] for e in self.ENGS}
        self.cnt = {e: 0 for e in self.ENGS}
        self.semh = {}
        self.dcnt = {}
        self.waited = {e: {} for e in self.ENGS}
        self.keymap = {}
        self._ctx = []
        for e in self.ENGS:
            self._sem("E_" + e)

    def _sem(self, key):
        if key not in self.semh:
            cm = self.nc.semaphore(key)
            self.semh[key] = cm.__enter__()
            self._ctx.append(cm)
        return self.semh[key]

    def _deps(self, reads, writes):
        deps = {}

        def add(k, v):
            if deps.get(k, 0) < v:
                deps[k] = v
        for t in reads:
            if t.w is not None:
                add(*t.w)
        for t in writes:
            if t.w is not None:
                add(*t.w)
            for k, v in t.r.items():
                add(k, v)
        return deps

    def _emit_waits(self, eng, deps):
        wd = self.waited[eng]
        own = "E_" + eng
        for k, v in deps.items():
            if eng == "pe" and k == own:
                continue
            if wd.get(k, 0) >= v:
                continue
            wd[k] = v
            h = self.semh[k]
            self.lists[eng].append(lambda e, h=h, v=v: e.wait_ge(h, v))

    def _mark(self, ev, reads, writes):
        k, v = ev
        for t in reads:
            if t.r.get(k, 0) < v:
                t.r[k] = v
        for t in writes:
            t.w = ev
            t.r = {}

    def op(self, eng, fn, reads=(), writes=()):
        xs = [t for t in reads if t.x]
        if xs:
            writes = list(writes) + xs
        self._emit_waits(eng, self._deps(reads, writes))
        self.cnt[eng] += 1
        h = self.semh["E_" + eng]
        self.lists[eng].append(lambda e, fn=fn, h=h: fn(e).then_inc(h, 1))
        self._mark(("E_" + eng, self.cnt[eng]), reads, writes)

    def dma(self, q, key, out, in_, reads=(), writes=()):
        if key not in self.keymap:
            idx = len(self.keymap)
            self.keymap[key] = "D_%d" % idx
        key = self.keymap[key]
        h = self._sem(key)
        deps = self._deps(reads, writes)
        prev = self.dcnt.get(key, 0)
        if prev and deps.get(key, 0) < prev:
            deps[key] = prev
        self._emit_waits(q, deps)
        self.dcnt[key] = prev + 16
        self.lists[q].append(lambda e, out=out, in_=in_, h=h: e.dma_start(out=out, in_=in_).then_inc(h, 16))
        self._mark((key, prev + 16), reads, writes)

    def barrier(self):
        cur = {("E_" + e): self.cnt[e] for e in self.ENGS if self.cnt[e]}
        cur.update({k: v for k, v in self.dcnt.items() if v})
        for e in self.ENGS:
            self._emit_waits(e, dict(cur))
        self.keymap = {}

    def emit(self):
        nc = self.nc
        L = self.lists
        with nc.Block() as block:
            @block.tensor
            def _(e):
                for f in L["pe"]:
                    f(e)

            @block.scalar
            def _(e):
                for f in L["act"]:
                    f(e)

            @block.vector
            def _(e):
                for f in L["dve"]:
                    f(e)

            @block.gpsimd
            def _(e):
                for f in L["pool"]:
                    f(e)

            @block.sync
            def _(e):
                for f in L["sp"]:
                    f(e)


class Tile:
    def __init__(self, t, n=1, name=""):
        self.t = t
        self.b = [Buf(name + str(i)) for i in range(n)]

    def __getitem__(self, idx):
        return self.t[idx]


def _rope_tables(n, nrows, row0):
    cos = np.ones((nrows, T), np.float32)
    sin = np.zeros((nrows, T), np.float32)
    perm = np.zeros((nrows, nrows), np.float32)
    t = np.arange(LAT)
    pos = [(t // GRID_W).astype(np.float32), (t % GRID_W).astype(np.float32)]
    half = n // 2
    m = half
    inv = (THETA ** (-np.arange(0, m, 2, dtype=np.float32) / np.float32(m))).astype(np.float32)
    for f in range(n):
        blk, j = f // half, f % half
        fr = j % (m // 2)
        ang = (pos[blk] * inv[fr]).astype(np.float32)
        cos[row0 + f, CTX:] = np.cos(ang).astype(np.float32)
        s = np.sin(ang).astype(np.float32)
        if j < m // 2:
            sin[row0 + f, CTX:] = -s
            src = f + m // 2
        else:
            sin[row0 + f, CTX:] = s
            src = f - m // 2
        perm[row0 + src, row0 + f] = 1.0
    for f in range(nrows):
        if not (row0 <= f < row0 + n):
            perm[f, f] = 1.0
    return cos, sin, perm


def _consts():
    c = {}
    c["ident"] = np.eye(128, dtype=np.float32)
    s = np.arange(128)
    c["maskf"] = (s[:, None] <= s[None, :]).astype(np.float32)
    c["maskb"] = (s[:, None] >= s[None, :]).astype(np.float32)
    c1, s1, p1 = _rope_tables(32, 96, 64)
    c["cos1"], c["sin1"], c["perm1"] = c1, s1, p1
    c["cos1k"], c["sin1k"] = np.ascontiguousarray(c1[64:96]), np.ascontiguousarray(s1[64:96])
    c["perm1k"] = np.ascontiguousarray(p1[64:96, 64:96])
    c2, s2, p2 = _rope_tables(128, 128, 0)
    c["cos2"], c["sin2"], c["perm2"] = c2, s2, p2
    c3, s3, p3 = _rope_tables(64, 64, 0)
    c["cos3"] = np.concatenate([c3, c3], 0)
    c["sin3"] = np.concatenate([s3, s3], 0)
    pp = np.zeros((128, 128), np.float32)
    pp[:64, :64] = p3
    pp[64:, 64:] = p3
    c["perm3"] = pp
    return c


CONST_SHAPES = {k: v.shape for k, v in _consts().items()}

IN_SHAPES = {
    "x": [LAT, D], "c": [1, D], "ctx": [CTX, D], "c_ctx": [1, D],
    "mod_w": [4, D, 6 * D], "mod_b": [24, D], "norm_g": [16, D],
    "a_w_in": [D, 5120], "a_lb": [10, D], "a_onorm_g": [1, 128], "a_w_out": [D, D],
    "b_w_in": [D, 672], "b_qnorm_g": [3, 128], "b_kvnorm_g": [2, 128], "b_w_qb": [384, 1536],
    "b_w_kvb": [256, 2048], "b_w_out": [D, D],
    "c_w_in": [D, 1536], "c_qnorm_g": [1, 128], "c_knorm_g": [1, 128], "c_w_out": [D, D],
    "d_w_in": [D, 3072], "d_lambda": [1, 256], "d_onorm_g": [1, 128], "d_w_out": [D, D],
    "ffn_w_gu": [2, FC, 128, KC * 256], "ffn_w_down": [2, KC, 128, FF], "moe_router": [2, D, NE],
    "moe_w_gu": [2, NE, FC, 128, KC * 256], "moe_w_down": [2, NE, KC, 128, FF],
}


class Builder:
    def __init__(self, n_layers=4, debug=False):
        self.n_layers = n_layers
        self.debug = debug
        nc = self.nc = bass.Bass("TRN2", target_bir_lowering=False)
        self.S = Sched(nc)
        self.I = {}
        for k, shp in IN_SHAPES.items():
            self.I[k] = nc.dram_tensor(k, list(shp), F32, kind="ExternalInput").ap()
        for k, shp in CONST_SHAPES.items():
            self.I[k] = nc.dram_tensor(k, list(shp), F32, kind="ExternalInput").ap()
        self.out = nc.dram_tensor("out", [LAT, D], F32, kind="ExternalOutput").ap()
        if debug:
            self.dbg = nc.dram_tensor("dbg", [D, T], F32, kind="ExternalOutput").ap()

        def scr(name, shape, dt):
            return nc.dram_tensor(name, shape, dt, kind="Internal").ap()
        self.hT = scr("hT", [D, T], F32)
        self.hb = [Buf("hT%d" % i) for i in range(NT)]
        self.sA = scr("sA", [1536, T], BF16)
        self.sB = scr("sB", [1024, T], BF16)
        self.sC = scr("sC", [1024, T], BF16)
        self.sKR = scr("sKR", [32, T], BF16)
        self.sG = scr("sG", [D, T], BF16)
        self.sV = scr("sV", [T, 1024], BF16)
        self.sF1 = scr("sF1", [D, T], F32)
        self.sF2 = scr("sF2", [D, T], F32)
        self.sF3 = scr("sF3", [D, T], F32)
        self.sF4 = scr("sF4", [D, T], F32)
        self.sOT = scr("sOT", [D, T], BF16)
        self.uid = 0

    def sb(self, es, name, shape, dt, n=1):
        self.uid += 1
        t = es.enter_context(self.nc.sbuf_tensor("s%d_%s" % (self.uid, name), list(shape), dt))
        return Tile(t, n, name)

    def ps(self, es, name, shape, dt=F32, n=1):
        self.uid += 1
        t = es.enter_context(self.nc.psum_tensor("p%d_%s" % (self.uid, name), list(shape), dt))
        tl = Tile(t, n, name)
        for b in tl.b:
            b.x = True
        return tl

    @staticmethod
    def view(tile, ap):
        v = Tile(ap, 0)
        v.b = tile.b
        return v

    def MM(self, out, lhsT, rhs, start, stop, r, w):
        self.S.op("pe", lambda e: e.matmul(out, lhsT=lhsT, rhs=rhs, start=start, stop=stop), r, w)

    def TR(self, out, in_, ident, r, w):
        self.S.op("pe", lambda e: e.transpose(out, in_, ident), r, w)

    def ACT(self, out, in_, func, r, w, bias=None, scale=None, eng="act"):
        kw = {}
        if bias is not None:
            kw["bias"] = bias
        if scale is not None:
            kw["scale"] = scale
        self.S.op(eng, lambda e: e.activation(out=out, in_=in_, func=func, **kw), r, w)

    def TT(self, out, a, b, op, r, w, eng="dve"):
        self.S.op(eng, lambda e: e.tensor_tensor(out=out, in0=a, in1=b, op=op), r, w)

    def TS(self, out, a, s1, s2, op0, op1, r, w, eng="dve"):
        if s2 is None:
            self.S.op(eng, lambda e: e.tensor_scalar(out=out, in0=a, scalar1=s1, scalar2=None, op0=op0), r, w)
        else:
            self.S.op(eng, lambda e: e.tensor_scalar(out=out, in0=a, scalar1=s1, scalar2=s2, op0=op0, op1=op1), r, w)

    def STT(self, out, a, s, b, op0, op1, r, w, eng="dve"):
        self.S.op(eng, lambda e: e.scalar_tensor_tensor(out=out, in0=a, scalar=s, in1=b, op0=op0, op1=op1), r, w)

    def CP(self, out, in_, r, w, eng="dve"):
        if eng == "act":
            self.S.op(eng, lambda e: e.copy(out=out, in_=in_), r, w)
        else:
            self.S.op(eng, lambda e: e.tensor_copy(out=out, in_=in_), r, w)

    def RCP(self, out, in_, r, w):
        self.S.op("dve", lambda e: e.reciprocal(out=out, in_=in_), r, w)

    def MEMSET(self, ap, val, w, eng="pool"):
        self.S.op(eng, lambda e: e.memset(ap, val), (), w)

    def DMA(self, q, key, out, in_, r=(), w=()):
        self.S.dma(q, key, out, in_, r, w)

    def rstd(self, out, ss, n, r, w, tmp, tmpb):
        self.ACT(tmp, ss, AF.Sqrt, r, tmpb, bias=self.epsc[0:ss.shape[0], :], scale=1.0 / n)
        self.RCP(out, tmp, tmpb, w)

    def setup(self, es):
        I = self.I
        self.ident = self.sb(es, "ident", [128, 128], F32)
        self.identb = self.sb(es, "identb", [128, 128], BF16)
        self.onesf = self.sb(es, "onesf", [128, 128], F32)
        self.onesb = self.sb(es, "onesb", [128, 128], BF16)
        self.epsc = self.sb(es, "epsc", [128, 1], F32)
        self.DMA("sp", "c0", self.ident[:], I["ident"][:, :], w=self.ident.b)
        self.CP(self.identb[:], self.ident[:], self.ident.b, self.identb.b)
        self.MEMSET(self.onesf[:], 1.0, self.onesf.b)
        self.MEMSET(self.onesb[:], 1.0, self.onesb.b)
        self.MEMSET(self.epsc[:], EPS, self.epsc.b)
        self.epsc = self.epsc
        epsT = self.epsc
        self.epsc = epsT.t
        self.epsb = epsT.b
        self.condT = self.sb(es, "condT", [128, KC, 2], F32)
        self.normg = self.sb(es, "normg", [128, KC, 16], F32)
        self.modb = self.sb(es, "modb", [128, KC, 24], F32)
        self.alb = self.sb(es, "alb", [128, KC, 10], F32)
        self.small = self.sb(es, "smallc", [128, 9], F32)
        self.modc = self.sb(es, "modc", [128, 6, KC, 2], F32)
        self.Acol = self.sb(es, "Acol", [128, 2, KC, 2], F32)
        self.Bcol = self.sb(es, "Bcol", [128, 2, KC, 2], F32)
        self.Gcol = self.sb(es, "Gcol", [128, 2, KC, 2], F32)
        self.lbc = self.sb(es, "lbc", [128, KC, 2, 2], F32)
        with ExitStack() as e2:
            rows = self.sb(e2, "rows", [24, D], F32)
            pt = self.ps(e2, "setup_ps", [128, 512], F32)

            def rows_to_cols(src_ap, R, dst, C, key):
                self.DMA("sp", key, rows[0:R, 0:C * 128], src_ap, w=rows.b)
                for c in range(C):
                    self.TR(pt[:, 0:R], rows[0:R, c * 128:(c + 1) * 128], self.ident[0:R, 0:R],
                            rows.b + self.ident.b, pt.b)
                    self.CP(dst(c), pt[:, 0:R], pt.b, self._colw)
            self._colw = self.condT.b
            rows_to_cols(I["c"][:, :], 1, lambda c: self.condT[:, c, 0:1], KC, "rw")
            rows_to_cols(I["c_ctx"][:, :], 1, lambda c: self.condT[:, c, 1:2], KC, "rw")
            sg = self.sb(e2, "sgc", [128, KC, 2], F32)
            self.ACT(sg[:], self.condT[:], AF.Sigmoid, self.condT.b, sg.b)
            self.TT(self.condT[:], self.condT[:], sg[:], ALU.mult, sg.b + self.condT.b, self.condT.b)
            self._colw = self.normg.b
            rows_to_cols(I["norm_g"][:, :], 16, lambda c: self.normg[:, c, :], KC, "rw")
            self._colw = self.modb.b
            rows_to_cols(I["mod_b"][:, :], 24, lambda c: self.modb[:, c, :], KC, "rw")
            self._colw = self.alb.b
            rows_to_cols(I["a_lb"][:, :], 10, lambda c: self.alb[:, c, :], KC, "rw")
            self._colw = self.small.b
            rows_to_cols(I["a_onorm_g"][:, :], 1, lambda c: self.small[:, 0:1], 1, "rw")
            rows_to_cols(I["b_qnorm_g"][:, :], 3, lambda c: self.small[:, 1:4], 1, "rw")
            rows_to_cols(I["b_kvnorm_g"][:, :], 2, lambda c: self.small[:, 4:6], 1, "rw")
            rows_to_cols(I["c_qnorm_g"][:, :], 1, lambda c: self.small[:, 6:7], 1, "rw")
            rows_to_cols(I["c_knorm_g"][:, :], 1, lambda c: self.small[:, 7:8], 1, "rw")
            rows_to_cols(I["d_onorm_g"][:, :], 1, lambda c: self.small[:, 8:9], 1, "rw")
            ex = self.sb(e2, "albx", [128, KC, 10], F32)
            sm = self.sb(e2, "albs", [128, KC, 2], F32)
            self.ACT(ex[:], self.alb[:], AF.Exp, self.alb.b, ex.b)
            for d in range(2):
                self.TT(sm[:, :, d], ex[:, :, 5 * d], ex[:, :, 5 * d + 1], ALU.add, ex.b, sm.b)
                for j in range(2, 5):
                    self.TT(sm[:, :, d], sm[:, :, d], ex[:, :, 5 * d + j], ALU.add, ex.b + sm.b, sm.b)
            self.RCP(sm[:], sm[:], sm.b, sm.b)
            for d in range(2):
                self.TT(self.lbc[:, :, d, 0], ex[:, :, 5 * d], sm[:, :, d], ALU.mult, ex.b + sm.b, self.lbc.b)
                self.TS(self.lbc[:, :, d, 1], self.lbc[:, :, d, 0], -1.0, 1.0, ALU.mult, ALU.add, self.lbc.b, self.lbc.b)
            xin = [self.sb(e2, "xin%d" % i, [128, D], F32) for i in range(2)]
            stg = [self.sb(e2, "xstg%d" % i, [128, KC, 512], F32) for i in range(2)]
            pts = [self.ps(e2, "xps%d" % i, [128, 512], F32) for i in range(2)]
            blocks = [(0, 2)] + [(2 + 4 * i, 4) for i in range(8)]
            for bi, (t0, nt) in enumerate(blocks):
                st = stg[bi % 2]
                for j in range(nt):
                    tt = t0 + j
                    xt = xin[tt % 2]
                    src = I["ctx"][tt * 128:(tt + 1) * 128, :] if tt < 2 else I["x"][(tt - 2) * 128:(tt - 1) * 128, :]
                    self.DMA("sp", "xin%d" % (tt % 2), xt[:], src, w=xt.b)
                    for k in range(KC):
                        p = pts[k % 2]
                        self.TR(p[:, 0:128], xt[:, k * 128:(k + 1) * 128], self.ident[:], xt.b + self.ident.b, p.b)
                        self.CP(st[:, k, j * 128:(j + 1) * 128], p[:, 0:128], p.b, st.b, eng=("act" if k % 2 else "dve"))
                self.DMA("pool", "xst%d" % (bi % 2), self.hT.rearrange("(k p) t -> p k t", p=128)[:, :, t0 * 128:(t0 + nt) * 128],
                         st[:, :, 0:nt * 128], r=st.b, w=[self.hb[t0 + j] for j in range(nt)])
            self.S.barrier()

    def mod_phase(self, i):
        I = self.I
        with ExitStack() as es:
            wb = [self.sb(es, "modw%d" % j, [128, KC, 512], F32) for j in range(2)]
            pm = self.ps(es, "modps", [128, 48, 2], F32)
            for cb in range(12):
                w = wb[cb % 2]
                self.DMA("sp", "modw%d" % (cb % 2), w[:], I["mod_w"][i].rearrange("(k p) n -> p k n", p=128)[:, :, cb * 512:(cb + 1) * 512], w=w.b)
                for f in range(4):
                    fc = cb * 4 + f
                    for k in range(KC):
                        self.MM(pm[:, fc, :], w[:, k, f * 128:(f + 1) * 128], self.condT[:, k, :], k == 0, k == KC - 1,
                                w.b + self.condT.b, pm.b)
            for s in range(2):
                self.TT(self.modc[:, :, :, s], pm[:, :, s].rearrange("p (j c) -> p j c", j=6),
                        self.modb[:, :, 6 * i:6 * i + 6].rearrange("p c j -> p j c"), ALU.add, pm.b + self.modb.b, self.modc.b)
            for sub in range(2):
                jb, ja, jg = 3 * sub, 3 * sub + 1, 3 * sub + 2
                gin, gout = 4 * i + 2 * sub, 4 * i + 2 * sub + 1
                for s in range(2):
                    self.STT(self.Acol[:, sub, :, s], self.modc[:, ja, :, s], 1.0, self.normg[:, :, gin], ALU.add, ALU.mult,
                             self.modc.b + self.normg.b, self.Acol.b)
                    self.CP(self.Bcol[:, sub, :, s], self.modc[:, jb, :, s], self.modc.b, self.Bcol.b)
                    self.TT(self.Gcol[:, sub, :, s], self.modc[:, jg, :, s], self.normg[:, :, gout], ALU.mult,
                            self.modc.b + self.normg.b, self.Gcol.b)
            self.S.barrier()

    @staticmethod
    def segs(t0, n):
        out = []
        if t0 < CTX:
            m = min(CTX, t0 + n) - t0
            out.append((0, m, 1))
            if n > m:
                out.append((m, n - m, 0))
        else:
            out.append((0, n, 0))
        return out

    def load_h(self, hblk, t0, n, key):
        self.DMA("sp", key, hblk[:, :, 0:n], self.hT.rearrange("(k p) t -> p k t", p=128)[:, :, t0:t0 + n],
                 r=[self.hb[t] for t in range(t0 // 128, (t0 + n) // 128)], w=hblk.b)

    def store_h(self, hblk, t0, n, key):
        self.DMA("pool", key, self.hT.rearrange("(k p) t -> p k t", p=128)[:, :, t0:t0 + n], hblk[:, :, 0:n],
                 r=hblk.b, w=[self.hb[t] for t in range(t0 // 128, (t0 + n) // 128)])

    def sumsq_rstd(self, src_fn, src_b, nchunks, n, ndim, W, out_rstd):
        for k in range(nchunks):
            sq = W["sq"][k % 2]
            self.ACT(sq[:, 0:n], src_fn(k), AF.Square, src_b, sq.b)
            self.MM(W["pss"][:, 0:n], self.onesb[:], sq[:, 0:n], k == 0, k == nchunks - 1, self.onesb.b + sq.b, W["pss"].b)
        self.rstd(out_rstd[:, 0:n], W["pss"][:, 0:n], ndim, W["pss"].b + self.epsb, out_rstd.b, W["rt"][:, 0:n], W["rt"].b)

    def norm_mod(self, hblk, t0, n, sub, uT, ucol0, W, u32=None):
        self.sumsq_rstd(lambda k: hblk[:, k, 0:n], hblk.b, KC, n, D, W, W["rs"])
        for k in range(KC):
            tmp = W["tmp"][k % 2]
            for (o, m, s) in self.segs(t0, n):
                self.STT(tmp[:, o:o + m], hblk[:, k, o:o + m], self.Acol[:, sub, k, s:s + 1], W["rs"][:, o:o + m], ALU.mult, ALU.mult,
                         hblk.b + self.Acol.b + W["rs"].b, tmp.b)
                dst = uT[:, k, ucol0 + o:ucol0 + o + m] if u32 is None else u32[k][:, o:o + m]
                dstb = [uT.b[k]] if u32 is None else u32[k].b
                self.ACT(dst, tmp[:, o:o + m], AF.Identity, tmp.b + self.Bcol.b, dstb, bias=self.Bcol[:, sub, k, s:s + 1], scale=1.0)

    def resid_update(self, hblk, t0, n, sub, y_fn, y_b, W):
        self.sumsq_rstd(y_fn, y_b, KC, n, D, W, W["rs"])
        for k in range(KC):
            tmp = W["tmp"][k % 2]
            for (o, m, s) in self.segs(t0, n):
                self.STT(tmp[:, o:o + m], y_fn(k)[:, o:o + m], self.Gcol[:, sub, k, s:s + 1], W["rs"][:, o:o + m], ALU.mult, ALU.mult,
                         y_b + self.Gcol.b + W["rs"].b, tmp.b)
                self.TT(hblk[:, k, o:o + m], hblk[:, k, o:o + m], tmp[:, o:o + m], ALU.add, hblk.b + tmp.b, hblk.b, eng="pool")

    def work_tiles(self, es, n=512):
        W = {}
        W["sq"] = [self.sb(es, "w_sq%d" % i, [128, n], BF16) for i in range(2)]
        W["pss"] = self.ps(es, "w_pss", [128, n], F32)
        W["rt"] = self.sb(es, "w_rt", [128, n], F32)
        W["rs"] = self.sb(es, "w_rs", [128, n], F32)
        W["tmp"] = [self.sb(es, "w_tmp%d" % i, [128, n], F32) for i in range(2)]
        return W

    def load_w(self, wt, src, key, kc=None):
        kc = kc or src.shape[0] // 128
        v = src.rearrange("(k p) n -> p k n", p=128)
        for k in range(kc):
            self.DMA("pool", key, wt[:, k, :], v[:, k, :], w=wt.b)

    def final_out(self):
        with ExitStack() as es:
            hblk = [self.sb(es, "fo_h%d" % i, [128, KC, 512], F32) for i in range(2)]
            ot = [self.sb(es, "fo_o%d" % i, [128, D], F32) for i in range(2)]
            pts = [self.ps(es, "fo_ps%d" % i, [128, 512], F32) for i in range(2)]
            for bi in range(8):
                t0 = CTX + bi * 512
                hb = hblk[bi % 2]
                self.load_h(hb, t0, 512, "foh%d" % (bi % 2))
                for j in range(4):
                    o = ot[j % 2]
                    for kk in range(2):
                        p = pts[kk]
                        for q in range(4):
                            k = kk * 4 + q
                            self.TR(p[:, q * 128:(q + 1) * 128], hb[:, k, j * 128:(j + 1) * 128], self.ident[:], hb.b + self.ident.b, p.b)
                        self.CP(o[:, kk * 512:(kk + 1) * 512], p[:], p.b, o.b, eng=("act" if kk else "dve"))
                    r0 = bi * 512 + j * 128
                    self.DMA("pool", "foo%d" % (j % 2), self.out[r0:r0 + 128, :], o[:], r=o.b)
            if self.debug:
                for bi in range(9):
                    t0, n = (0, 256) if bi == 0 else (CTX + (bi - 1) * 512, 512)
                    hb = hblk[bi % 2]
                    self.load_h(hb, t0, n, "foh%d" % (bi % 2))
                    self.DMA("pool", "dbg%d" % (bi % 2), self.dbg.rearrange("(k p) t -> p k t", p=128)[:, :, t0:t0 + n], hb[:, :, 0:n], r=hb.b)
            self.S.barrier()


def _blocks(t0, n, bs=512):
    out = []
    o = 0
    while o < n:
        m = min(bs, n - o)
        out.append((t0 + o, o, m))
        o += m
    return out


def ffn_phase(self, i, wout_ap):
    I = self.I
    moe = (i % 2 == 1) or FORCE_MOE
    li = i // 2
    E = NE if moe else 1
    NS = 4
    CS = FC // NS
    last = (i == 3)
    gt = [(2, 8), (10, 8), (18, 8), (26, 8)] if last else [(0, 9), (9, 9), (18, 8), (26, 8)]
    TGM = 9 * 128
    wgu_l = I["moe_w_gu"][li] if moe else I["ffn_w_gu"]
    wd_l = I["moe_w_down"][li] if moe else I["ffn_w_down"]
    if not moe:
        wgu_l = wgu_l[li:li + 1]
        wd_l = wd_l[li:li + 1]
    with ExitStack() as es:
        W = self.work_tiles(es)
        wout = self.sb(es, "f_wout", [128, KC, D], BF16)
        self.load_w(wout, wout_ap, "f_wout")
        uT = self.sb(es, "f_uT", [128, KC, TGM], BF16, n=KC)
        hid = [self.sb(es, "f_hid%d" % j, [128, CS, TGM], BF16, n=CS) for j in range(2)]
        yacc = self.sb(es, "f_yacc", [128, KC, TGM], F32, n=KC)
        hblk = self.sb(es, "f_hblk", [128, KC, 512], F32)
        otb = self.sb(es, "f_otb", [128, KC, 512], BF16)
        ysb = self.sb(es, "f_ysb", [128, KC, 512], F32, n=KC)
        wgu = [self.sb(es, "f_wgu%d" % j, [128, KC, 256], BF16) for j in range(3)]
        wdt = [self.sb(es, "f_wd%d" % j, [128, CS, 128], BF16) for j in range(3)]
        sgt = [self.sb(es, "f_sg%d" % j, [128, 512], F32) for j in range(2)]
        pG = [self.ps(es, "f_pG%d" % j, [128, 512]) for j in range(2)]
        pU = [self.ps(es, "f_pU%d" % j, [128, 512]) for j in range(2)]
        pY = [self.ps(es, "f_pY%d" % j, [128, 512]) for j in range(2)]
        pX = self.ps(es, "f_pX", [128, 512])
        if moe:
            rt_w = self.sb(es, "f_router", [128, KC, NE], F32)
            self.DMA("sp", "f_router", rt_w[:], I["moe_router"][li].rearrange("(k p) e -> p k e", p=128), w=rt_w.b)
            u32 = self.sb(es, "f_u32", [128, KC, 512], F32, n=KC)
            t1 = [self.sb(es, "f_t1%d" % j, [128, 512], F32) for j in range(2)]
            CB = self.sb(es, "f_CB", [128, TGM], BF16)
            comb = self.sb(es, "f_comb", [128, 9, NE], F32)
            dg = [self.sb(es, "f_dg%d" % j, [128, 128], F32) for j in range(2)]
            rsm = self.sb(es, "f_rsm", [128, 40], F32)
        cnt = {"wgu": 0, "wd": 0, "gu": 0, "y": 0}
        for (tile0, ntile) in gt:
            t0g, ng = tile0 * 128, ntile * 128
            blks = _blocks(t0g, ng)
            for (t0, o, n) in blks:
                self.load_h(hblk, t0, n, "f_hblk")
                self.DMA("sp", "f_otb", otb[:, :, 0:n], self.sOT.rearrange("(k p) t -> p k t", p=128)[:, :, t0:t0 + n], w=otb.b)
                for fo in range(KC):
                    p = pY[fo % 2]
                    for k in range(KC):
                        self.MM(p[:, 0:n], wout[:, k, fo * 128:(fo + 1) * 128], otb[:, k, 0:n], k == 0, k == KC - 1, wout.b + otb.b, p.b)
                    self.CP(ysb[:, fo, 0:n], p[:, 0:n], p.b, [ysb.b[fo]], eng=("act" if fo % 2 else "dve"))
                self.resid_update(hblk, t0, n, 0, lambda k: ysb[:, k, 0:n], ysb.b, W)
                self.store_h(hblk, t0, n, "f_hst")
                if not moe:
                    self.norm_mod(hblk, t0, n, 1, uT, o, W)
                else:
                    self.sumsq_rstd(lambda k: hblk[:, k, 0:n], hblk.b, KC, n, D, W, W["rs"])
                    nt_ = n // 128
                    for k in range(KC):
                        tmp = W["tmp"][k % 2]
                        for (oo, m, s) in self.segs(t0, n):
                            self.STT(tmp[:, oo:oo + m], hblk[:, k, oo:oo + m], self.Acol[:, 1, k, s:s + 1], W["rs"][:, oo:oo + m], ALU.mult, ALU.mult,
                                     hblk.b + self.Acol.b + W["rs"].b, tmp.b)
                            self.ACT(u32[:, k, oo:oo + m], tmp[:, oo:oo + m], AF.Identity, tmp.b + self.Bcol.b, [u32.b[k]], bias=self.Bcol[:, 1, k, s:s + 1], scale=1.0)
                        self.CP(uT[:, k, o:o + n], u32[:, k, 0:n], [u32.b[k]], [uT.b[k]], eng="pool")
                    for j in range(nt_):
                        for k in range(KC):
                            self.MM(pX[:, j * 8:(j + 1) * 8], u32[:, k, j * 128:(j + 1) * 128], rt_w[:, k, :], k == 0, k == KC - 1, [u32.b[k]] + rt_w.b, pX.b)
                    for j in range(nt_):
                        tt = o // 128 + j
                        lg, m8, mk, ex = rsm[:, 0:8], rsm[:, 8:16], rsm[:, 16:24], rsm[:, 24:32]
                        self.CP(lg, pX[:, j * 8:(j + 1) * 8], pX.b, rsm.b)
                        self.S.op("dve", lambda e, m8=m8, lg=lg: e.max(out=m8, in_=lg), rsm.b, rsm.b)
                        self.TS(mk, lg, rsm[:, 9:10], None, ALU.is_ge, None, rsm.b, rsm.b)
                        self.TS(rsm[:, 32:33], rsm[:, 8:9], -1.0, None, ALU.mult, None, rsm.b, rsm.b)
                        self.ACT(ex, lg, AF.Exp, rsm.b, rsm.b, bias=rsm[:, 32:33], scale=1.0)
                        self.TT(ex, ex, mk, ALU.mult, rsm.b, rsm.b)
                        self.S.op("dve", lambda e, ex=ex: e.reduce_sum(out=rsm[:, 33:34], in_=ex, axis=mybir.AxisListType.X), rsm.b, rsm.b)
                        self.RCP(rsm[:, 34:35], rsm[:, 33:34], rsm.b, rsm.b)
                        self.TS(comb[:, tt, :], ex, rsm[:, 34:35], None, ALU.mult, None, rsm.b, comb.b)
            jobs = [(e, s) for e in range(E) for s in range(NS)]

            def GU(j):
                e, s = jobs[j]
                hd = hid[j % 2]
                if moe and s == 0:
                    for (t0, o, n) in blks:
                        for q in range(n // 128):
                            tt = o // 128 + q
                            d_ = dg[tt % 2]
                            self.TS(d_[:], self.ident[:], comb[:, tt, e:e + 1], None, ALU.mult, None, self.ident.b + comb.b, d_.b)
                            self.MM(pX[:, q * 128:(q + 1) * 128], self.onesf[:], d_[:], True, True, self.onesf.b + d_.b, pX.b)
                        self.CP(CB[:, o:o + n], pX[:, 0:n], pX.b, CB.b, eng="act")
                for cc in range(CS):
                    c = s * CS + cc
                    w = wgu[cnt["wgu"] % 3]
                    cnt["wgu"] += 1
                    self.DMA("pool", "f_wgu%d" % ((cnt["wgu"] - 1) % 3), w[:].rearrange("p k n -> p (k n)"), wgu_l[e, c], w=w.b)
                    for (t0, o, n) in blks:
                        g_, u_ = pG[cnt["gu"] % 2], pU[cnt["gu"] % 2]
                        sg = sgt[cnt["gu"] % 2]
                        for k in range(KC):
                            self.MM(g_[:, 0:n], w[:, k, 0:128], uT[:, k, o:o + n], k == 0, k == KC - 1, w.b + [uT.b[k]], g_.b)
                        for k in range(KC):
                            self.MM(u_[:, 0:n], w[:, k, 128:256], uT[:, k, o:o + n], k == 0, k == KC - 1, w.b + [uT.b[k]], u_.b)
                        self.ACT(sg[:, 0:n], g_[:, 0:n], AF.Silu, g_.b, sg.b)
                        if moe:
                            tt1 = t1[cnt["gu"] % 2]
                            self.TT(tt1[:, 0:n], sg[:, 0:n], u_[:, 0:n], ALU.mult, sg.b + u_.b, tt1.b)
                            self.TT(hd[:, cc, o:o + n], tt1[:, 0:n], CB[:, o:o + n], ALU.mult, tt1.b + CB.b, [hd.b[cc]])
                        else:
                            self.TT(hd[:, cc, o:o + n], sg[:, 0:n], u_[:, 0:n], ALU.mult, sg.b + u_.b, [hd.b[cc]])
                        cnt["gu"] += 1

            def DN(j):
                e, s = jobs[j]
                hd = hid[j % 2]
                for fo in range(KC):
                    w = wdt[cnt["wd"] % 3]
                    cnt["wd"] += 1
                    self.DMA("pool", "f_wd%d" % ((cnt["wd"] - 1) % 3), w[:].rearrange("p c n -> p (c n)"), wd_l[e, fo][:, s * CS * 128:(s + 1) * CS * 128], w=w.b)
                    for (t0, o, n) in blks:
                        p = pY[cnt["y"] % 2]
                        cnt["y"] += 1
                        for cc in range(CS):
                            self.MM(p[:, 0:n], w[:, cc, :], hd[:, cc, o:o + n], cc == 0, cc == CS - 1, w.b + [hd.b[cc]], p.b)
                        if j == 0:
                            self.CP(yacc[:, fo, o:o + n], p[:, 0:n], p.b, [yacc.b[fo]], eng="act")
                        else:
                            self.TT(yacc[:, fo, o:o + n], yacc[:, fo, o:o + n], p[:, 0:n], ALU.add, p.b + [yacc.b[fo]], [yacc.b[fo]])
            GU(0)
            for j in range(len(jobs)):
                if j + 1 < len(jobs):
                    GU(j + 1)
                DN(j)
            for (t0, o, n) in blks:
                self.load_h(hblk, t0, n, "f_hblk")
                self.resid_update(hblk, t0, n, 1, lambda k: yacc[:, k, o:o + n], yacc.b, W)
                self.store_h(hblk, t0, n, "f_hst")
        self.S.barrier()


Builder.ffn_phase = ffn_phase


P1_BLOCKS = [(0, 256)] + [(CTX + 512 * b, 512) for b in range(8)]


def attn_tiles(self, es, nS=5):
    A = {}
    A["nS"] = nS
    A["pS"] = [self.ps(es, "a_pS%d" % j, [128, 512]) for j in range(nS)]
    A["pT"] = [self.sb(es, "a_pT%d" % j, [128, 512], BF16) for j in range(6)]
    A["pO"] = [self.ps(es, "a_pO%d" % j, [128, 512]) for j in range(2)]
    A["pB"] = self.ps(es, "a_pB", [128, 512])
    A["acc"] = [[self.sb(es, "a_acc%d%d" % (i, j), [128, 512], F32) for j in range(2)] for i in range(2)]
    A["osb"] = [self.sb(es, "a_osb%d" % j, [128, 512], F32) for j in range(2)]
    A["n"] = 0
    A["ns"] = 0
    return A


def attn_block(self, A, kfn, q_ap, vfn, kts, nq, dv, scale, rb, out_ap, out_b, after=None):
    i0 = A["n"]
    A["n"] += 1
    pO, osb, acc = A["pO"][i0 % 2], A["osb"][i0 % 2], A["acc"][i0 % 2]
    n = len(kts)
    LA = A["nS"] - 1
    slots = {}
    seen = [False, False]

    def qk(i):
        ps_ = A["pS"][A["ns"] % A["nS"]]
        pt = A["pT"][A["ns"] % 6]
        A["ns"] += 1
        slots[i] = (ps_, pt)
        self.MM(ps_[:, 0:nq], kfn(kts[i]), q_ap, True, True, rb, ps_.b)
        self.ACT(pt[:, 0:nq], ps_[:, 0:nq], AF.Exp, ps_.b, pt.b, scale=scale)

    def pv(i):
        ps_, pt = slots.pop(i)
        self.MM(pO[0:dv, 0:nq], vfn(kts[i]), pt[:, 0:nq], i == 0, i == n - 1, rb + pt.b, pO.b)
        j = 1 if (i % 2 == 1 or i == 16) else 0
        eng = "pool" if j else "dve"
        a_ = acc[j]
        if not seen[j]:
            seen[j] = True
            self.CP(a_[:, 0:nq], pt[:, 0:nq], pt.b, a_.b, eng=eng)
        else:
            self.TT(a_[:, 0:nq], a_[:, 0:nq], pt[:, 0:nq], ALU.add, a_.b + pt.b, a_.b, eng=eng)

    for i in range(min(LA, n)):
        qk(i)
    if A.get("pending") is not None:
        A["pending"]()
        A["pending"] = None
    for i in range(n):
        if i + LA < n:
            qk(i + LA)
        pv(i)

    def fin():
        self.MM(A["pB"][0:dv, 0:nq], self.onesf[:, 0:dv], acc[0][:, 0:nq], True, False, self.onesf.b + acc[0].b, A["pB"].b)
        self.MM(A["pB"][0:dv, 0:nq], self.onesf[:, 0:dv], acc[1][:, 0:nq], False, True, self.onesf.b + acc[1].b, A["pB"].b)
        self.RCP(osb[0:dv, 0:nq], A["pB"][0:dv, 0:nq], A["pB"].b, osb.b)
        self.TT(out_ap, pO[0:dv, 0:nq], osb[0:dv, 0:nq], ALU.mult, pO.b + osb.b, out_b)
        if after is not None:
            after()
    A["pending"] = fin


def attn_flush(self, A):
    if A.get("pending") is not None:
        A["pending"]()
        A["pending"] = None


Builder.attn_flush = attn_flush
Builder.attn_tiles = attn_tiles
Builder.attn_block = attn_block


def rope_chunk(self, R, pq, nrow, n, qn_fn, cosb, sinb, permb, out_ap, out_b, rs=None):
    qn, pr, t1, t2 = R["qn"][R["i"] % 2], R["pr"], R["t1"][R["i"] % 2], R["t2"][R["i"] % 2]
    R["i"] += 1
    qn_fn(qn)
    self.MM(pr[0:nrow, 0:n], permb[0:nrow, 0:nrow], qn[0:nrow, 0:n], True, True, permb.b + qn.b, pr.b)
    self.TT(t1[0:nrow, 0:n], qn[0:nrow, 0:n], cosb[0:nrow, 0:n], ALU.mult, qn.b + cosb.b, t1.b, eng="pool")
    self.TT(t2[0:nrow, 0:n], pr[0:nrow, 0:n], sinb[0:nrow, 0:n], ALU.mult, pr.b + sinb.b, t2.b)
    if rs is None:
        self.TT(out_ap, t1[0:nrow, 0:n], t2[0:nrow, 0:n], ALU.add, t1.b + t2.b, out_b, eng="pool")
    else:
        self.TT(t1[0:nrow, 0:n], t1[0:nrow, 0:n], t2[0:nrow, 0:n], ALU.add, t1.b + t2.b, t1.b, eng="pool")
        self.TT(out_ap, t1[0:nrow, 0:n], rs[0:nrow, 0:n], ALU.mult, t1.b + rs.b, out_b)


def rope_tiles(self, es, cos_name, sin_name, perm_name, nrow):
    R = {"i": 0}
    R["qn"] = [self.sb(es, "r_qn%d" % j, [128, 512], BF16) for j in range(2)]
    R["t1"] = [self.sb(es, "r_t1%d" % j, [128, 512], F32) for j in range(2)]
    R["t2"] = [self.sb(es, "r_t2%d" % j, [128, 512], F32) for j in range(2)]
    R["pr"] = self.ps(es, "r_pr", [128, 512])
    R["cos"] = [self.sb(es, "r_cos%d" % j, [128, 512], F32) for j in range(2)]
    R["sin"] = [self.sb(es, "r_sin%d" % j, [128, 512], F32) for j in range(2)]
    pf = self.sb(es, "r_permf", [128, 128], F32)
    R["perm"] = self.sb(es, "r_perm", [128, 128], BF16)
    self.DMA("sp", "r_perm", pf[0:nrow, 0:nrow], self.I[perm_name][:, :], w=pf.b)
    self.CP(R["perm"][0:nrow, 0:nrow], pf[0:nrow, 0:nrow], pf.b, R["perm"].b)
    R["names"] = (cos_name, sin_name, nrow)
    return R


def rope_load(self, R, bi, t0, n):
    cn, sn, nrow = R["names"]
    cb, sbb = R["cos"][bi % 2], R["sin"][bi % 2]
    self.DMA("sp", "r_cos%d" % (bi % 2), cb[0:nrow, 0:n], self.I[cn][:, t0:t0 + n], w=cb.b)
    self.DMA("sp", "r_sin%d" % (bi % 2), sbb[0:nrow, 0:n], self.I[sn][:, t0:t0 + n], w=sbb.b)
    return cb, sbb


Builder.rope_chunk = rope_chunk
Builder.rope_tiles = rope_tiles
Builder.rope_load = rope_load


def gqa_p1(self):
    I = self.I
    with ExitStack() as es:
        W = self.work_tiles(es)
        R = self.rope_tiles(es, "cos2", "sin2", "perm2", 128)
        win = self.sb(es, "g_win", [128, KC, 1536], BF16)
        self.load_w(win, I["c_w_in"], "g_win")
        uT = self.sb(es, "g_uT", [128, KC, 512], BF16, n=KC)
        hblk = [self.sb(es, "g_h%d" % j, [128, KC, 512], F32) for j in range(2)]
        pq = [self.ps(es, "g_pq%d" % j, [128, 512]) for j in range(2)]
        pv = self.ps(es, "g_pv", [128, 512])
        rs2 = self.sb(es, "g_rs2", [128, 512], F32)
        qo = [self.sb(es, "g_qo%d" % j, [128, 512], BF16) for j in range(3)]
        vt = [self.sb(es, "g_vt%d" % j, [128, 256], BF16) for j in range(2)]
        for bi, (t0, n) in enumerate(P1_BLOCKS):
            hb = hblk[bi % 2]
            self.load_h(hb, t0, n, "g_h%d" % (bi % 2))
            cb, sbb = self.rope_load(R, bi, t0, n)
            self.norm_mod(hb, t0, n, 0, uT, 0, W)
            for ch in range(10):
                p = pq[ch % 2]
                for k in range(KC):
                    self.MM(p[:, 0:n], win[:, k, ch * 128:(ch + 1) * 128], uT[:, k, 0:n], k == 0, k == KC - 1, win.b + [uT.b[k]], p.b)
                self.sumsq_rstd(lambda k: p[:, 0:n], p.b, 1, n, 128, W, rs2)
                gcol = self.small[:, 6:7] if ch < 8 else self.small[:, 7:8]
                o_ = qo[ch % 3]
                self.rope_chunk(R, p, 128, n, lambda dst: self.TS(dst[:, 0:n], p[:, 0:n], gcol, None, ALU.mult, None, p.b + self.small.b, dst.b),
                                cb, sbb, R["perm"], o_[:, 0:n], o_.b, rs=rs2)
                dst = self.sA[ch * 128:(ch + 1) * 128, t0:t0 + n] if ch < 8 else self.sB[(ch - 8) * 128:(ch - 7) * 128, t0:t0 + n]
                self.DMA("pool", "g_qo%d" % (ch % 3), dst, o_[:, 0:n], r=o_.b)
            for j in range(n // 128):
                for k in range(KC):
                    self.MM(pv[:, 0:256], uT[:, k, j * 128:(j + 1) * 128], win[:, k, 1280:1536], k == 0, k == KC - 1, win.b + [uT.b[k]], pv.b)
                v_ = vt[j % 2]
                self.CP(v_[:], pv[:, 0:256], pv.b, v_.b, eng="act")
                self.DMA("pool", "g_vt%d" % (j % 2), self.sV[t0 + j * 128:t0 + (j + 1) * 128, 0:256], v_[:], r=v_.b)
        self.S.barrier()


def attn_qblocks(do_ctx=True):
    out = []
    if do_ctx:
        out.append((0, 256, [0, 1]))
    for b in range(8):
        out.append((CTX + 512 * b, 512, list(range(NT))))
    return out


def gqa_p2(self):
    with ExitStack() as es:
        A = self.attn_tiles(es)
        KT = self.sb(es, "g2_K", [128, T], BF16)
        V = self.sb(es, "g2_V", [128, NT, 128], BF16)
        QT = [self.sb(es, "g2_Q%d" % j, [128, T], BF16) for j in range(2)]
        ob = [self.sb(es, "g2_o%d" % j, [128, 512], BF16) for j in range(2)]
        scale = 128 ** -0.5
        cnt = 0
        for kvh in range(2):
            self.DMA("sp", "g2_K", KT[:], self.sB[kvh * 128:(kvh + 1) * 128, :], w=KT.b)
            self.DMA("sp", "g2_V", V[:], self.sV.rearrange("(t p) d -> p t d", p=128)[:, :, kvh * 128:(kvh + 1) * 128], w=V.b)
            for g in range(4):
                h = kvh * 4 + g
                Q = QT[h % 2]
                self.DMA("sp", "g2_Q%d" % (h % 2), Q[:], self.sA[h * 128:(h + 1) * 128, :], w=Q.b)
                for (q0, nq, kts) in attn_qblocks(True):
                    o_ = ob[cnt % 2]
                    def after(o_=o_, c=cnt % 2, h=h, q0=q0, nq=nq):
                        self.DMA("sp", "g2_o%d" % c, self.sOT[h * 128:(h + 1) * 128, q0:q0 + nq], o_[:, 0:nq], r=o_.b)
                    self.attn_block(A, lambda kt: KT[:, kt * 128:(kt + 1) * 128], Q[:, q0:q0 + nq], lambda kt: V[:, kt, :], kts, nq, 128, scale,
                                    KT.b + V.b + Q.b, o_[:, 0:nq], o_.b, after=after)
                    cnt += 1
        self.attn_flush(A)
        self.S.barrier()


Builder.gqa_p1 = gqa_p1
Builder.gqa_p2 = gqa_p2


def build(layers=(0, 1, 2, 3), debug=False):
    B = Builder(len(layers), debug)
    I = B.I
    es = ExitStack()
    B.setup(es)
    for i in layers:
        B.mod_phase(i)
        if i == 0:
            B.hgrn_p1()
            B.hgrn_p2()
            B.hgrn_p2b()
            wout = I["a_w_out"]
        elif i == 1:
            B.mla_p1()
            B.mla_p2()
            wout = I["b_w_out"]
        elif i == 2:
            B.gqa_p1()
            B.gqa_p2()
            wout = I["c_w_out"]
        else:
            B.diff_p1()
            B.diff_p2()
            wout = I["d_w_out"]
        B.ffn_phase(i, wout)
    B.final_out()
    B.S.barrier()
    B.S.emit()
    return B.nc


def prep_shared(inp):
    f = lambda a: np.ascontiguousarray(a, dtype=np.float32)
    sh = {}
    sh["c_ctx"] = f(inp["c_ctx"]).reshape(1, D)
    sh["mod_w"] = f(inp["mod_w"])
    sh["mod_b"] = f(inp["mod_b"]).reshape(24, D)
    sh["norm_g"] = f(inp["norm_g"]).reshape(16, D)
    sh["a_w_in"] = f(inp["a_w_in"][0])
    sh["a_lb"] = f(inp["a_lb"]).reshape(10, D)
    sh["a_onorm_g"] = f(inp["a_onorm_g"]).reshape(1, 128)
    sh["a_w_out"] = f(inp["a_w_out"][0])
    sh["b_w_in"] = f(inp["b_w_in"][0])
    sh["b_qnorm_g"] = f(inp["b_qnorm_g"]).reshape(3, 128)
    sh["b_kvnorm_g"] = f(inp["b_kvnorm_g"]).reshape(2, 128)
    sh["b_w_qb"] = f(inp["b_w_qb"][0])
    kvb = f(inp["b_w_kvb"][0]).reshape(256, 16, 2, 64)
    sh["b_w_kvb"] = np.ascontiguousarray(kvb.transpose(0, 2, 1, 3).reshape(256, 2048))
    sh["b_w_out"] = f(inp["b_w_out"][0])
    sh["c_w_in"] = f(inp["c_w_in"][0])
    sh["c_qnorm_g"] = f(inp["c_qnorm_g"]).reshape(1, 128)
    sh["c_knorm_g"] = f(inp["c_knorm_g"]).reshape(1, 128)
    sh["c_w_out"] = f(inp["c_w_out"][0])
    sh["d_w_in"] = f(inp["d_w_in"][0])
    sh["d_lambda"] = f(inp["d_lambda"]).reshape(1, 256)
    sh["d_onorm_g"] = f(inp["d_onorm_g"]).reshape(1, 128)
    sh["d_w_out"] = f(inp["d_w_out"][0])
    g = f(inp["ffn_w_gu"]).reshape(2, KC, 128, 2, FC, 128)
    sh["ffn_w_gu"] = np.ascontiguousarray(g.transpose(0, 4, 2, 1, 3, 5).reshape(2, FC, 128, KC * 256))
    d_ = f(inp["ffn_w_down"]).reshape(2, FC, 128, KC, 128)
    sh["ffn_w_down"] = np.ascontiguousarray(d_.transpose(0, 3, 2, 1, 4).reshape(2, KC, 128, FF))
    sh["moe_router"] = f(inp["moe_router"])
    g = f(inp["moe_w_gu"]).reshape(2, NE, KC, 128, 2, FC, 128)
    sh["moe_w_gu"] = np.ascontiguousarray(g.transpose(0, 1, 5, 3, 2, 4, 6).reshape(2, NE, FC, 128, KC * 256))
    d_ = f(inp["moe_w_down"]).reshape(2, NE, FC, 128, KC, 128)
    sh["moe_w_down"] = np.ascontiguousarray(d_.transpose(0, 1, 4, 3, 2, 5).reshape(2, NE, KC, 128, FF))
    sh.update(_consts())
    return sh


def kernel(**inp):
    nc = build()
    sh = prep_shared(inp)
    in_maps = []
    for b in range(8):
        m = dict(sh)
        m["x"] = np.ascontiguousarray(inp["x"][b], dtype=np.float32)
        m["c"] = np.ascontiguousarray(inp["c"][b:b + 1], dtype=np.float32)
        m["ctx"] = np.ascontiguousarray(inp["ctx"][b], dtype=np.float32)
        in_maps.append(m)
    res = run_bass_kernel_spmd(nc, in_maps, core_ids=list(range(8)))
    return np.stack([np.asarray(r["out"], dtype=np.float32) for r in res.results], axis=0)


def diff_p1(self):
    I = self.I
    with ExitStack() as es:
        W = self.work_tiles(es)
        R = self.rope_tiles(es, "cos3", "sin3", "perm3", 128)
        win = self.sb(es, "d_win", [128, KC, 3072], BF16)
        self.load_w(win, I["d_w_in"], "d_win")
        uT = self.sb(es, "d_uT", [128, KC, 512], BF16, n=KC)
        hblk = [self.sb(es, "d_h%d" % j, [128, KC, 512], F32) for j in range(2)]
        pq = [self.ps(es, "d_pq%d" % j, [128, 512]) for j in range(2)]
        pv = [self.ps(es, "d_pv%d" % j, [128, 512]) for j in range(2)]
        qo = [self.sb(es, "d_qo%d" % j, [128, 512], BF16) for j in range(3)]
        vt = [self.sb(es, "d_vt%d" % j, [128, 1024], BF16) for j in range(2)]
        for bi, (t0, n) in enumerate(P1_BLOCKS):
            hb = hblk[bi % 2]
            self.load_h(hb, t0, n, "d_h%d" % (bi % 2))
            cb, sbb = self.rope_load(R, bi, t0, n)
            self.norm_mod(hb, t0, n, 0, uT, 0, W)
            for ch in range(16):
                p = pq[ch % 2]
                for k in range(KC):
                    self.MM(p[:, 0:n], win[:, k, ch * 128:(ch + 1) * 128], uT[:, k, 0:n], k == 0, k == KC - 1, win.b + [uT.b[k]], p.b)
                o_ = qo[ch % 3]
                self.rope_chunk(R, p, 128, n, lambda dst: self.CP(dst[:, 0:n], p[:, 0:n], p.b, dst.b, eng="act"),
                                cb, sbb, R["perm"], o_[:, 0:n], o_.b)
                dst = self.sA[ch * 128:(ch + 1) * 128, t0:t0 + n] if ch < 8 else self.sB[(ch - 8) * 128:(ch - 7) * 128, t0:t0 + n]
                self.DMA("pool", "d_qo%d" % (ch % 3), dst, o_[:, 0:n], r=o_.b)
            for j in range(n // 128):
                v_ = vt[j % 2]
                for hh in range(2):
                    p = pv[hh]
                    for k in range(KC):
                        self.MM(p[:, :], uT[:, k, j * 128:(j + 1) * 128], win[:, k, 2048 + hh * 512:2048 + (hh + 1) * 512], k == 0, k == KC - 1,
                                win.b + [uT.b[k]], p.b)
                    self.CP(v_[:, hh * 512:(hh + 1) * 512], p[:, :], p.b, v_.b, eng=("act" if hh else "dve"))
                self.DMA("pool", "d_vt%d" % (j % 2), self.sV[t0 + j * 128:t0 + (j + 1) * 128, :], v_[:], r=v_.b)
        self.S.barrier()


def diff_p2(self, i=3):
    I = self.I
    lam_init = 0.8 - 0.6 * math.exp(-0.3 * i)
    with ExitStack() as es:
        A = self.attn_tiles(es, 4)
        W = self.work_tiles(es)
        KT = [self.sb(es, "d2_K%d" % j, [128, T], BF16) for j in range(2)]
        V = [self.sb(es, "d2_V%d" % j, [128, NT, 128], BF16) for j in range(2)]
        QT = [self.sb(es, "d2_Q%d" % j, [128, T], BF16) for j in range(2)]
        ams = [[self.sb(es, "d2_a%d%d" % (i, j), [128, 512], F32) for j in range(2)] for i in range(2)]
        od = self.sb(es, "d2_od", [128, 512], F32)
        ob = [self.sb(es, "d2_o%d" % j, [128, 512], BF16) for j in range(2)]
        lt = self.sb(es, "d2_lam", [128, 256], F32)
        lc = self.sb(es, "d2_lc", [128, 8], F32)
        self.DMA("sp", "d2_lam", lt[:], I["d_lambda"][0:1, :].to_broadcast([128, 256]), w=lt.b)
        for q in range(2):
            self.TT(lt[:, q * 128:q * 128 + 64], lt[:, q * 128:q * 128 + 64], lt[:, q * 128 + 64:q * 128 + 128], ALU.mult, lt.b, lt.b)
            self.S.op("dve", lambda e, q=q: e.reduce_sum(out=lc[:, q:q + 1], in_=lt[:, q * 128:q * 128 + 64], axis=mybir.AxisListType.X), lt.b, lc.b)
        self.ACT(lc[:, 2:4], lc[:, 0:2], AF.Exp, lc.b, lc.b)
        self.TT(lc[:, 4:5], lc[:, 3:4], lc[:, 2:3], ALU.subtract, lc.b, lc.b)
        self.TS(lc[:, 5:6], lc[:, 4:5], -lam_init, None, ALU.add, None, lc.b, lc.b)
        self.TS(lc[:, 6:7], self.small[:, 8:9], 1.0 - lam_init, None, ALU.mult, None, lc.b + self.small.b, lc.b)
        scale = 64 ** -0.5
        cnt = 0
        for h in range(8):
            K_, V_, Q_ = KT[h % 2], V[h % 2], QT[h % 2]
            self.DMA("sp", "d2_K%d" % (h % 2), K_[:], self.sB[h * 128:(h + 1) * 128, :], w=K_.b)
            self.DMA("sp", "d2_V%d" % (h % 2), V_[:], self.sV.rearrange("(t p) d -> p t d", p=128)[:, :, h * 128:(h + 1) * 128], w=V_.b)
            self.DMA("sp", "d2_Q%d" % (h % 2), Q_[:], self.sA[h * 128:(h + 1) * 128, :], w=Q_.b)
            for (q0, nq, kts) in attn_qblocks(False):
                am = ams[cnt % 2]
                o_ = ob[cnt % 2]

                def combine(am=am, o_=o_, c=cnt % 2, h=h, q0=q0, nq=nq):
                    self.STT(od[:, 0:nq], am[1][:, 0:nq], lc[:, 5:6], am[0][:, 0:nq], ALU.mult, ALU.add, am[0].b + am[1].b + lc.b, od.b)
                    self.sumsq_rstd(lambda k: od[:, 0:nq], od.b, 1, nq, 128, W, W["rs"])
                    self.STT(o_[:, 0:nq], od[:, 0:nq], lc[:, 6:7], W["rs"][:, 0:nq], ALU.mult, ALU.mult, od.b + lc.b + W["rs"].b, o_.b)
                    self.DMA("sp", "d2_o%d" % c, self.sOT[h * 128:(h + 1) * 128, q0:q0 + nq], o_[:, 0:nq], r=o_.b)
                for m in range(2):
                    lo, hi = 64 * m, 64 * m + 64
                    self.attn_block(A, lambda kt, K_=K_, lo=lo, hi=hi: K_[lo:hi, kt * 128:(kt + 1) * 128], Q_[lo:hi, q0:q0 + nq],
                                    lambda kt, V_=V_: V_[:, kt, :], kts, nq, 128, scale,
                                    K_.b + V_.b + Q_.b, am[m][:, 0:nq], am[m].b, after=(combine if m == 1 else None))
                cnt += 1
        self.attn_flush(A)
        self.S.barrier()


Builder.diff_p1 = diff_p1
Builder.diff_p2 = diff_p2


def mla_p1(self):
    I = self.I
    with ExitStack() as es:
        W = self.work_tiles(es)
        R = self.rope_tiles(es, "cos1", "sin1", "perm1", 96)
        ck = self.sb(es, "m_cosk", [32, T], F32)
        sk = self.sb(es, "m_sink", [32, T], F32)
        pkf = self.sb(es, "m_permkf", [32, 32], F32)
        pkb = self.sb(es, "m_permk", [32, 32], BF16)
        self.DMA("sp", "m_ck", ck[:], I["cos1k"][:, :], w=ck.b)
        self.DMA("sp", "m_sk", sk[:], I["sin1k"][:, :], w=sk.b)
        self.DMA("sp", "m_pk", pkf[:], I["perm1k"][:, :], w=pkf.b)
        self.CP(pkb[:], pkf[:], pkf.b, pkb.b)
        win = self.sb(es, "m_win", [128, KC, 768], BF16)
        wqb = self.sb(es, "m_wqb", [128, 3, 1536], BF16)
        wkvb = self.sb(es, "m_wkvb", [128, 2, 2048], BF16)
        for k in range(KC):
            self.DMA("pool", "m_win", win[:, k, 0:672], I["b_w_in"].rearrange("(k p) n -> p k n", p=128)[:, k, :], w=win.b)
        self.load_w(wqb, I["b_w_qb"], "m_wqb")
        self.load_w(wkvb, I["b_w_kvb"], "m_wkvb")
        for k in range(3 if 'scale' in MLA_PARTS else 0):
            self.TS(wqb[:, k, :], wqb[:, k, :], self.small[:, 1 + k:2 + k], None, ALU.mult, None, wqb.b + self.small.b, wqb.b)
        for k in range(2 if 'scale' in MLA_PARTS else 0):
            self.TS(wkvb[:, k, :], wkvb[:, k, :], self.small[:, 4 + k:5 + k], None, ALU.mult, None, wkvb.b + self.small.b, wkvb.b)
        uT = self.sb(es, "m_uT", [128, KC, 512], BF16, n=KC)
        hblk = [self.sb(es, "m_h%d" % j, [128, KC, 512], F32) for j in range(2)]
        pq = [self.ps(es, "m_pq%d" % j, [128, 512]) for j in range(2)]
        pv = [self.ps(es, "m_pv%d" % j, [128, 512]) for j in range(2)]
        pss2 = self.ps(es, "m_pss2", [128, 512])
        cqT = self.sb(es, "m_cqT", [128, 3, 512], BF16)
        ckvT = self.sb(es, "m_ckvT", [128, 2, 512], BF16)
        sqq = [self.sb(es, "m_sqq%d" % j, [128, 512], BF16) for j in range(3)]
        sqkv = [self.sb(es, "m_sqkv%d" % j, [128, 512], BF16) for j in range(2)]
        rsq = self.sb(es, "m_rsq", [128, 512], F32)
        rskv = self.sb(es, "m_rskv", [128, 512], F32)
        rcol = self.sb(es, "m_rcol", [128, 4], F32)
        qo = [self.sb(es, "m_qo%d" % j, [128, 512], BF16) for j in range(3)]
        vt = [self.sb(es, "m_vt%d" % j, [128, 1024], BF16) for j in range(2)]
        for bi, (t0, n) in enumerate(P1_BLOCKS):
            hb = hblk[bi % 2]
            self.load_h(hb, t0, n, "m_h%d" % (bi % 2))
            cb, sbb = self.rope_load(R, bi, t0, n)
            self.norm_mod(hb, t0, n, 0, uT, 0, W)
            if 'lat' not in MLA_PARTS:
                continue
            for ch in range(5):
                p = pq[ch % 2]
                for k in range(KC):
                    self.MM(p[:, 0:n], win[:, k, ch * 128:(ch + 1) * 128], uT[:, k, 0:n], k == 0, k == KC - 1, win.b + [uT.b[k]], p.b)
                if 'nosq' in MLA_PARTS:
                    continue
                if ch < 3:
                    sq = sqq[ch]
                    self.ACT(sq[:, 0:n], p[:, 0:n], AF.Square, p.b, sq.b)
                    self.CP(cqT[:, ch, 0:n], p[:, 0:n], p.b, cqT.b)
                else:
                    sq = sqkv[ch - 3]
                    self.ACT(sq[:, 0:n], p[:, 0:n], AF.Square, p.b, sq.b)
                    self.CP(ckvT[:, ch - 3, 0:n], p[:, 0:n], p.b, ckvT.b)
            if 'nosq' in MLA_PARTS or 'noones' in MLA_PARTS:
                continue
            for ch in range(3):
                self.MM(W["pss"][:, 0:n], self.onesb[:], sqq[ch][:, 0:n], ch == 0, ch == 2, self.onesb.b + sqq[ch].b, W["pss"].b)
            for ch in range(2):
                self.MM(pss2[:, 0:n], self.onesb[:], sqkv[ch][:, 0:n], ch == 0, ch == 1, self.onesb.b + sqkv[ch].b, pss2.b)
            self.rstd(rsq[:, 0:n], W["pss"][:, 0:n], 384, W["pss"].b + self.epsb, rsq.b, W["rt"][:, 0:n], W["rt"].b)
            self.rstd(rskv[:, 0:n], pss2[:, 0:n], 256, pss2.b + self.epsb, rskv.b, W["rt"][:, 0:n], W["rt"].b)
            p = pq[1]
            if 'kr' not in MLA_PARTS:
                continue
            for k in range(KC):
                self.MM(p[0:32, 0:n], win[:, k, 640:672], uT[:, k, 0:n], k == 0, k == KC - 1, win.b + [uT.b[k]], p.b)
            o_ = qo[0]
            self.rope_chunk(R, p, 32, n, lambda dst: self.CP(dst[0:32, 0:n], p[0:32, 0:n], p.b, dst.b, eng="act"),
                            self.view(ck, ck.t[:, t0:t0 + n]), self.view(sk, sk.t[:, t0:t0 + n]), pkb, o_[0:32, 0:n], o_.b)
            self.DMA("pool", "m_qo0", self.sKR[:, t0:t0 + n], o_[0:32, 0:n], r=o_.b)
            for h in range(16 if 'q' in MLA_PARTS else 0):
                p = pq[h % 2]
                for k in range(3):
                    self.MM(p[0:96, 0:n], wqb[:, k, h * 96:(h + 1) * 96], cqT[:, k, 0:n], k == 0, k == 2, wqb.b + cqT.b, p.b)
                o_ = qo[(h + 1) % 3]
                self.rope_chunk(R, p, 96, n, lambda dst: self.TT(dst[0:96, 0:n], p[0:96, 0:n], rsq[0:96, 0:n], ALU.mult, p.b + rsq.b, dst.b),
                                cb, sbb, R["perm"], o_[0:96, 0:n], o_.b)
                self.DMA("pool", "m_qo%d" % ((h + 1) % 3), self.sA[h * 96:(h + 1) * 96, t0:t0 + n], o_[0:96, 0:n], r=o_.b)
            for j in range(8 if 'kn' in MLA_PARTS else 0):
                p = pq[j % 2]
                for k in range(2):
                    self.MM(p[:, 0:n], wkvb[:, k, j * 128:(j + 1) * 128], ckvT[:, k, 0:n], k == 0, k == 1, wkvb.b + ckvT.b, p.b)
                o_ = qo[j % 3]
                self.TT(o_[:, 0:n], p[:, 0:n], rskv[:, 0:n], ALU.mult, p.b + rskv.b, o_.b)
                self.DMA("pool", "m_qo%d" % (j % 3), self.sB[j * 128:(j + 1) * 128, t0:t0 + n], o_[:, 0:n], r=o_.b)
            for j in range(n // 128 if 'v' in MLA_PARTS else 0):
                v_ = vt[j % 2]
                for k in range(2):
                    self.MM(pss2[:, 0:8], sqkv[k][:, j * 128:(j + 1) * 128], self.onesb[:, 0:8], k == 0, k == 1, sqkv[k].b + self.onesb.b, pss2.b)
                self.ACT(rcol[:, 0:1], pss2[:, 0:1], AF.Sqrt, pss2.b + self.epsb, rcol.b, bias=self.epsc[:, :], scale=1.0 / 256)
                self.RCP(rcol[:, 1:2], rcol[:, 0:1], rcol.b, rcol.b)
                for hh in range(2):
                    p = pv[hh]
                    for k in range(2):
                        self.MM(p[:, :], ckvT[:, k, j * 128:(j + 1) * 128], wkvb[:, k, 1024 + hh * 512:1024 + (hh + 1) * 512], k == 0, k == 1,
                                wkvb.b + ckvT.b, p.b)
                    self.TS(v_[:, hh * 512:(hh + 1) * 512], p[:, :], rcol[:, 1:2], None, ALU.mult, None, p.b + rcol.b, v_.b)
                self.DMA("pool", "m_vt%d" % (j % 2), self.sV[t0 + j * 128:t0 + (j + 1) * 128, :], v_[:], r=v_.b)
        self.S.barrier()


def mla_p2(self):
    with ExitStack() as es:
        A = self.attn_tiles(es)
        KT = [self.sb(es, "m2_K%d" % j, [96, T], BF16) for j in range(2)]
        V = [self.sb(es, "m2_V%d" % j, [128, NT, 64], BF16) for j in range(2)]
        QT = [self.sb(es, "m2_Q%d" % j, [96, T], BF16) for j in range(2)]
        ob = [self.sb(es, "m2_o%d" % j, [64, 512], BF16) for j in range(2)]
        scale = 96 ** -0.5
        cnt = 0
        for h in range(16):
            K_, V_, Q_ = KT[h % 2], V[h % 2], QT[h % 2]
            self.DMA("sp", "m2_K%d" % (h % 2), K_[0:64, :], self.sB[h * 64:(h + 1) * 64, :], w=K_.b)
            self.DMA("sp", "m2_K%d" % (h % 2), K_[64:96, :], self.sKR[:, :], w=K_.b)
            self.DMA("sp", "m2_V%d" % (h % 2), V_[:], self.sV.rearrange("(t p) d -> p t d", p=128)[:, :, h * 64:(h + 1) * 64], w=V_.b)
            self.DMA("sp", "m2_Q%d" % (h % 2), Q_[:], self.sA[h * 96:(h + 1) * 96, :], w=Q_.b)
            for (q0, nq, kts) in attn_qblocks(True):
                o_ = ob[cnt % 2]
                def after(o_=o_, c=cnt % 2, h=h, q0=q0, nq=nq):
                    self.DMA("sp", "m2_o%d" % c, self.sOT[h * 64:(h + 1) * 64, q0:q0 + nq], o_[:, 0:nq], r=o_.b)
                self.attn_block(A, lambda kt, K_=K_: K_[:, kt * 128:(kt + 1) * 128], Q_[:, q0:q0 + nq], lambda kt, V_=V_: V_[:, kt, :], kts, nq, 64, scale,
                                K_.b + V_.b + Q_.b, o_[:, 0:nq], o_.b, after=after)
                cnt += 1
        self.attn_flush(A)
        self.S.barrier()


Builder.mla_p1 = mla_p1
Builder.mla_p2 = mla_p2


def hgrn_p1(self):
    I = self.I
    with ExitStack() as es:
        W = self.work_tiles(es)
        win = self.sb(es, "h_win", [128, KC, 5120], BF16)
        self.load_w(win, I["a_w_in"], "h_win")
        uT = self.sb(es, "h_uT", [128, KC, 512], BF16, n=KC)
        hblk = [self.sb(es, "h_h%d" % j, [128, KC, 512], F32) for j in range(2)]
        pq = [self.ps(es, "h_pq%d" % j, [128, 512]) for j in range(3)]
        pv = [self.ps(es, "h_pv%d" % j, [128, 512]) for j in range(2)]
        ob = [self.sb(es, "h_ob%d" % j, [128, 512], BF16) for j in range(3)]
        sg = [self.sb(es, "h_sg%d" % j, [128, 512], F32) for j in range(2)]
        ff = [self.sb(es, "h_ff%d" % j, [128, 512], F32) for j in range(2)]
        lf = [self.sb(es, "h_lf%d" % j, [128, 512], F32) for j in range(2)]
        kk = [self.sb(es, "h_kk%d" % j, [128, 512], BF16) for j in range(2)]
        vt = [self.sb(es, "h_vt%d" % j, [128, 1024], BF16) for j in range(2)]
        c = 0
        for bi, (t0, n) in enumerate(P1_BLOCKS):
            hb = hblk[bi % 2]
            self.load_h(hb, t0, n, "h_h%d" % (bi % 2))
            self.norm_mod(hb, t0, n, 0, uT, 0, W)
            for ch in range(32):
                p = pq[c % 3]
                for k in range(KC):
                    self.MM(p[:, 0:n], win[:, k, ch * 128:(ch + 1) * 128] if ch < 24 else win[:, k, 4096 + (ch - 24) * 128:4096 + (ch - 23) * 128],
                            uT[:, k, 0:n], k == 0, k == KC - 1, win.b + [uT.b[k]], p.b)
                if ch < 8:
                    o_ = ob[c % 3]
                    self.TS(o_[:, 0:n], p[:, 0:n], 128 ** -0.5, None, ALU.mult, None, p.b, o_.b)
                    self.DMA("pool", "h_ob%d" % (c % 3), self.sA[ch * 128:(ch + 1) * 128, t0:t0 + n], o_[:, 0:n], r=o_.b)
                elif ch < 24:
                    d, hh = (ch - 8) // 8, (ch - 8) % 8
                    s_, f_, l_, k_ = sg[c % 2], ff[c % 2], lf[c % 2], kk[c % 2]
                    self.ACT(s_[:, 0:n], p[:, 0:n], AF.Sigmoid, p.b, s_.b)
                    self.TS(f_[:, 0:n], s_[:, 0:n], self.lbc[:, hh, d, 1:2], self.lbc[:, hh, d, 0:1], ALU.mult, ALU.add, s_.b + self.lbc.b, f_.b)
                    self.ACT(l_[:, 0:n], f_[:, 0:n], AF.Ln, f_.b, l_.b)
                    self.TS(k_[:, 0:n], f_[:, 0:n], -1.0, 1.0, ALU.mult, ALU.add, f_.b, k_.b)
                    self.DMA("pool", "h_lf%d" % (c % 2), (self.sF1 if d == 0 else self.sF2)[hh * 128:(hh + 1) * 128, t0:t0 + n], l_[:, 0:n], r=l_.b)
                    self.DMA("pool", "h_kk%d" % (c % 2), (self.sB if d == 0 else self.sC)[hh * 128:(hh + 1) * 128, t0:t0 + n], k_[:, 0:n], r=k_.b)
                else:
                    hh = ch - 24
                    o_ = ob[c % 3]
                    self.ACT(o_[:, 0:n], p[:, 0:n], AF.Silu, p.b, o_.b)
                    self.DMA("pool", "h_ob%d" % (c % 3), self.sG[hh * 128:(hh + 1) * 128, t0:t0 + n], o_[:, 0:n], r=o_.b)
                c += 1
            for j in range(n // 128):
                v_ = vt[j % 2]
                for hh in range(2):
                    p = pv[hh]
                    for k in range(KC):
                        self.MM(p[:, :], uT[:, k, j * 128:(j + 1) * 128], win[:, k, 3072 + hh * 512:3072 + (hh + 1) * 512], k == 0, k == KC - 1,
                                win.b + [uT.b[k]], p.b)
                    self.CP(v_[:, hh * 512:(hh + 1) * 512], p[:, :], p.b, v_.b, eng=("act" if hh else "dve"))
                self.DMA("pool", "h_vt%d" % (j % 2), self.sV[t0 + j * 128:t0 + (j + 1) * 128, :], v_[:], r=v_.b)
        self.S.barrier()


def hgrn_p2(self):
    I = self.I
    NU = 16
    with ExitStack() as es:
        mk = [self.sb(es, "h2_mask%d" % d, [128, 128], F32) for d in range(2)]
        self.DMA("sp", "h2_m0", mk[0][:], I["maskf"][:, :], w=mk[0].b)
        self.DMA("sp", "h2_m1", mk[1][:], I["maskb"][:, :], w=mk[1].b)
        St = self.sb(es, "h2_S", [128, NU, 128], F32, n=NU)
        self.MEMSET(St[:], 0.0, St.b)
        onesr = self.sb(es, "h2_ones", [128, 128], F32)
        self.MEMSET(onesr[:], 1.0, onesr.b)
        f32t = {nm: self.sb(es, "h2_" + nm, [128, NU, 128], F32, n=NU) for nm in ("cpre", "base", "e1", "e2", "e3")}
        bft = {nm: self.sb(es, "h2_" + nm, [128, NU, 128], BF16, n=NU) for nm in ("qd", "qi", "attm", "ketm", "Sbf")}
        ekt = [self.sb(es, "h2_ek%d" % j, [128, NU, 128], F32, n=NU) for j in range(4)]
        kdt = [self.sb(es, "h2_kd%d" % j, [128, NU, 128], BF16, n=NU) for j in range(4)]
        for j in range(4):
            self.MEMSET(kdt[j][:], 0.0, kdt[j].b)
        bft["ke"] = self.sb(es, "h2_ke", [128, NU, 128], F32, n=NU)
        cols = self.sb(es, "h2_cols", [128, NU, 16], F32, n=NU)
        self.MEMSET(cols[:], 0.0, cols.b)
        ld = {}
        for d in range(2):
            for j in range(2):
                ld[(d, j)] = dict(q=self.sb(es, "h2_q%d%d" % (d, j), [128, 8, 128], BF16), k=self.sb(es, "h2_k%d%d" % (d, j), [128, 8, 128], BF16),
                                  lf=self.sb(es, "h2_lf%d%d" % (d, j), [128, 8, 128], F32), v=self.sb(es, "h2_v%d%d" % (d, j), [128, 1024], BF16),
                                  o=self.sb(es, "h2_o%d%d" % (d, j), [128, 8, 128], F32, n=8))
        pA = [self.ps(es, "h2_pA%d" % j, [128, 4, 128], F32) for j in range(2)]
        pO = [self.ps(es, "h2_pO%d" % j, [128, 4, 128], F32) for j in range(2)]
        pS = [self.ps(es, "h2_pS%d" % j, [128, 4, 128], F32) for j in range(2)]
        pT = [self.ps(es, "h2_pT%d" % j, [128, 4, 128], F32) for j in range(2)]
        order = [list(range(NT)), [1, 0] + list(range(NT - 1, 1, -1))]
        srcK = [self.sB, self.sC]
        srcL = [self.sF1, self.sF2]
        dstO = [self.sF3, self.sF4]
        for step in range(min(NT, HG_STEPS)):
            L = []
            for d in range(2):
                tl = order[d][step]
                c0 = tl * 128
                b = ld[(d, step % 2)]
                key = "h2_%d%d" % (d, step % 2)
                self.DMA("sp", key + "q", b["q"][:], self.sA[0:1024, :].rearrange("(h p) t -> p h t", p=128)[:, :, c0:c0 + 128], w=b["q"].b)
                self.DMA("sp", key + "k", b["k"][:], srcK[d].rearrange("(h p) t -> p h t", p=128)[:, :, c0:c0 + 128], w=b["k"].b)
                self.DMA("sp", key + "l", b["lf"][:], srcL[d].rearrange("(h p) t -> p h t", p=128)[:, :, c0:c0 + 128], w=b["lf"].b)
                self.DMA("sp", key + "v", b["v"][:], self.sV[c0:c0 + 128, :], w=b["v"].b)
                L.append(b)
            units = [(d, h) for d in range(2) for h in range(8)]
            for u, (d, h) in enumerate(units):
                if HG_STOP < 1:
                    break
                b = L[d]
                cp, ba, e1, e2, e3 = (f32t[nm] for nm in ("cpre", "base", "e1", "e2", "e3"))
                cl = cols[:, u, :]
                cb_ = [cols.b[u]]
                self.S.op("dve", lambda e, o=cp[:, u, :], x=b["lf"][:, h, :]: e.tensor_tensor_scan(out=o, data0=onesr[:], data1=x, initial=0.0,
                                                                                            op0=ALU.mult, op1=ALU.add),
                          b["lf"].b + onesr.b, [cp.b[u]])
                tot = cp[:, u, 127:128]
                if d == 0:
                    base, baseb = cp[:, u, :], [cp.b[u]]
                    self.CP(cl[:, 1:4], cp[:, u, 31:96:32], [cp.b[u]], cb_)
                    self.TS(cl[:, 5:8], cp[:, u, 31:96:32], -1.0, None, ALU.mult, None, [cp.b[u]], cb_)
                else:
                    self.TT(ba[:, u, :], b["lf"][:, h, :], cp[:, u, :], ALU.subtract, b["lf"].b + [cp.b[u]], [ba.b[u]])
                    base, baseb = ba[:, u, :], [ba.b[u]]
                    self.CP(cl[:, 0:3], ba[:, u, 32:128:32], baseb, cb_)
                    self.TS(cl[:, 4:7], ba[:, u, 32:128:32], -1.0, None, ALU.mult, None, baseb, cb_)
                    self.TS(cl[:, 3:4], tot, -1.0, None, ALU.mult, None, [cp.b[u]], cb_)
                    self.CP(cl[:, 7:8], tot, [cp.b[u]], cb_)
                rb_ = baseb + cb_ + [cp.b[u]]
                for j in range(4):
                    sl = slice(32 * j, 32 * j + 32)
                    rng = slice(0, 32 * j + 32) if d == 0 else slice(32 * j, 128)
                    if d == 0 and j == 0:
                        self.ACT(e1[:, u, sl], base[:, sl], AF.Exp, rb_, [e1.b[u]])
                        self.ACT(ekt[j][:, u, rng], base[:, rng], AF.Exp, rb_, [ekt[j].b[u]], scale=-1.0)
                    else:
                        self.ACT(e1[:, u, sl], base[:, sl], AF.Exp, rb_, [e1.b[u]], bias=cl[:, 4 + j:5 + j], scale=1.0)
                        self.ACT(ekt[j][:, u, rng], base[:, rng], AF.Exp, rb_, [ekt[j].b[u]], bias=cl[:, j:j + 1], scale=-1.0)
                    self.TT(kdt[j][:, u, rng], b["k"][:, h, rng], ekt[j][:, u, rng], ALU.mult, b["k"].b + [ekt[j].b[u]], [kdt[j].b[u]], eng="pool")
                if d == 0:
                    self.ACT(e2[:, u, :], base, AF.Exp, rb_, [e2.b[u]])
                    self.ACT(e3[:, u, :], base, AF.Exp, rb_, [e3.b[u]], bias=tot, scale=-1.0)
                else:
                    self.ACT(e2[:, u, :], base, AF.Exp, rb_, [e2.b[u]], bias=tot, scale=1.0)
                    self.ACT(e3[:, u, :], base, AF.Exp, rb_, [e3.b[u]], scale=-1.0)
                self.ACT(cl[:, 8:9], tot, AF.Exp, [cp.b[u]], cb_)
                self.TT(bft["qd"][:, u, :], b["q"][:, h, :], e1[:, u, :], ALU.mult, b["q"].b + [e1.b[u]], [bft["qd"].b[u]])
                self.TT(bft["qi"][:, u, :], b["q"][:, h, :], e2[:, u, :], ALU.mult, b["q"].b + [e2.b[u]], [bft["qi"].b[u]])
                self.TT(bft["ke"][:, u, :], b["k"][:, h, :], e3[:, u, :], ALU.mult, b["k"].b + [e3.b[u]], [bft["ke"].b[u]], eng="pool")
                self.CP(bft["Sbf"][:, u, :], St[:, u, :], [St.b[u]], [bft["Sbf"].b[u]])
            for g0 in range(0, NU, 4):
                if HG_STOP < 2:
                    break
                pa, pt_ = pA[(g0 // 4) % 2], pT[(g0 // 4) % 2]
                for u in range(g0, g0 + 4):
                    for j in range(4):
                        self.MM(pa[:, u % 4, 32 * j:32 * j + 32], kdt[j][:, u, :], bft["qd"][:, u, 32 * j:32 * j + 32], True, True,
                                [kdt[j].b[u], bft["qd"].b[u]], pa.b)
                    self.TR(pt_[:, u % 4, :], bft["ke"][:, u, :], self.ident[:], [bft["ke"].b[u]] + self.ident.b, pt_.b)
                for u in range(g0, g0 + 4):
                    d = units[u][0]
                    self.TT(bft["attm"][:, u, :], pa[:, u % 4, :], mk[d][:], ALU.mult, pa.b + mk[d].b, [bft["attm"].b[u]])
                    self.CP(bft["ketm"][:, u, :], pt_[:, u % 4, :], pt_.b, [bft["ketm"].b[u]], eng="act")
            for g0 in range(0, NU, 4):
                if HG_STOP < 3:
                    break
                po, ps_ = pO[(g0 // 4) % 2], pS[(g0 // 4) % 2]
                for u in range(g0, g0 + 4):
                    d, h = units[u]
                    b = L[d]
                    vh = b["v"][:, h * 128:(h + 1) * 128]
                    self.MM(po[:, u % 4, :], vh, bft["attm"][:, u, :], True, False, b["v"].b + [bft["attm"].b[u]], po.b)
                    self.MM(po[:, u % 4, :], bft["Sbf"][:, u, :], bft["qi"][:, u, :], False, True, [bft["Sbf"].b[u], bft["qi"].b[u]], po.b)
                    self.MM(ps_[:, u % 4, :], bft["ketm"][:, u, :], vh, True, True, b["v"].b + [bft["ketm"].b[u]], ps_.b)
                for u in range(g0, g0 + 4):
                    d, h = units[u]
                    b = L[d]
                    self.CP(b["o"][:, h, :], po[:, u % 4, :], po.b, [b["o"].b[h]], eng="act")
                    self.STT(St[:, u, :], St[:, u, :], cols[:, u, 8:9], ps_[:, u % 4, :], ALU.mult, ALU.add, [St.b[u], cols.b[u]] + ps_.b, [St.b[u]])
            for d in range(2):
                c0 = order[d][step] * 128
                self.DMA("pool", "h2_o%d%d" % (d, step % 2), dstO[d].rearrange("(h p) t -> p h t", p=128)[:, :, c0:c0 + 128], L[d]["o"][:], r=L[d]["o"].b)
        self.S.barrier()


def hgrn_p2b(self):
    with ExitStack() as es:
        W = self.work_tiles(es)
        of = [self.sb(es, "h3_of%d" % j, [128, 8, 512], F32) for j in range(2)]
        obk = [self.sb(es, "h3_ob%d" % j, [128, 8, 512], F32) for j in range(2)]
        gt = [self.sb(es, "h3_g%d" % j, [128, 8, 512], BF16) for j in range(2)]
        ot = [self.sb(es, "h3_ot%d" % j, [128, 8, 512], BF16) for j in range(2)]
        od = [self.sb(es, "h3_od%d" % j, [128, 512], F32) for j in range(2)]
        rsh = self.sb(es, "h3_rs", [128, 512], F32)
        for bi, (t0, n) in enumerate(P1_BLOCKS):
            j = bi % 2
            v = lambda a: a.rearrange("(h p) t -> p h t", p=128)[:, :, t0:t0 + n]
            self.DMA("sp", "h3_of%d" % j, of[j][:, :, 0:n], v(self.sF3), w=of[j].b)
            self.DMA("sp", "h3_ob%d" % j, obk[j][:, :, 0:n], v(self.sF4), w=obk[j].b)
            self.DMA("sp", "h3_g%d" % j, gt[j][:, :, 0:n], v(self.sG), w=gt[j].b)
            for h in range(8):
                o_ = od[h % 2]
                self.TT(o_[:, 0:n], of[j][:, h, 0:n], obk[j][:, h, 0:n], ALU.add, of[j].b + obk[j].b, o_.b, eng="pool")
                self.sumsq_rstd(lambda k: o_[:, 0:n], o_.b, 1, n, 128, W, rsh)
                self.STT(o_[:, 0:n], o_[:, 0:n], self.small[:, 0:1], rsh[:, 0:n], ALU.mult, ALU.mult, o_.b + self.small.b + rsh.b, o_.b)
                self.TT(ot[j][:, h, 0:n], o_[:, 0:n], gt[j][:, h, 0:n], ALU.mult, o_.b + gt[j].b, ot[j].b, eng="pool")
            self.DMA("pool", "h3_ot%d" % j, v(self.sOT), ot[j][:, :, 0:n], r=ot[j].b)
        self.S.barrier()


Builder.hgrn_p1 = hgrn_p1
Builder.hgrn_p2 = hgrn_p2
Builder.hgrn_p2b = hgrn_p2b
```
